# Optimizing a Trainium2 kernel written in Bass

```python
import math
import jax, jax.numpy as jnp
from jax import lax
import numpy as np

D_MODEL = 1024
BATCH = 8
SEQ = 2048
DEPTH = 4

N_A_LAYERS = DEPTH // 2
N_B_LAYERS = DEPTH - N_A_LAYERS
MIX_W = D_MODEL
MEM_LEN = 256
MEM_HEADS = 4
MEM_HEAD_DIM = 64
MEM_W = MEM_HEADS * MEM_HEAD_DIM
SEQ_MIX_W = MIX_W - MEM_W
HGRN_EXPAND = 128
HGRN_HEADS = SEQ_MIX_W // HGRN_EXPAND
HGRN_DK = HGRN_EXPAND
HGRN_DV = SEQ_MIX_W // HGRN_HEADS
HGRN_CHUNK = 64
SB_HEAD_DIM = 64
SB_HEADS = SEQ_MIX_W // SB_HEAD_DIM
SB_BLOCK = 128
A_IN_W = 4 * SEQ_MIX_W + MEM_W
B_IN_W = SEQ_MIX_W + MEM_W
N_GROUPS = 4
EXPERTS_PER_GROUP = 4
N_EXPERTS = N_GROUPS * EXPERTS_PER_GROUP
TOP_K_IN_GROUP = 2
D_EXPERT = 256
DEEPNORM_ALPHA = (2 * DEPTH) ** 0.25
DEEPNORM_BETA = (8 * DEPTH) ** -0.25
LN_EPS = 1e-5
RMS_EPS = 1e-6

kernel_name = "yoco_hgrn2_stickbreaking_hmoe_deepnorm"


def layer_norm(x, g, b):
    xf = x.astype(jnp.float32)
    mu = jnp.mean(xf, axis=-1, keepdims=True)
    var = jnp.mean(jnp.square(xf - mu), axis=-1, keepdims=True)
    y = (xf - mu) * lax.rsqrt(var + LN_EPS) * g.astype(jnp.float32) + b.astype(jnp.float32)
    return y.astype(x.dtype)


def post_norm(x, y, g, b):
    return layer_norm(DEEPNORM_ALPHA * x + y.astype(x.dtype), g, b)


def gla_chunkwise(q, k, v, logf):
    B, S, H, dk = q.shape
    dv = v.shape[-1]
    nc = S // HGRN_CHUNK

    def to_chunks(t):
        return t.reshape(B, nc, HGRN_CHUNK, H, t.shape[-1]).transpose(1, 0, 3, 2, 4)

    qc, kc, vc = to_chunks(q), to_chunks(k), to_chunks(v)
    G = jnp.cumsum(to_chunks(logf), axis=3)
    incl = jnp.tril(jnp.ones((HGRN_CHUNK, HGRN_CHUNK), dtype=bool))

    def step(state, inp):
        q_, k_, v_, G_ = inp
        diff = G_[:, :, :, None, :] - G_[:, :, None, :, :]
        decay = jnp.exp(jnp.where(incl[:, :, None], diff, -jnp.inf))
        scores = jnp.einsum('bhtd,bhsd,bhtsd->bhts', q_, k_, decay)
        out = (jnp.einsum('bhts,bhse->bhte', scores, v_)
               + jnp.einsum('bhtd,bhde->bhte', q_ * jnp.exp(G_), state))
        g_last = G_[:, :, -1]
        state = (jnp.exp(g_last)[..., None] * state
                 + jnp.einsum('bhsd,bhse->bhde', k_ * jnp.exp(g_last[:, :, None] - G_), v_))
        return state, out

    s0 = jnp.zeros((B, H, dk, dv), jnp.float32)
    _, out = lax.scan(step, s0, (qc, kc, vc, G))
    return out.transpose(1, 0, 3, 2, 4).reshape(B, S, H, dv)


def hgrn2_mix(q, f_raw, i_in, g, lb, gnorm):
    B, S, _ = q.shape
    qh = (jax.nn.silu(q.astype(jnp.float32)) * HGRN_DK ** -0.5).reshape(B, S, HGRN_HEADS, HGRN_DK)
    lbf = lb.astype(jnp.float32)
    logf = jnp.logaddexp(jnp.log(lbf), jnp.log1p(-lbf) + jax.nn.log_sigmoid(f_raw.astype(jnp.float32)))
    logf = logf.reshape(B, S, HGRN_HEADS, HGRN_DK)
    kh = -jnp.expm1(logf)
    vh = i_in.astype(jnp.float32).reshape(B, S, HGRN_HEADS, HGRN_DV)
    o = gla_chunkwise(qh, kh, vh, logf)
    o = o * lax.rsqrt(jnp.mean(jnp.square(o), axis=-1, keepdims=True) + RMS_EPS)
    o = o * gnorm.astype(jnp.float32).reshape(HGRN_HEADS, HGRN_DV)
    o = o.reshape(B, S, SEQ_MIX_W) * jax.nn.silu(g.astype(jnp.float32))
    return o


def stick_breaking_attention(q, k, v):
    B, S, _ = q.shape
    qh = q.astype(jnp.float32).reshape(B, S, SB_HEADS, SB_HEAD_DIM).transpose(0, 2, 1, 3)
    scale = 1.0 / math.sqrt(SB_HEAD_DIM)
    outs = []
    for blk in range(S // SB_BLOCK):
        start, end = blk * SB_BLOCK, (blk + 1) * SB_BLOCK
        qb = qh[:, :, start:end]
        kb, vb = k[:, :, :end], v[:, :, :end]
        z = jnp.einsum('bhtd,bhsd->bhts', qb, kb) * scale
        t_pos = start + jnp.arange(SB_BLOCK)
        s_pos = jnp.arange(end)
        mask = s_pos[None, :] < t_pos[:, None]
        neg_log_1m_beta = jnp.where(mask, jax.nn.softplus(z), 0.0)
        rest = lax.cumsum(neg_log_1m_beta, axis=3, reverse=True) - neg_log_1m_beta
        log_a = jax.nn.log_sigmoid(z) - rest
        a = jnp.where(mask, jnp.exp(log_a), 0.0)
        outs.append(jnp.einsum('bhts,bhsd->bhtd', a, vb))
    o = jnp.concatenate(outs, axis=2)
    return o.transpose(0, 2, 1, 3).reshape(B, S, SEQ_MIX_W)


def memory_attention(qm, mem_k, mem_v):
    B, S, _ = qm.shape
    qh = qm.astype(jnp.float32).reshape(B, S, MEM_HEADS, MEM_HEAD_DIM)
    scores = jnp.einsum('bthd,bmhd->bhtm', qh, mem_k.astype(jnp.float32)) / math.sqrt(MEM_HEAD_DIM)
    p = jax.nn.softmax(scores, axis=-1)
    o = jnp.einsum('bhtm,bmhd->bthd', p, mem_v.astype(jnp.float32))
    return o.reshape(B, S, MEM_W)


def hier_moe(x, w_group, b_group, w_router, b_router, w_gate, w_up, w_down):
    B, S, D = x.shape
    xf = x.reshape(B * S, D)
    T = xf.shape[0]
    probs_g = jax.nn.softmax((xf @ w_group + b_group).astype(jnp.float32), axis=-1)
    p_top, g_idx = lax.top_k(probs_g, 1)
    logits_e = (xf @ w_router + b_router).astype(jnp.float32).reshape(T, N_GROUPS, EXPERTS_PER_GROUP)
    in_group = jnp.take_along_axis(logits_e, g_idx[:, :, None], axis=1)[:, 0]
    probs_in = jax.nn.softmax(in_group, axis=-1)
    w_top, e_local = lax.top_k(probs_in, TOP_K_IN_GROUP)
    w_top = w_top / jnp.sum(w_top, axis=-1, keepdims=True)
    ids = g_idx * EXPERTS_PER_GROUP + e_local
    gates = jnp.einsum('tk,tke->te', p_top * w_top, jax.nn.one_hot(ids, N_EXPERTS, dtype=jnp.float32))
    h = jax.nn.silu(jnp.einsum('td,edf->tef', xf, w_gate)) * jnp.einsum('td,edf->tef', xf, w_up)
    y = jnp.einsum('tef,efd->td', h * gates[:, :, None].astype(h.dtype), w_down)
    return y.reshape(B, S, D)


def setup_inputs(seed: int = 0) -> dict:
    key = jax.random.key(seed)
    ks = iter(jax.random.split(key, 40))

    def nrm(shape, scale):
        return jax.random.normal(next(ks), shape, jnp.float32) * scale

    d_s = D_MODEL ** -0.5
    x = nrm((BATCH, SEQ, D_MODEL), 1.0)
    mem = nrm((BATCH, MEM_LEN, D_MODEL), 1.0)
    a_w_in = jnp.concatenate([
        nrm((N_A_LAYERS, D_MODEL, SEQ_MIX_W), d_s),
        nrm((N_A_LAYERS, D_MODEL, SEQ_MIX_W), d_s),
        nrm((N_A_LAYERS, D_MODEL, SEQ_MIX_W), d_s * DEEPNORM_BETA),
        nrm((N_A_LAYERS, D_MODEL, SEQ_MIX_W), d_s),
        nrm((N_A_LAYERS, D_MODEL, MEM_W), d_s),
    ], axis=-1)
    a_lower_bounds = nrm((N_A_LAYERS, SEQ_MIX_W), 0.1)
    a_gnorm = 1.0 + nrm((N_A_LAYERS, SEQ_MIX_W), 0.02)
    b_w_in = nrm((N_B_LAYERS, D_MODEL, B_IN_W), d_s)
    w_kv_shared = jnp.concatenate([nrm((D_MODEL, SEQ_MIX_W), d_s),
                                   nrm((D_MODEL, SEQ_MIX_W), d_s * DEEPNORM_BETA)], axis=-1)
    w_mem_kv = jnp.concatenate([nrm((DEPTH, D_MODEL, MEM_W), d_s),
                                nrm((DEPTH, D_MODEL, MEM_W), d_s * DEEPNORM_BETA)], axis=-1)
    w_o = nrm((DEPTH, MIX_W, D_MODEL), MIX_W ** -0.5 * DEEPNORM_BETA)
    ln_mix_g = 1.0 + nrm((DEPTH, D_MODEL), 0.02)
    ln_mix_b = nrm((DEPTH, D_MODEL), 0.02)
    ln_ffn_g = 1.0 + nrm((DEPTH, D_MODEL), 0.02)
    ln_ffn_b = nrm((DEPTH, D_MODEL), 0.02)
    w_group = nrm((DEPTH, D_MODEL, N_GROUPS), d_s)
    b_group = nrm((DEPTH, N_GROUPS), 0.01)
    w_router = nrm((DEPTH, D_MODEL, N_EXPERTS), d_s)
    b_router = nrm((DEPTH, N_EXPERTS), 0.01)
    w_gate = nrm((DEPTH, N_EXPERTS, D_MODEL, D_EXPERT), d_s)
    w_up = nrm((DEPTH, N_EXPERTS, D_MODEL, D_EXPERT), d_s)
    w_down = nrm((DEPTH, N_EXPERTS, D_EXPERT, D_MODEL), D_EXPERT ** -0.5 * DEEPNORM_BETA)
    return {"x": x, "mem": mem, "a_w_in": a_w_in, "a_lower_bounds": a_lower_bounds,
            "a_gnorm": a_gnorm, "b_w_in": b_w_in, "w_kv_shared": w_kv_shared,
            "w_mem_kv": w_mem_kv, "w_o": w_o, "ln_mix_g": ln_mix_g, "ln_mix_b": ln_mix_b,
            "ln_ffn_g": ln_ffn_g, "ln_ffn_b": ln_ffn_b, "w_group": w_group, "b_group": b_group,
            "w_router": w_router, "b_router": b_router, "w_gate": w_gate, "w_up": w_up,
            "w_down": w_down}


def reference(x, mem, a_w_in, a_lower_bounds, a_gnorm, b_w_in, w_kv_shared, w_mem_kv, w_o,
              ln_mix_g, ln_mix_b, ln_ffn_g, ln_ffn_b, w_group, b_group, w_router, b_router,
              w_gate, w_up, w_down):
    B, S, _ = x.shape
    lb_all = jnp.cumsum(jax.nn.softmax(a_lower_bounds.astype(jnp.float32), axis=0), axis=0)
    lb_all = lb_all - lb_all[0]
    shared_k = None
    shared_v = None
    for layer in range(DEPTH):
        mem_kv = mem @ w_mem_kv[layer]
        mem_k = mem_kv[..., :MEM_W].reshape(B, MEM_LEN, MEM_HEADS, MEM_HEAD_DIM)
        mem_v = mem_kv[..., MEM_W:].reshape(B, MEM_LEN, MEM_HEADS, MEM_HEAD_DIM)
        if layer < N_A_LAYERS:
            h = x @ a_w_in[layer]
            q, f_raw, i_in, g, qm = jnp.split(
                h, [SEQ_MIX_W, 2 * SEQ_MIX_W, 3 * SEQ_MIX_W, 4 * SEQ_MIX_W], axis=-1)
            seq_out = hgrn2_mix(q, f_raw, i_in, g, lb_all[layer], a_gnorm[layer])
        else:
            if layer == N_A_LAYERS:
                kv = x @ w_kv_shared
                shared_k = kv[..., :SEQ_MIX_W].astype(jnp.float32).reshape(
                    B, S, SB_HEADS, SB_HEAD_DIM).transpose(0, 2, 1, 3)
                shared_v = kv[..., SEQ_MIX_W:].astype(jnp.float32).reshape(
                    B, S, SB_HEADS, SB_HEAD_DIM).transpose(0, 2, 1, 3)
            h = x @ b_w_in[layer - N_A_LAYERS]
            q, qm = jnp.split(h, [SEQ_MIX_W], axis=-1)
            seq_out = stick_breaking_attention(q, shared_k, shared_v)
        mem_out = memory_attention(qm, mem_k, mem_v)
        mixed = jnp.concatenate([seq_out.astype(x.dtype), mem_out.astype(x.dtype)], axis=-1)
        x = post_norm(x, mixed @ w_o[layer], ln_mix_g[layer], ln_mix_b[layer])
        moe_out = hier_moe(x, w_group[layer], b_group[layer], w_router[layer], b_router[layer],
                           w_gate[layer], w_up[layer], w_down[layer])
        x = post_norm(x, moe_out, ln_ffn_g[layer], ln_ffn_b[layer])
    return x
```

```python
import numpy as np
import concourse.bass as bass
import concourse.mybir as mybir

F32 = mybir.dt.float32
BF16 = mybir.dt.bfloat16
U8 = mybir.dt.uint8
I32 = mybir.dt.int32
ALU = mybir.AluOpType
AF = mybir.ActivationFunctionType
AX = mybir.AxisListType

_ESZ = {F32: 4, BF16: 2, U8: 1, I32: 4}

SB_BYTES = 207 * 1024
N_DSEM = 32
BUCKET = 2048


def esz(dt):
    return _ESZ[dt]


class Sched:
    ENG = ("pe", "act", "dve", "pool", "sp")

    def __init__(self, nc, big, pbig):
        self.nc = nc
        self.big = big
        self.pbig = pbig
        self.ops = {e: [] for e in self.ENG}
        self.keys = list(self.ENG[:4]) + ["d%d" % i for i in range(N_DSEM)]
        self.kidx = {k: i for i, k in enumerate(self.keys)}
        self.nk = len(self.keys)
        self.know = {e: [0] * self.nk for e in self.ENG}
        self.clock = {}
        self.seq = {k: 0 for k in self.keys}
        self.hist = {"sb": {}, "ps": {}, "dr": {}}
        self.ndma = 0
        self.ndma_sw = 0
        self.sb_top = 0
        self.marked = set()
        self.out_deps = []

    def alloc(self, shape, dt, name=None):
        n = int(np.prod(shape[1:])) * esz(dt)
        n = (n + 63) // 64 * 64
        off = self.sb_top
        self.sb_top += n
        assert self.sb_top <= SB_BYTES, ("SBUF overflow", name, self.sb_top)
        return self.view(off, shape, dt)

    def view(self, off, shape, dt):
        nb = int(np.prod(shape[1:])) * esz(dt)
        v = self.big[0:shape[0], off:off + nb]
        if dt != U8:
            v = v.bitcast(dt)
        if len(shape) == 3:
            v = v.rearrange("p (a b) -> p a b", a=shape[1])
        elif len(shape) == 4:
            v = v.rearrange("p (a b c) -> p a b c", a=shape[1], b=shape[2])
        return v

    def mark(self):
        return self.sb_top

    def release(self, m):
        self.sb_top = m

    def psum(self, bank, shape, dt=F32, off=0):
        nb = int(np.prod(shape[1:])) * esz(dt)
        assert off + nb <= 2048 * 8
        e0 = (bank * 2048 + off) // 4
        v = self.pbig[0:shape[0], e0:e0 + (nb + 3) // 4]
        if dt != F32:
            v = v.bitcast(dt)
        if len(shape) == 3:
            v = v.rearrange("p (a b) -> p a b", a=shape[1])
        return v

    def rect(self, ap):
        t = ap.tensor
        nm = t.name
        e = esz(ap.dtype)
        a = ap.ap
        off = int(ap.offset)
        if nm == "big":
            sp = "sb"
        elif nm == "pbig":
            sp = "ps"
        else:
            return ("dr", nm, 0, 1, 0, 1)
        pstep, pn = a[0]
        if pstep == 0:
            pstep = 1 << 40
        p0 = off // pstep if pstep < (1 << 40) else 0
        fo = off - p0 * pstep if pstep < (1 << 40) else off
        span = 1
        for st, cn in a[1:]:
            span += abs(st) * (cn - 1)
        b0 = fo * e
        b1 = (fo + span) * e
        p1 = p0 + pn
        if sp == "ps":
            b0 = (b0 // 2048) * 2048
            b1 = ((b1 + 2047) // 2048) * 2048
            p0 = (p0 // 32) * 32
            p1 = ((p1 + 31) // 32) * 32
        return (sp, None, p0, p1, b0, b1)

    def _deps_for(self, accesses, own=None):
        deps = {}
        regs = []
        for r, kind in accesses:
            sp, nm, p0, p1, b0, b1 = r
            h = self.hist[sp]
            if sp == "dr":
                bks = [nm]
            else:
                bks = range(b0 // BUCKET, (b1 - 1) // BUCKET + 1)
            for bk in bks:
                d = h.get(bk)
                if d is None:
                    d = {}
                    h[bk] = d
                dead = []
                for (k2, kind2, q0, q1, c0, c1), s2 in d.items():
                    if q0 < p1 and p0 < q1 and c0 < b1 and b0 < c1:
                        if kind == "w" or kind2 == "w":
                            skip = False
                            if k2 == own:
                                if own == "pe" or not (kind == "r" and kind2 == "w"):
                                    skip = True
                            if not skip and deps.get(k2, 0) < s2:
                                deps[k2] = s2
                        if kind == "w" and p0 <= q0 and q1 <= p1 and b0 <= c0 and c1 <= b1:
                            dead.append((k2, kind2, q0, q1, c0, c1))
                for kk in dead:
                    del d[kk]
                regs.append((d, kind, p0, p1, b0, b1))
        return deps, regs

    def _register(self, regs, key, seq):
        for d, kind, p0, p1, b0, b1 in regs:
            d[(key, kind, p0, p1, b0, b1)] = seq

    def op(self, eng, fn, reads=(), writes=()):
        acc = [(self.rect(a), "r") for a in reads] + [(self.rect(a), "w") for a in writes]
        key = eng
        deps, regs = self._deps_for(acc, own=key)
        self.seq[key] += 1
        seq = self.seq[key]
        waits = self._resolve(eng, deps, own=key)
        clk = list(self.know[eng])
        clk[self.kidx[key]] = seq
        self.clock[(key, seq)] = clk
        self._register(regs, key, seq)
        self.ops[eng].append({"fn": fn, "waits": waits, "inc": (key, seq)})

    def _resolve(self, eng, deps, own=None):
        know = list(self.know[eng])
        waits = []
        items = sorted(deps.items(), key=lambda kv: -kv[1])
        for k, s in items:
            if k == own:
                pass
            i = self.kidx[k]
            if know[i] >= s:
                continue
            waits.append((k, s))
            self.marked.add((k, s))
            c = self.clock[(k, s)]
            know = [a if a >= b else b for a, b in zip(know, c)]
        self.know[eng] = know
        return waits

    def dma(self, eng, out, in_, reads=None, writes=None, **kw):
        rd = [in_] if reads is None else reads
        wr = [out] if writes is None else writes
        acc = [(self.rect(a), "r") for a in rd] + [(self.rect(a), "w") for a in wr]
        deps, regs = self._deps_for(acc)
        half = N_DSEM // 2
        if eng == "pool":
            k = "d%d" % (half + self.ndma_sw % half)
            self.ndma_sw += 1
        else:
            k = "d%d" % (self.ndma % half)
            self.ndma += 1
        prev = self.seq[k]
        if prev > 0:
            deps[k] = max(deps.get(k, 0), prev)
        self.seq[k] += 1
        seq = self.seq[k]
        waits = self._resolve(eng, deps)
        clk = list(self.know[eng])
        clk[self.kidx[k]] = seq
        self.clock[(k, seq)] = clk
        self._register(regs, k, seq)
        self.ops[eng].append({"dma": (out, in_), "kw": kw, "waits": waits, "inc": (k, seq)})
        return (k, seq)

    def final_wait(self, eng, deps):
        waits = self._resolve(eng, dict(deps))
        self.ops[eng].append({"waits": waits})

    def emit(self, sems):
        rank = {}
        for k in self.ENG[:4]:
            ms = sorted(s for (kk, s) in self.marked if kk == k)
            rank[k] = {s: i + 1 for i, s in enumerate(ms)}

        def val(k, s):
            if k[0] == "d" and k[1:].isdigit():
                return 16 * s
            return rank[k][s]

        nc = self.nc
        engobj = {"pe": "tensor", "act": "scalar", "dve": "vector", "pool": "gpsimd", "sp": "sync"}
        with nc.Block() as block:
            for e in self.ENG:
                ops = self.ops[e]

                def body(engine, ops=ops, e=e):
                    for o in ops:
                        for (k, s) in o["waits"]:
                            engine.wait_ge(sems[k], val(k, s))
                        if "fn" in o:
                            ins = o["fn"](engine)
                            k, s = o["inc"]
                            if (k, s) in self.marked:
                                ins.then_inc(sems[k], 1)
                        elif "dma" in o:
                            out, in_ = o["dma"]
                            k, s = o["inc"]
                            engine.dma_start(out=out, in_=in_, **o["kw"]).then_inc(sems[k], 16)

                getattr(block, engobj[e])(body)
from contextlib import ExitStack
from concourse.bass_utils import run_bass_kernel_spmd

T = 2048
D = 1024
ALPHA = 8 ** 0.25
WSPEC = [("a_w_in", [2, 1024, 3328]), ("a_lower_bounds", [2, 768]), ("a_gnorm", [2, 768]),
         ("b_w_in", [2, 1024, 1024]), ("w_kv_shared", [1024, 1536]), ("w_mem_kv", [4, 1024, 512]),
         ("w_o", [4, 1024, 1024]), ("ln_mix_g", [4, 1024]), ("ln_mix_b", [4, 1024]),
         ("ln_ffn_g", [4, 1024]), ("ln_ffn_b", [4, 1024]), ("w_group", [4, 1024, 4]),
         ("b_group", [4, 4]), ("w_router", [4, 1024, 16]), ("b_router", [4, 16]),
         ("w_gate", [4, 16, 1024, 256]), ("w_up", [4, 16, 1024, 256]), ("w_down", [4, 16, 256, 1024])]


def make_consts():
    c = {}
    c["c_ident"] = np.eye(128, dtype=np.float32)
    sel = np.zeros((16, 16, 128), np.float32)
    for e in range(16):
        sel[e, e, :] = 1.0
    c["c_sel"] = sel
    s = np.arange(128)
    c["c_hmask"] = ((s[:, None] // 64 == s[None, :] // 64) & (s[None, :] >= s[:, None])).astype(np.float32)
    M = np.zeros((128, 132), np.float32)
    for t in range(128):
        ch = t // 64
        ref = ch * 64 + 31
        for sp in range(ch * 64, ch * 64 + 64):
            M[sp, t] = float(sp <= t) - float(sp <= ref)
    for ch in range(2):
        for sp in range(ch * 64, ch * 64 + 64):
            M[sp, 128 + 2 * ch] = float(sp <= ch * 64 + 31)
            M[sp, 129 + 2 * ch] = 1.0
    c["c_M"] = M
    c["c_U"] = (s[:, None] > s[None, :]).astype(np.float32)
    c["c_L"] = (s[:, None] <= s[None, :]).astype(np.float32)
    c["c_dmask"] = (s[:, None] < s[None, :]).astype(np.float32)
    oz = np.zeros((128, 2, 128), np.float32)
    oz[:, 0, 0:64] = 1.0
    oz[:, 1, 64:128] = 1.0
    c["c_onesz"] = oz
    return c


class K:
    pass


def build(mode="full"):
    nc = bass.Bass("TRN2", target_bir_lowering=False)
    k = K()
    dr = {}
    dr["x"] = nc.dram_tensor("x", [T, D], F32, kind="ExternalInput").ap()
    dr["mem"] = nc.dram_tensor("mem", [256, D], F32, kind="ExternalInput").ap()
    for nm, shp in WSPEC:
        dr[nm] = nc.dram_tensor(nm, shp, F32, kind="ExternalInput").ap()
    cs = make_consts()
    for nm, arr in cs.items():
        dr[nm] = nc.dram_tensor(nm, list(arr.shape), F32, kind="ExternalInput").ap()
    y_d = nc.dram_tensor("y", [T, D], F32, kind="ExternalOutput").ap()
    with ExitStack() as es:
        big = es.enter_context(nc.sbuf_tensor("big", [128, SB_BYTES], U8))
        pbig = es.enter_context(nc.psum_tensor("pbig", [128, 4096], F32))
        S = Sched(nc, big, pbig)
        sems = {kk: es.enter_context(nc.semaphore("q%d" % i)) for i, kk in enumerate(S.keys)}
        P = S.psum

        def aps(*xs):
            return [a for a in xs if a is not None and not isinstance(a, (int, float))]

        def MM(out, lhsT, rhs, start=True, stop=True):
            S.op("pe", lambda e: e.matmul(out, lhsT, rhs, start=start, stop=stop), reads=[lhsT, rhs], writes=[out])

        def TR(out, in_, ident):
            S.op("pe", lambda e: e.transpose(out, in_, ident), reads=[in_, ident], writes=[out])

        def ACT(out, in_, func, scale=None, bias=None, accum=None):
            kw = {}
            if scale is not None:
                kw["scale"] = scale
            if bias is not None:
                kw["bias"] = bias
            if accum is not None:
                kw["accum_out"] = accum
            S.op("act", lambda e: e.activation(out, in_, func, **kw), reads=aps(in_, scale, bias), writes=aps(out, accum))

        def TT(out, a, b, op, eng="dve"):
            S.op(eng, lambda e: e.tensor_tensor(out, a, b, op), reads=[a, b], writes=[out])

        def TS(out, a, s1, s2, op0, op1=None, eng="dve"):
            if op1 is None:
                S.op(eng, lambda e: e.tensor_scalar(out, a, s1, None, op0), reads=aps(a, s1), writes=[out])
            else:
                S.op(eng, lambda e: e.tensor_scalar(out, a, s1, s2, op0, op1), reads=aps(a, s1, s2), writes=[out])

        def STT(out, in0, sc, in1, op0, op1, eng="dve"):
            S.op(eng, lambda e: e.scalar_tensor_tensor(out, in0, sc, in1, op0, op1), reads=aps(in0, sc, in1), writes=[out])

        def CP(out, in_, eng="dve"):
            if eng == "act":
                S.op("act", lambda e: e.copy(out, in_), reads=[in_], writes=[out])
            else:
                S.op(eng, lambda e: e.tensor_copy(out, in_), reads=[in_], writes=[out])

        def RED(out, in_, op):
            S.op("dve", lambda e: e.tensor_reduce(out, in_, AX.X, op), reads=[in_], writes=[out])

        def RECIP(out, in_):
            S.op("dve", lambda e: e.reciprocal(out, in_), reads=[in_], writes=[out])

        def MEMSET(out, v, eng="dve"):
            S.op(eng, lambda e: e.memset(out, v), writes=[out])

        def WLOAD(dst, src):
            S.dma("pool", dst, src.rearrange("(k p) n -> p k n", p=128))

        xT = S.alloc([128, 8, T], BF16)
        identf = S.alloc([128, 128], F32)
        identb = S.alloc([128, 128], BF16)
        onesD = S.alloc([128, 128], BF16)
        selb = S.alloc([16, 16, 128], BF16)
        lnp = S.alloc([128, 4, 4, 8], F32)
        S.dma("sp", identf, dr["c_ident"])
        S.dma("pool", identb, dr["c_ident"])
        S.dma("pool", selb, dr["c_sel"])
        MEMSET(onesD, 1.0 / 1024.0)
        for wi, nm in enumerate(["ln_mix_g", "ln_mix_b", "ln_ffn_g", "ln_ffn_b"]):
            for l in range(4):
                S.dma("sp", lnp[:, wi, l, :], dr[nm][l].rearrange("(k p) -> p k", p=128), allow_slow_non_contiguous=True)

        def load_xT():
            m = S.mark()
            xt = [S.alloc([128, 1024], BF16) for _ in range(2)]
            for i in range(16):
                b = xt[i % 2]
                S.dma("pool", b, dr["x"][i * 128:(i + 1) * 128, :])
                pt = P(i % 2, [128, 8, 128], BF16)
                for kk in range(8):
                    TR(pt[:, kk, :], b[:, kk * 128:(kk + 1) * 128], identb)
                CP(xT[:, :, i * 128:(i + 1) * 128], pt, eng="act" if i % 2 else "dve")
            S.release(m)

        def ln_stage(ybuf, wi, l, final=False):
            m = S.mark()
            ysq = S.alloc([128, 8, 512], BF16)
            yb = S.alloc([128, 8, 512], BF16)
            mean_sb = S.alloc([128, 512], F32)
            m2 = S.alloc([128, 512], F32)
            rstd = S.alloc([128, 512], F32)
            otile = [S.alloc([128, 1024], F32) for _ in range(2)] if final else None
            for c in range(4):
                tc = slice(c * 512, (c + 1) * 512)
                for kk in range(8):
                    ACT(ysq[:, kk, :], ybuf[:, kk, tc], AF.Square)
                    CP(yb[:, kk, :], ybuf[:, kk, tc], eng="dve")
                mps = P(0, [128, 512])
                sps = P(1, [128, 512])
                for kk in range(8):
                    MM(mps, onesD, yb[:, kk, :], kk == 0, kk == 7)
                for kk in range(8):
                    MM(sps, onesD, ysq[:, kk, :], kk == 0, kk == 7)
                CP(mean_sb, mps, eng="act")
                ACT(m2, mps, AF.Square)
                TT(m2, sps, m2, ALU.subtract)
                ACT(m2, m2, AF.Ln, bias=1e-5)
                ACT(rstd, m2, AF.Exp, scale=-0.5)
                for kk in range(8):
                    yk = ybuf[:, kk, tc]
                    TT(yk, yk, mean_sb, ALU.subtract)
                    TT(yk, yk, rstd, ALU.mult)
                    if final:
                        ACT(yk, yk, AF.Identity, scale=lnp[:, wi, l, kk:kk + 1], bias=lnp[:, wi + 1, l, kk:kk + 1])
                    else:
                        ACT(xT[:, kk, tc], yk, AF.Identity, scale=lnp[:, wi, l, kk:kk + 1], bias=lnp[:, wi + 1, l, kk:kk + 1])
                if final:
                    for j in range(4):
                        tt = c * 4 + j
                        ot = otile[tt % 2]
                        for half in range(2):
                            pp = P(2 + half, [128, 4, 128])
                            for q in range(4):
                                kk = half * 4 + q
                                TR(pp[:, q, :], ybuf[:, kk, tt * 128:(tt + 1) * 128], identf)
                            CP(ot[:, half * 512:(half + 1) * 512], pp.rearrange("p a b -> p (a b)"), eng="act" if half else "dve")
                        k.outd.append(S.dma("sp", y_d[tt * 128:(tt + 1) * 128, :], ot))
            S.release(m)

        def moe_stage(l, ybuf):
            m = S.mark()
            wr = S.alloc([128, 8, 20], BF16)
            S.dma("pool", wr[:, :, 0:4], dr["w_group"][l].rearrange("(k p) n -> p k n", p=128))
            S.dma("pool", wr[:, :, 4:20], dr["w_router"][l].rearrange("(k p) n -> p k n", p=128))
            bias = S.alloc([128, 20], F32)
            S.dma("sp", bias[:, 0:4], dr["b_group"][l:l + 1, :].broadcast_to([128, 4]))
            S.dma("sp", bias[:, 4:20], dr["b_router"][l:l + 1, :].broadcast_to([128, 16]))
            PR = P(7, [128, 16, 20])
            for i in range(16):
                for kk in range(8):
                    MM(PR[:, i, :], xT[:, kk, i * 128:(i + 1) * 128], wr[:, kk, :], kk == 0, kk == 7)
            L = S.alloc([128, 16, 20], F32)
            TT(L, PR, bias.unsqueeze(1).broadcast_to([128, 16, 20]), ALU.add)
            Lg = L[:, :, 0:4]
            Le = L[:, :, 4:20]
            gmax = S.alloc([128, 16], F32)
            RED(gmax, Lg, ALU.max)
            gb3 = gmax.unsqueeze(2).broadcast_to([128, 16, 4])
            dg = S.alloc([128, 16, 4], F32)
            TT(dg, Lg, gb3, ALU.subtract)
            ACT(dg, dg, AF.Exp)
            sg = S.alloc([128, 16], F32)
            RED(sg, dg, ALU.add)
            ptop = S.alloc([128, 16], F32)
            RECIP(ptop, sg)
            ohg = S.alloc([128, 16, 4], F32)
            TT(ohg, Lg, gb3, ALU.is_equal)
            TS(ohg, ohg, -1.0, 1.0e4, ALU.add, ALU.mult)
            Lm = S.alloc([128, 16, 16], F32)
            Lm4 = Lm.rearrange("p a (g e) -> p a g e", g=4)
            TT(Lm4, Le.rearrange("p a (g e) -> p a g e", g=4), ohg.unsqueeze(3).broadcast_to([128, 16, 4, 4]), ALU.add)
            m1 = S.alloc([128, 16], F32)
            RED(m1, Lm, ALU.max)
            oh1 = S.alloc([128, 16, 16], F32)
            TT(oh1, Lm, m1.unsqueeze(2).broadcast_to([128, 16, 16]), ALU.is_equal)
            Lm2 = S.alloc([128, 16, 16], F32)
            STT(Lm2, oh1, -1.0e4, Lm, ALU.mult, ALU.add)
            mm2 = S.alloc([128, 16], F32)
            RED(mm2, Lm2, ALU.max)
            oh2 = S.alloc([128, 16, 16], F32)
            TT(oh2, Lm2, mm2.unsqueeze(2).broadcast_to([128, 16, 16]), ALU.is_equal)
            ee = S.alloc([128, 16], F32)
            TT(ee, mm2, m1, ALU.subtract)
            ACT(ee, ee, AF.Exp)
            den = S.alloc([128, 16], F32)
            TS(den, ee, 1.0, None, ALU.add)
            RECIP(den, den)
            g1 = S.alloc([128, 16], F32)
            TT(g1, den, ptop, ALU.mult)
            g2 = S.alloc([128, 16], F32)
            TT(g2, g1, ee, ALU.mult)
            TT(oh1, oh1, g1.unsqueeze(2).broadcast_to([128, 16, 16]), ALU.mult)
            TT(oh2, oh2, g2.unsqueeze(2).broadcast_to([128, 16, 16]), ALU.mult)
            gb = S.alloc([128, 16, 16], BF16)
            TT(gb, oh1, oh2, ALU.add)
            if mode == "gates":
                k.dbg = (oh1, oh2)
            PT = P(5, [16, T], BF16)
            for i in range(16):
                TR(PT[:, i * 128:(i + 1) * 128], gb[:, i, :], identb)
            gatesT = S.alloc([16, T], BF16)
            CP(gatesT, PT)
            wg = [S.alloc([128, 8, 256], BF16) for _ in range(2)]
            wu = [S.alloc([128, 8, 256], BF16) for _ in range(2)]
            wd = [S.alloc([128, 2, 1024], BF16) for _ in range(2)]
            hT = S.alloc([128, 2, T], BF16)
            sgt = [S.alloc([128, 512], F32) for _ in range(2)]
            nblk = [0]
            ndn = [0]

            def gateup_mm(e, c, ft):
                b = e % 2
                tc = slice(c * 512, (c + 1) * 512)
                bb = nblk[0] % 2
                nblk[0] += 1
                Pg = P(bb * 2, [128, 512])
                Pu = P(bb * 2 + 1, [128, 512])
                for kk in range(8):
                    MM(Pg, wg[b][:, kk, ft * 128:(ft + 1) * 128], xT[:, kk, tc], kk == 0, kk == 7)
                for kk in range(8):
                    MM(Pu, wu[b][:, kk, ft * 128:(ft + 1) * 128], xT[:, kk, tc], kk == 0, kk == 7)
                return (bb, Pg, Pu, ft, tc)

            def gateup_ev(st):
                bb, Pg, Pu, ft, tc = st
                ACT(sgt[bb], Pg, AF.Silu)
                TT(sgt[bb], sgt[bb], Pu, ALU.mult)
                TT(hT[:, ft, tc], sgt[bb], P(4, [128, 512]), ALU.mult)

            def down(e, c, kks):
                b = e % 2
                tc = slice(c * 512, (c + 1) * 512)
                for kk in kks:
                    Pd = P(5 + ndn[0] % 3, [128, 512])
                    ndn[0] += 1
                    MM(Pd, wd[b][:, 0, kk * 128:(kk + 1) * 128], hT[:, 0, tc], True, False)
                    MM(Pd, wd[b][:, 1, kk * 128:(kk + 1) * 128], hT[:, 1, tc], False, True)
                    TT(ybuf[:, kk, tc], ybuf[:, kk, tc], Pd, ALU.add)

            prev = None
            for e in range(16):
                b = e % 2
                WLOAD(wg[b], dr["w_gate"][l, e])
                WLOAD(wu[b], dr["w_up"][l, e])
                WLOAD(wd[b], dr["w_down"][l, e])
                for c in range(4):
                    tc = slice(c * 512, (c + 1) * 512)
                    Gp = P(4, [128, 512])
                    MM(Gp, selb[0:16, e, :], gatesT[0:16, tc])
                    for ft in range(2):
                        st = gateup_mm(e, c, ft)
                        if prev is not None:
                            down(prev[0], prev[1], range(4 * ft, 4 * ft + 4))
                        gateup_ev(st)
                    prev = (e, c)
            down(prev[0], prev[1], range(0, 8))
            S.release(m)

        def init_ybuf(ybuf):
            for kk in range(8):
                TS(ybuf[:, kk, :], xT[:, kk, :], ALPHA, None, ALU.mult)


        ones1 = S.alloc([128, 128], BF16)
        MEMSET(ones1, 1.0)
        ones128 = S.alloc([128, 128], BF16)
        MEMSET(ones128, 1.0 / 128.0)
        Ub = S.alloc([128, 128], BF16)
        Lcb = S.alloc([128, 128], BF16)
        S.dma("pool", Ub, dr["c_U"])
        S.dma("pool", Lcb, dr["c_L"])
        memT = S.alloc([128, 8, 256], BF16)

        def MMs(out, lhsT, rhs, start, stop):
            S.op("pe", lambda e: e.matmul(out, lhsT, rhs, start=start, stop=stop, skip_group_check=True), reads=[lhsT, rhs], writes=[out])

        def load_memT():
            m = S.mark()
            mt_ = S.alloc([128, 2, 1024], BF16)
            S.dma("pool", mt_, dr["mem"].rearrange("(a p) d -> p a d", p=128))
            for a in range(2):
                pt = P(2 + a, [128, 8, 128], BF16)
                for kk in range(8):
                    TR(pt[:, kk, :], mt_[:, a, kk * 128:(kk + 1) * 128], identb)
                CP(memT[:, :, a * 128:(a + 1) * 128], pt)
            S.release(m)

        def proj_fm(wsrc, tiles, dst_fn, func=None):
            m = S.mark()
            wb = [S.alloc([128, 8, 128], BF16) for _ in range(3)]
            for n, c0 in enumerate(tiles):
                w = wb[n % 3]
                WLOAD(w, wsrc[:, c0:c0 + 128])
                for c in range(4):
                    tc = slice(c * 512, (c + 1) * 512)
                    pp = P((n * 4 + c) % 2, [128, 512])
                    for kk in range(8):
                        MM(pp, w[:, kk, :], xT[:, kk, tc], kk == 0, kk == 7)
                    if func is not None and func(n) is not None:
                        ACT(dst_fn(n, c), pp, func(n))
                    else:
                        CP(dst_fn(n, c), pp, eng="act" if (n * 4 + c) % 2 else "dve")
            S.release(m)

        def mem_attn(l, qmT, mixedT):
            m = S.mark()
            wm = S.alloc([128, 8, 512], BF16)
            WLOAD(wm, dr["w_mem_kv"][l])
            kmT = S.alloc([128, 2, 256], BF16)
            vm = S.alloc([128, 2, 256], BF16)
            for j in range(2):
                pp = P(2 + j, [128, 256])
                for kk in range(8):
                    MM(pp, wm[:, kk, j * 128:(j + 1) * 128], memT[:, kk, :], kk == 0, kk == 7)
                CP(kmT[:, j, :], pp)
            for mt in range(2):
                pp = P(4 + mt, [128, 256])
                for kk in range(8):
                    MM(pp, memT[:, kk, mt * 128:(mt + 1) * 128], wm[:, kk, 256:512], kk == 0, kk == 7)
                CP(vm[:, mt, :], pp, eng="act")
            em = [S.alloc([128, 512], BF16) for _ in range(4)]
            rec = S.alloc([128, 512], F32)
            n = 0
            for j in range(2):
                for half in range(2):
                    pb = 64 * half
                    for c in range(4):
                        tc = slice(c * 512, (c + 1) * 512)
                        es_ = []
                        for mt in range(2):
                            zp = P(2 + (n % 2), [128, 512])
                            MM(zp, kmT[pb:pb + 64, j, mt * 128:(mt + 1) * 128], qmT[pb:pb + 64, j, tc])
                            eb = em[n % 4]
                            ACT(eb, zp, AF.Exp, scale=0.125)
                            es_.append(eb)
                            n += 1
                        pn = P(4, [128, 512])
                        pd = P(5, [128, 512])
                        for mt in range(2):
                            MM(pn, vm[:, mt, j * 128:(j + 1) * 128], es_[mt], mt == 0, mt == 1)
                        for mt in range(2):
                            MM(pd, ones1, es_[mt], mt == 0, mt == 1)
                        RECIP(rec[pb:pb + 64, :], pd[pb:pb + 64, :])
                        TT(mixedT[pb:pb + 64, 6 + j, tc], pn[pb:pb + 64, :], rec[pb:pb + 64, :], ALU.mult)
            S.release(m)

        def wo_stage(l, mixedT, ybuf):
            m = S.mark()
            wob = [S.alloc([128, 8, 256], BF16) for _ in range(2)]
            n = 0
            for q in range(4):
                w = wob[q % 2]
                WLOAD(w, dr["w_o"][l][:, q * 256:(q + 1) * 256])
                for kq in range(2):
                    kk = 2 * q + kq
                    for c in range(4):
                        tc = slice(c * 512, (c + 1) * 512)
                        pp = P(n % 2, [128, 512])
                        n += 1
                        for f in range(8):
                            MM(pp, w[:, f, kq * 128:(kq + 1) * 128], mixedT[:, f, tc], f == 0, f == 7)
                        STT(ybuf[:, kk, tc], xT[:, kk, tc], ALPHA, pp, ALU.mult, ALU.add)
            S.release(m)

        def kv_stage(kT, vtok):
            proj_fm(dr["w_kv_shared"], [j * 128 for j in range(6)], lambda n, c: kT[:, n, c * 512:(c + 1) * 512])
            m = S.mark()
            wv = S.alloc([128, 8, 768], BF16)
            WLOAD(wv, dr["w_kv_shared"][:, 768:1536])
            for i in range(16):
                ts_ = slice(i * 128, (i + 1) * 128)
                pa = P(2 + 2 * (i % 2), [128, 512])
                pb_ = P(3 + 2 * (i % 2), [128, 256])
                for kk in range(8):
                    MM(pa, xT[:, kk, ts_], wv[:, kk, 0:512], kk == 0, kk == 7)
                for kk in range(8):
                    MM(pb_, xT[:, kk, ts_], wv[:, kk, 512:768], kk == 0, kk == 7)
                CP(vtok[:, i, 0:512], pa, eng="act")
                CP(vtok[:, i, 512:768], pb_, eng="dve")
            S.release(m)

        def sb_attn(qT, kT, vtok, mixedT):
            m = S.mark()
            dm = S.alloc([128, 4, 512], BF16)
            dmf = S.alloc([128, 128], F32)
            S.dma("sp", dmf, dr["c_dmask"])
            for q in range(4):
                if q > 0:
                    MEMSET(dm[:, q, 0:q * 128], 0.0)
                CP(dm[:, q, q * 128:(q + 1) * 128], dmf)
                if q < 3:
                    MEMSET(dm[:, q, (q + 1) * 128:512], 1.0)
            yo = [32768]

            def ya(shape, dt):
                nb = int(np.prod(shape[1:])) * esz(dt)
                v = yview(yo[0], shape, dt)
                yo[0] += nb
                assert yo[0] <= 65536
                return v
            REF, RLF, RLB, RAB = 5, 3, 4, 3
            ef = [ya([128, 512], F32) for _ in range(REF)]
            Lf = [ya([128, 512], F32) for _ in range(RLF)]
            Lb = [ya([128, 512], BF16) for _ in range(RLB)]
            ab = [ya([128, 512], BF16) for _ in range(RAB)]
            zbanks = [0, 1, 6, 7]
            items = []
            for j in range(6):
                for c in range(4):
                    top = 4 * c + 3
                    for I in range(top, -1, -1):
                        for half in range(2):
                            items.append((j, half, c, I, half, I == top, I == 0))
            N = len(items)

            def Zb(n):
                return P(zbanks[n % 4], [128, 512])

            def accb(n):
                return P(2 + items[n][4], [128, 512])

            def Ob(n):
                return P(4 + items[n][4], [128, 512])

            def s0(n):
                j, half, c, I, _, _, _ = items[n]
                pb = 64 * half
                MM(Zb(n), kT[pb:pb + 64, j, I * 128:(I + 1) * 128], qT[pb:pb + 64, j, c * 512:(c + 1) * 512])

            def s1(n):
                ACT(ef[n % REF], Zb(n), AF.Exp, scale=0.125)
                ACT(Lf[n % RLF], ef[n % REF], AF.Ln, bias=1.0)

            def s2(n):
                j, half, c, I, _, _, _ = items[n]
                if I >= 4 * c:
                    TT(Lb[n % RLB], Lf[n % RLF], dm[:, I - 4 * c, :], ALU.mult)
                else:
                    CP(Lb[n % RLB], Lf[n % RLF], eng="pool")
                STT(ef[n % REF], Zb(n), 0.125, Lf[n % RLF], ALU.mult, ALU.subtract)

            def s3a(n):
                MMs(accb(n), Ub, Lb[n % RLB], items[n][5], True)

            def s3b(n):
                TT(ef[n % REF], ef[n % REF], accb(n), ALU.subtract)

            def s3c(n):
                MMs(accb(n), Lcb, Lb[n % RLB], False, True)

            def s4(n):
                j, half, c, I, _, _, _ = items[n]
                ACT(ab[n % RAB], ef[n % REF], AF.Exp)
                if I >= 4 * c:
                    TT(ab[n % RAB], ab[n % RAB], dm[:, I - 4 * c, :], ALU.mult)

            def s5(n):
                j, half, c, I, _, first, last = items[n]
                MMs(Ob(n), vtok[:, I, j * 128:(j + 1) * 128], ab[n % RAB], first, last)
                if last:
                    pb = 64 * half
                    CP(mixedT[pb:pb + 64, j, c * 512:(c + 1) * 512], Ob(n)[pb:pb + 64, :], eng="act")

            def ok(n):
                return 0 <= n < N
            for t in range(N + 7):
                if ok(t - 5):
                    s4(t - 5)
                if ok(t):
                    s0(t)
                if ok(t - 1):
                    s1(t - 1)
                if ok(t - 2):
                    s2(t - 2)
                if ok(t - 5):
                    s3c(t - 5)
                if ok(t - 3):
                    s3a(t - 3)
                if ok(t - 4):
                    s3b(t - 4)
                if ok(t - 6):
                    s5(t - 6)
            S.release(m)

        def yview(off, shape, dt):
            return S.view(k.ybuf_off + off, shape, dt)

        def b_mix(l, kT, vtok, ybuf):
            m = S.mark()
            qT = yview(0, [128, 6, T], BF16)
            qmT = yview(6 * T * 2, [128, 2, T], BF16)
            proj_fm(dr["b_w_in"][l - 2], [j * 128 for j in range(8)],
                    lambda n, c: (qT[:, n, c * 512:(c + 1) * 512] if n < 6 else qmT[:, n - 6, c * 512:(c + 1) * 512]))
            mixedT = S.alloc([128, 8, T], BF16)
            mem_attn(l, qmT, mixedT)
            sb_attn(qT, kT, vtok, mixedT)
            if mode == "Bmix":
                k.dbg_mixedT = mixedT
                return
            wo_stage(l, mixedT, ybuf)
            S.release(m)

        def a_mix(l, ybuf):
            import os
            m = S.mark()
            qT = yview(0, [128, 6, T], BF16)
            gT = yview(6 * T * 2, [128, 6, T], BF16)
            qmT = yview(12 * T * 2, [128, 2, T], BF16)
            tiles = [j * 128 for j in range(6)] + [2304 + j * 128 for j in range(6)] + [3072, 3200]

            def dst(n, c):
                tc = slice(c * 512, (c + 1) * 512)
                if n < 6:
                    return qT[:, n, tc]
                if n < 12:
                    return gT[:, n - 6, tc]
                return qmT[:, n - 12, tc]
            proj_fm(dr["a_w_in"][l], tiles, dst, func=lambda n: AF.Silu if n < 12 else None)
            mixedT = S.alloc([128, 8, T], BF16)
            mem_attn(l, qmT, mixedT)
            Wfi = S.alloc([128, 8, 1536], BF16)
            if os.environ.get("NOWFI"):
                MEMSET(Wfi, 0.01)
            for ch_ in range(0 if os.environ.get("NOWFI") else 6):
                WLOAD(Wfi[:, :, ch_ * 256:(ch_ + 1) * 256], dr["a_w_in"][l][:, 768 + ch_ * 256:768 + (ch_ + 1) * 256])
            c1bc = S.alloc([128, 768], F32)
            c1fm = S.alloc([128, 6], F32)
            gnfm = S.alloc([128, 6], F32)
            S.dma("sp", gnfm, dr["a_gnorm"][l].rearrange("(h p) -> p h", p=128), allow_slow_non_contiguous=True)
            if l == 0:
                MEMSET(c1bc, 1.0)
                MEMSET(c1fm, 1.0)
            else:
                a1bc = S.alloc([128, 768], F32)
                S.dma("sp", c1bc, dr["a_lower_bounds"][0:1, :].broadcast_to([128, 768]))
                S.dma("sp", a1bc, dr["a_lower_bounds"][1:2, :].broadcast_to([128, 768]))
                TT(c1bc, c1bc, a1bc, ALU.subtract)
                ACT(c1bc, c1bc, AF.Sigmoid)
                a1fm = S.alloc([128, 6], F32)
                S.dma("sp", c1fm, dr["a_lower_bounds"][0].rearrange("(h p) -> p h", p=128), allow_slow_non_contiguous=True)
                S.dma("sp", a1fm, dr["a_lower_bounds"][1].rearrange("(h p) -> p h", p=128), allow_slow_non_contiguous=True)
                TT(c1fm, c1fm, a1fm, ALU.subtract)
                ACT(c1fm, c1fm, AF.Sigmoid)
            Mf = S.alloc([128, 132], F32)
            S.dma("sp", Mf, dr["c_M"])
            hmf = S.alloc([128, 128], F32)
            S.dma("sp", hmf, dr["c_hmask"])
            Sst = S.alloc([128, 6, 128], F32)
            MEMSET(Sst, 0.0)
            sneg = S.alloc([128, 768], F32)
            wtmp = S.alloc([128, 768], F32)
            logf = S.alloc([128, 768], F32)
            einv = S.alloc([128, 768], F32)
            ktok = S.alloc([128, 768], BF16)
            vtk = S.alloc([128, 768], BF16)
            E = [S.alloc([128, 132], F32) for _ in range(2)]
            qt = [S.alloc([128, 128], BF16) for _ in range(2)]
            kt = [S.alloc([128, 128], BF16) for _ in range(2)]
            sc = [S.alloc([128, 128], BF16) for _ in range(2)]
            SpA = [S.alloc([128, 128], BF16) for _ in range(2)]
            SpB = [S.alloc([128, 128], BF16) for _ in range(2)]
            SA = S.alloc([128, 128], F32)
            ptmp = S.alloc([128, 128], F32)
            sAB = S.alloc([128, 2], F32)
            osq = [S.alloc([128, 128], BF16) for _ in range(2)]
            rs = [S.alloc([128, 128], F32) for _ in range(2)]
            o1 = S.alloc([128, 128], F32)
            n = 0
            ACUT = int(os.environ.get('ACUT', '9'))
            for i in range(16 if ACUT > 0 else 0):
                ts_ = slice(i * 128, (i + 1) * 128)
                pf = [P(0, [128, 512]), P(1, [128, 512]), P(2, [128, 512])]
                for ch in range(3):
                    for kk in range(8):
                        MM(pf[ch], xT[:, kk, ts_], Wfi[:, kk, ch * 512:(ch + 1) * 512], kk == 0, kk == 7)
                ACT(sneg[:, 0:512], pf[0], AF.Exp)
                ACT(sneg[:, 512:768], pf[1][:, 0:256], AF.Exp)
                TS(sneg, sneg, 1.0, None, ALU.add)
                RECIP(sneg, sneg)
                CP(vtk[:, 0:256], pf[1][:, 256:512])
                CP(vtk[:, 256:768], pf[2])
                ASUB = int(os.environ.get('ASUB', '9'))
                if ASUB < 2:
                    continue
                TT(wtmp, sneg, c1bc, ALU.mult)
                ACT(logf, wtmp, AF.Ln, scale=-1.0, bias=1.0)
                if ASUB < 3:
                    continue
                pdk = [P(3, [128, 512]), P(4, [128, 256])]
                MM(pdk[0][:, 0:256], Mf[:, 0:128], logf[:, 0:256])
                MM(pdk[0][:, 256:512], Mf[:, 0:128], logf[:, 256:512])
                MM(pdk[1], Mf[:, 0:128], logf[:, 512:768])
                ACT(einv[:, 0:512], pdk[0], AF.Exp, scale=-1.0)
                ACT(einv[:, 512:768], pdk[1], AF.Exp, scale=-1.0)
                TT(ktok, sneg, einv, ALU.mult)
                for h in range(6 if ACUT > 1 else 0):
                    r = n % 2
                    n += 1
                    hs = slice(h * 128, (h + 1) * 128)
                    PD = P(5, [128, 132])
                    MM(PD, logf[:, hs], Mf)
                    ACT(E[r], PD, AF.Exp)
                    STT(qt[r], qT[:, h, ts_], 128.0 ** -0.5, E[r][:, 0:128], ALU.mult, ALU.mult)
                    PK = P(6, [128, 128], BF16)
                    TR(PK, ktok[:, hs], identb)
                    ACT(kt[r], PK, AF.Identity, scale=c1fm[:, h:h + 1])
                    PS_ = P(6, [128, 128], F32, off=512)
                    MM(PS_, kt[r], qt[r])
                    TT(sc[r], PS_, hmf, ALU.mult)
                    if ACUT < 3:
                        continue
                    TS(sAB[:, 0:1], E[r][:, 63:64], c1fm[:, h:h + 1], None, ALU.mult)
                    TS(sAB[:, 1:2], E[r][:, 127:128], c1fm[:, h:h + 1], None, ALU.mult)
                    TS(SpA[r], Sst[:, h, :], E[r][:, 128:129], None, ALU.mult)
                    PA = P(7, [128, 128], F32)
                    MM(PA, ktok[0:64, hs], vtk[0:64, hs])
                    TS(ptmp, PA, sAB[:, 0:1], None, ALU.mult)
                    STT(SA, Sst[:, h, :], E[r][:, 129:130], ptmp, ALU.mult, ALU.add)
                    TS(SpB[r], SA, E[r][:, 130:131], None, ALU.mult)
                    PB = P(7, [128, 128], F32, off=512)
                    MM(PB, ktok[64:128, hs], vtk[64:128, hs])
                    TS(ptmp, PB, sAB[:, 1:2], None, ALU.mult)
                    STT(Sst[:, h, :], SA, E[r][:, 131:132], ptmp, ALU.mult, ALU.add)
                    if ACUT < 4:
                        continue
                    PO = P(6, [128, 128], F32, off=1024)
                    MMs(PO, vtk[:, hs], sc[r], True, True)
                    MMs(PO[:, 0:64], SpA[r], qt[r][:, 0:64], False, True)
                    MMs(PO[:, 64:128], SpB[r], qt[r][:, 64:128], False, True)
                    ACT(osq[r], PO, AF.Square)
                    PM = P(7, [128, 128], F32, off=1024)
                    MM(PM, ones128, osq[r])
                    ACT(rs[r], PM, AF.Ln, bias=1e-6)
                    ACT(rs[r], rs[r], AF.Exp, scale=-0.5)
                    STT(o1, PO, gnfm[:, h:h + 1], rs[r], ALU.mult, ALU.mult)
                    TT(mixedT[:, h, ts_], o1, gT[:, h, ts_], ALU.mult)
            if mode == "Amix":
                k.dbg_mixedT = mixedT
                return
            wo_stage(l, mixedT, ybuf)
            S.release(m)

        def dump_fm(src):
            m = S.mark()
            ot = [S.alloc([128, 1024], F32) for _ in range(2)]
            for tt in range(16):
                o = ot[tt % 2]
                pp = P(tt % 2, [128, 8, 128], BF16)
                for kk in range(8):
                    TR(pp[:, kk, :], src[:, kk, tt * 128:(tt + 1) * 128], identb)
                CP(o, pp.rearrange("p a b -> p (a b)"))
                k.outd.append(S.dma("sp", y_d[tt * 128:(tt + 1) * 128, :], o))
            S.release(m)

        k.outd = []
        load_xT()
        load_memT()
        k.ybuf_off = S.mark()
        ybuf = S.alloc([128, 8, T], F32)
        if mode == "moe0":
            init_ybuf(ybuf)
            moe_stage(0, ybuf)
            ln_stage(ybuf, 2, 0, final=True)
        elif mode == "ln0":
            init_ybuf(ybuf)
            ln_stage(ybuf, 2, 0, final=True)
        elif mode == "Bmix":
            kT = S.alloc([128, 6, T], BF16)
            vtok = S.alloc([128, 16, 768], BF16)
            kv_stage(kT, vtok)
            b_mix(2, kT, vtok, ybuf)
            dump_fm(k.dbg_mixedT)
        elif mode == "Amix":
            a_mix(0, ybuf)
            dump_fm(k.dbg_mixedT)
        else:
            kT = vtok = None
            nl = 4 if mode == "full" else int(mode[1:])
            for l in range(nl):
                if l < 2:
                    a_mix(l, ybuf)
                else:
                    if l == 2:
                        kT = S.alloc([128, 6, T], BF16)
                        vtok = S.alloc([128, 16, 768], BF16)
                        kv_stage(kT, vtok)
                    b_mix(l, kT, vtok, ybuf)
                ln_stage(ybuf, 0, l)
                init_ybuf(ybuf)
                moe_stage(l, ybuf)
                ln_stage(ybuf, 2, l, final=(l == nl - 1))
        S.final_wait("sp", k.outd)
        S.emit(sems)
        k.nops = {e: len(S.ops[e]) for e in S.ENG}
    return nc, cs, k


_CACHE = {}


def kernel(**inputs):
    if "nc" not in _CACHE:
        _CACHE["nc"] = build("full")
    nc, cs, _k = _CACHE["nc"]
    x = np.ascontiguousarray(inputs["x"], dtype=np.float32)
    mem = np.ascontiguousarray(inputs["mem"], dtype=np.float32)
    in_maps = []
    for b in range(8):
        m = {"x": x[b], "mem": mem[b]}
        for nm, _shp in WSPEC:
            m[nm] = np.ascontiguousarray(inputs[nm], dtype=np.float32)
        m.update(cs)
        in_maps.append(m)
    res = run_bass_kernel_spmd(nc, in_maps, core_ids=list(range(8)))
    return np.stack([np.asarray(r["y"], dtype=np.float32) for r in res.results], axis=0)
```

```python
import numpy as np
import concourse.bass as bass
import concourse.mybir as mybir

F32 = mybir.dt.float32
BF16 = mybir.dt.bfloat16
U8 = mybir.dt.uint8
I32 = mybir.dt.int32
ALU = mybir.AluOpType
AF = mybir.ActivationFunctionType
AX = mybir.AxisListType

_ESZ = {F32: 4, BF16: 2, U8: 1, I32: 4}

SB_BYTES = 207 * 1024
N_DSEM = 32
BUCKET = 2048


def esz(dt):
    return _ESZ[dt]


class Sched:
    ENG = ("pe", "act", "dve", "pool", "sp")

    def __init__(self, nc, big, pbig):
        self.nc = nc
        self.big = big
        self.pbig = pbig
        self.ops = {e: [] for e in self.ENG}
        self.keys = list(self.ENG[:4]) + ["d%d" % i for i in range(N_DSEM)]
        self.kidx = {k: i for i, k in enumerate(self.keys)}
        self.nk = len(self.keys)
        self.know = {e: [0] * self.nk for e in self.ENG}
        self.clock = {}
        self.seq = {k: 0 for k in self.keys}
        self.hist = {"sb": {}, "ps": {}, "dr": {}}
        self.ndma = 0
        self.ndma_sw = 0
        self.sb_top = 0
        self.marked = set()
        self.out_deps = []

    def alloc(self, shape, dt, name=None):
        n = int(np.prod(shape[1:])) * esz(dt)
        n = (n + 63) // 64 * 64
        off = self.sb_top
        self.sb_top += n
        assert self.sb_top <= SB_BYTES, ("SBUF overflow", name, self.sb_top)
        return self.view(off, shape, dt)

    def view(self, off, shape, dt):
        nb = int(np.prod(shape[1:])) * esz(dt)
        v = self.big[0:shape[0], off:off + nb]
        if dt != U8:
            v = v.bitcast(dt)
        if len(shape) == 3:
            v = v.rearrange("p (a b) -> p a b", a=shape[1])
        elif len(shape) == 4:
            v = v.rearrange("p (a b c) -> p a b c", a=shape[1], b=shape[2])
        return v

    def mark(self):
        return self.sb_top

    def release(self, m):
        self.sb_top = m

    def psum(self, bank, shape, dt=F32, off=0):
        nb = int(np.prod(shape[1:])) * esz(dt)
        assert off + nb <= 2048 * 8
        e0 = (bank * 2048 + off) // 4
        v = self.pbig[0:shape[0], e0:e0 + (nb + 3) // 4]
        if dt != F32:
            v = v.bitcast(dt)
        if len(shape) == 3:
            v = v.rearrange("p (a b) -> p a b", a=shape[1])
        return v

    def rect(self, ap):
        t = ap.tensor
        nm = t.name
        e = esz(ap.dtype)
        a = ap.ap
        off = int(ap.offset)
        if nm == "big":
            sp = "sb"
        elif nm == "pbig":
            sp = "ps"
        else:
            return ("dr", nm, 0, 1, 0, 1)
        pstep, pn = a[0]
        if pstep == 0:
            pstep = 1 << 40
        p0 = off // pstep if pstep < (1 << 40) else 0
        fo = off - p0 * pstep if pstep < (1 << 40) else off
        span = 1
        for st, cn in a[1:]:
            span += abs(st) * (cn - 1)
        b0 = fo * e
        b1 = (fo + span) * e
        p1 = p0 + pn
        if sp == "ps":
            b0 = (b0 // 2048) * 2048
            b1 = ((b1 + 2047) // 2048) * 2048
            p0 = (p0 // 32) * 32
            p1 = ((p1 + 31) // 32) * 32
        return (sp, None, p0, p1, b0, b1)

    def _deps_for(self, accesses, own=None):
        deps = {}
        regs = []
        for r, kind in accesses:
            sp, nm, p0, p1, b0, b1 = r
            h = self.hist[sp]
            if sp == "dr":
                bks = [nm]
            else:
                bks = range(b0 // BUCKET, (b1 - 1) // BUCKET + 1)
            for bk in bks:
                d = h.get(bk)
                if d is None:
                    d = {}
                    h[bk] = d
                dead = []
                for (k2, kind2, q0, q1, c0, c1), s2 in d.items():
                    if q0 < p1 and p0 < q1 and c0 < b1 and b0 < c1:
                        rr_ps = (sp == "ps" and kind == "r" and kind2 == "r" and k2 != own)
                        if kind == "w" or kind2 == "w" or rr_ps:
                            skip = False
                            if k2 == own:
                                if own == "pe" or not (kind == "r" and kind2 == "w"):
                                    skip = True
                            if not skip and deps.get(k2, 0) < s2:
                                deps[k2] = s2
                        if kind == "w" and p0 <= q0 and q1 <= p1 and b0 <= c0 and c1 <= b1:
                            dead.append((k2, kind2, q0, q1, c0, c1))
                for kk in dead:
                    del d[kk]
                regs.append((d, kind, p0, p1, b0, b1))
        return deps, regs

    def _register(self, regs, key, seq):
        for d, kind, p0, p1, b0, b1 in regs:
            d[(key, kind, p0, p1, b0, b1)] = seq

    def op(self, eng, fn, reads=(), writes=()):
        acc = [(self.rect(a), "r") for a in reads] + [(self.rect(a), "w") for a in writes]
        key = eng
        deps, regs = self._deps_for(acc, own=key)
        self.seq[key] += 1
        seq = self.seq[key]
        waits = self._resolve(eng, deps, own=key)
        clk = list(self.know[eng])
        clk[self.kidx[key]] = seq
        self.clock[(key, seq)] = clk
        self._register(regs, key, seq)
        self.ops[eng].append({"fn": fn, "waits": waits, "inc": (key, seq)})

    def _resolve(self, eng, deps, own=None):
        know = list(self.know[eng])
        waits = []
        items = sorted(deps.items(), key=lambda kv: -kv[1])
        for k, s in items:
            if k == own:
                pass
            i = self.kidx[k]
            if know[i] >= s:
                continue
            waits.append((k, s))
            self.marked.add((k, s))
            c = self.clock[(k, s)]
            know = [a if a >= b else b for a, b in zip(know, c)]
        self.know[eng] = know
        return waits

    def dma(self, eng, out, in_, reads=None, writes=None, **kw):
        rd = [in_] if reads is None else reads
        wr = [out] if writes is None else writes
        acc = [(self.rect(a), "r") for a in rd] + [(self.rect(a), "w") for a in wr]
        deps, regs = self._deps_for(acc)
        half = N_DSEM // 2
        if eng == "pool":
            k = "d%d" % (half + self.ndma_sw % half)
            self.ndma_sw += 1
        else:
            k = "d%d" % (self.ndma % half)
            self.ndma += 1
        prev = self.seq[k]
        if prev > 0:
            deps[k] = max(deps.get(k, 0), prev)
        self.seq[k] += 1
        seq = self.seq[k]
        waits = self._resolve(eng, deps)
        clk = list(self.know[eng])
        clk[self.kidx[k]] = seq
        self.clock[(k, seq)] = clk
        self._register(regs, k, seq)
        self.ops[eng].append({"dma": (out, in_), "kw": kw, "waits": waits, "inc": (k, seq)})
        return (k, seq)

    def final_wait(self, eng, deps):
        waits = self._resolve(eng, dict(deps))
        self.ops[eng].append({"waits": waits})

    def emit(self, sems):
        rank = {}
        for k in self.ENG[:4]:
            ms = sorted(s for (kk, s) in self.marked if kk == k)
            rank[k] = {s: i + 1 for i, s in enumerate(ms)}

        def val(k, s):
            if k[0] == "d" and k[1:].isdigit():
                return 16 * s
            return rank[k][s]

        nc = self.nc
        engobj = {"pe": "tensor", "act": "scalar", "dve": "vector", "pool": "gpsimd", "sp": "sync"}
        with nc.Block() as block:
            for e in self.ENG:
                ops = self.ops[e]

                def body(engine, ops=ops, e=e):
                    for o in ops:
                        for (k, s) in o["waits"]:
                            engine.wait_ge(sems[k], val(k, s))
                        if "fn" in o:
                            ins = o["fn"](engine)
                            k, s = o["inc"]
                            if (k, s) in self.marked:
                                ins.then_inc(sems[k], 1)
                        elif "dma" in o:
                            out, in_ = o["dma"]
                            k, s = o["inc"]
                            engine.dma_start(out=out, in_=in_, **o["kw"]).then_inc(sems[k], 16)

                getattr(block, engobj[e])(body)
from contextlib import ExitStack
from concourse.bass_utils import run_bass_kernel_spmd

T = 2048
D = 1024
ALPHA = 8 ** 0.25
WSPEC = [("a_w_in", [2, 1024, 3328]), ("a_lower_bounds", [2, 768]), ("a_gnorm", [2, 768]),
         ("b_w_in", [2, 1024, 1024]), ("w_kv_shared", [1024, 1536]), ("w_mem_kv", [4, 1024, 512]),
         ("w_o", [4, 1024, 1024]), ("ln_mix_g", [4, 1024]), ("ln_mix_b", [4, 1024]),
         ("ln_ffn_g", [4, 1024]), ("ln_ffn_b", [4, 1024]), ("w_group", [4, 1024, 4]),
         ("b_group", [4, 4]), ("w_router", [4, 1024, 16]), ("b_router", [4, 16]),
         ("w_gate", [4, 16, 1024, 256]), ("w_up", [4, 16, 1024, 256]), ("w_down", [4, 16, 256, 1024])]


def make_consts():
    c = {}
    c["c_ident"] = np.eye(128, dtype=np.float32)
    sel = np.zeros((16, 16, 128), np.float32)
    for e in range(16):
        sel[e, e, :] = 1.0
    c["c_sel"] = sel
    s = np.arange(128)
    c["c_hmask"] = ((s[:, None] // 64 == s[None, :] // 64) & (s[None, :] >= s[:, None])).astype(np.float32)
    M = np.zeros((128, 132), np.float32)
    for t in range(128):
        ch = t // 64
        ref = ch * 64 + 31
        for sp in range(ch * 64, ch * 64 + 64):
            M[sp, t] = float(sp <= t) - float(sp <= ref)
    for ch in range(2):
        for sp in range(ch * 64, ch * 64 + 64):
            M[sp, 128 + 2 * ch] = float(sp <= ch * 64 + 31)
            M[sp, 129 + 2 * ch] = 1.0
    c["c_M"] = M
    c["c_U"] = (s[:, None] > s[None, :]).astype(np.float32)
    c["c_L"] = (s[:, None] <= s[None, :]).astype(np.float32)
    c["c_dmask"] = (s[:, None] < s[None, :]).astype(np.float32)
    oz = np.zeros((128, 2, 128), np.float32)
    oz[:, 0, 0:64] = 1.0
    oz[:, 1, 64:128] = 1.0
    c["c_onesz"] = oz
    return c


class K:
    pass


def build(mode="full"):
    nc = bass.Bass("TRN2", target_bir_lowering=False)
    k = K()
    dr = {}
    dr["x"] = nc.dram_tensor("x", [T, D], F32, kind="ExternalInput").ap()
    dr["mem"] = nc.dram_tensor("mem", [256, D], F32, kind="ExternalInput").ap()
    for nm, shp in WSPEC:
        dr[nm] = nc.dram_tensor(nm, shp, F32, kind="ExternalInput").ap()
    cs = make_consts()
    for nm, arr in cs.items():
        dr[nm] = nc.dram_tensor(nm, list(arr.shape), F32, kind="ExternalInput").ap()
    y_d = nc.dram_tensor("y", [T, D], F32, kind="ExternalOutput").ap()
    with ExitStack() as es:
        big = es.enter_context(nc.sbuf_tensor("big", [128, SB_BYTES], U8))
        pbig = es.enter_context(nc.psum_tensor("pbig", [128, 4096], F32))
        S = Sched(nc, big, pbig)
        sems = {kk: es.enter_context(nc.semaphore("q%d" % i)) for i, kk in enumerate(S.keys)}
        P = S.psum

        def aps(*xs):
            return [a for a in xs if a is not None and not isinstance(a, (int, float))]

        def MM(out, lhsT, rhs, start=True, stop=True):
            S.op("pe", lambda e: e.matmul(out, lhsT, rhs, start=start, stop=stop), reads=[lhsT, rhs], writes=[out])

        def TR(out, in_, ident):
            S.op("pe", lambda e: e.transpose(out, in_, ident), reads=[in_, ident], writes=[out])

        def ACT(out, in_, func, scale=None, bias=None, accum=None):
            kw = {}
            if scale is not None:
                kw["scale"] = scale
            if bias is not None:
                kw["bias"] = bias
            if accum is not None:
                kw["accum_out"] = accum
            S.op("act", lambda e: e.activation(out, in_, func, **kw), reads=aps(in_, scale, bias), writes=aps(out, accum))

        def TT(out, a, b, op, eng="dve"):
            S.op(eng, lambda e: e.tensor_tensor(out, a, b, op), reads=[a, b], writes=[out])

        def TS(out, a, s1, s2, op0, op1=None, eng="dve"):
            if op1 is None:
                S.op(eng, lambda e: e.tensor_scalar(out, a, s1, None, op0), reads=aps(a, s1), writes=[out])
            else:
                S.op(eng, lambda e: e.tensor_scalar(out, a, s1, s2, op0, op1), reads=aps(a, s1, s2), writes=[out])

        def STT(out, in0, sc, in1, op0, op1, eng="dve"):
            S.op(eng, lambda e: e.scalar_tensor_tensor(out, in0, sc, in1, op0, op1), reads=aps(in0, sc, in1), writes=[out])

        def CP(out, in_, eng="dve"):
            if eng == "act":
                S.op("act", lambda e: e.copy(out, in_), reads=[in_], writes=[out])
            else:
                S.op(eng, lambda e: e.tensor_copy(out, in_), reads=[in_], writes=[out])

        def RED(out, in_, op):
            S.op("dve", lambda e: e.tensor_reduce(out, in_, AX.X, op), reads=[in_], writes=[out])

        def RECIP(out, in_):
            S.op("dve", lambda e: e.reciprocal(out, in_), reads=[in_], writes=[out])

        def MEMSET(out, v, eng="dve"):
            S.op(eng, lambda e: e.memset(out, v), writes=[out])

        def WLOAD(dst, src):
            S.dma("pool", dst, src.rearrange("(k p) n -> p k n", p=128))

        xT = S.alloc([128, 8, T], BF16)
        identf = S.alloc([128, 128], F32)
        identb = S.alloc([128, 128], BF16)
        onesD = S.alloc([128, 128], BF16)
        selb = S.alloc([16, 16, 128], BF16)
        lnp = S.alloc([128, 4, 4, 8], F32)
        S.dma("sp", identf, dr["c_ident"])
        S.dma("pool", identb, dr["c_ident"])
        S.dma("pool", selb, dr["c_sel"])
        MEMSET(onesD, 1.0 / 1024.0)
        for wi, nm in enumerate(["ln_mix_g", "ln_mix_b", "ln_ffn_g", "ln_ffn_b"]):
            for l in range(4):
                S.dma("sp", lnp[:, wi, l, :], dr[nm][l].rearrange("(k p) -> p k", p=128), allow_slow_non_contiguous=True)

        def load_xT():
            m = S.mark()
            xt = [S.alloc([128, 1024], BF16) for _ in range(2)]
            for i in range(16):
                b = xt[i % 2]
                S.dma("pool", b, dr["x"][i * 128:(i + 1) * 128, :])
                pt = P(i % 2, [128, 8, 128], BF16)
                for kk in range(8):
                    TR(pt[:, kk, :], b[:, kk * 128:(kk + 1) * 128], identb)
                CP(xT[:, :, i * 128:(i + 1) * 128], pt, eng="act" if i % 2 else "dve")
            S.release(m)

        def ln_stage(ybuf, wi, l, final=False):
            m = S.mark()
            ysq = S.alloc([128, 8, 512], BF16)
            yb = S.alloc([128, 8, 512], BF16)
            mean_sb = S.alloc([128, 512], F32)
            m2 = S.alloc([128, 512], F32)
            rstd = S.alloc([128, 512], F32)
            otile = [S.alloc([128, 1024], F32) for _ in range(2)] if final else None
            for c in range(4):
                tc = slice(c * 512, (c + 1) * 512)
                for kk in range(8):
                    ACT(ysq[:, kk, :], ybuf[:, kk, tc], AF.Square)
                    CP(yb[:, kk, :], ybuf[:, kk, tc], eng="dve")
                mps = P(0, [128, 512])
                sps = P(1, [128, 512])
                for kk in range(8):
                    MM(mps, onesD, yb[:, kk, :], kk == 0, kk == 7)
                for kk in range(8):
                    MM(sps, onesD, ysq[:, kk, :], kk == 0, kk == 7)
                CP(mean_sb, mps, eng="act")
                ACT(m2, mps, AF.Square)
                TT(m2, sps, m2, ALU.subtract)
                ACT(m2, m2, AF.Ln, bias=1e-5)
                ACT(rstd, m2, AF.Exp, scale=-0.5)
                for kk in range(8):
                    yk = ybuf[:, kk, tc]
                    TT(yk, yk, mean_sb, ALU.subtract)
                    TT(yk, yk, rstd, ALU.mult)
                    if final:
                        ACT(yk, yk, AF.Identity, scale=lnp[:, wi, l, kk:kk + 1], bias=lnp[:, wi + 1, l, kk:kk + 1])
                    else:
                        ACT(xT[:, kk, tc], yk, AF.Identity, scale=lnp[:, wi, l, kk:kk + 1], bias=lnp[:, wi + 1, l, kk:kk + 1])
                if final:
                    for j in range(4):
                        tt = c * 4 + j
                        ot = otile[tt % 2]
                        for half in range(2):
                            pp = P(2 + half, [128, 4, 128])
                            for q in range(4):
                                kk = half * 4 + q
                                TR(pp[:, q, :], ybuf[:, kk, tt * 128:(tt + 1) * 128], identf)
                            CP(ot[:, half * 512:(half + 1) * 512], pp.rearrange("p a b -> p (a b)"), eng="act" if half else "dve")
                        k.outd.append(S.dma("sp", y_d[tt * 128:(tt + 1) * 128, :], ot))
            S.release(m)

        def moe_stage(l, ybuf):
            m = S.mark()
            wr = S.alloc([128, 8, 20], BF16)
            S.dma("pool", wr[:, :, 0:4], dr["w_group"][l].rearrange("(k p) n -> p k n", p=128))
            S.dma("pool", wr[:, :, 4:20], dr["w_router"][l].rearrange("(k p) n -> p k n", p=128))
            bias = S.alloc([128, 20], F32)
            S.dma("sp", bias[:, 0:4], dr["b_group"][l:l + 1, :].broadcast_to([128, 4]))
            S.dma("sp", bias[:, 4:20], dr["b_router"][l:l + 1, :].broadcast_to([128, 16]))
            PR = P(7, [128, 16, 20])
            for i in range(16):
                for kk in range(8):
                    MM(PR[:, i, :], xT[:, kk, i * 128:(i + 1) * 128], wr[:, kk, :], kk == 0, kk == 7)
            L = S.alloc([128, 16, 20], F32)
            TT(L, PR, bias.unsqueeze(1).broadcast_to([128, 16, 20]), ALU.add)
            Lg = L[:, :, 0:4]
            Le = L[:, :, 4:20]
            gmax = S.alloc([128, 16], F32)
            RED(gmax, Lg, ALU.max)
            gb3 = gmax.unsqueeze(2).broadcast_to([128, 16, 4])
            dg = S.alloc([128, 16, 4], F32)
            TT(dg, Lg, gb3, ALU.subtract)
            ACT(dg, dg, AF.Exp)
            sg = S.alloc([128, 16], F32)
            RED(sg, dg, ALU.add)
            ptop = S.alloc([128, 16], F32)
            RECIP(ptop, sg)
            ohg = S.alloc([128, 16, 4], F32)
            TT(ohg, Lg, gb3, ALU.is_equal)
            TS(ohg, ohg, -1.0, 1.0e4, ALU.add, ALU.mult)
            Lm = S.alloc([128, 16, 16], F32)
            Lm4 = Lm.rearrange("p a (g e) -> p a g e", g=4)
            TT(Lm4, Le.rearrange("p a (g e) -> p a g e", g=4), ohg.unsqueeze(3).broadcast_to([128, 16, 4, 4]), ALU.add)
            m1 = S.alloc([128, 16], F32)
            RED(m1, Lm, ALU.max)
            oh1 = S.alloc([128, 16, 16], F32)
            TT(oh1, Lm, m1.unsqueeze(2).broadcast_to([128, 16, 16]), ALU.is_equal)
            Lm2 = S.alloc([128, 16, 16], F32)
            STT(Lm2, oh1, -1.0e4, Lm, ALU.mult, ALU.add)
            mm2 = S.alloc([128, 16], F32)
            RED(mm2, Lm2, ALU.max)
            oh2 = S.alloc([128, 16, 16], F32)
            TT(oh2, Lm2, mm2.unsqueeze(2).broadcast_to([128, 16, 16]), ALU.is_equal)
            ee = S.alloc([128, 16], F32)
            TT(ee, mm2, m1, ALU.subtract)
            ACT(ee, ee, AF.Exp)
            den = S.alloc([128, 16], F32)
            TS(den, ee, 1.0, None, ALU.add)
            RECIP(den, den)
            g1 = S.alloc([128, 16], F32)
            TT(g1, den, ptop, ALU.mult)
            g2 = S.alloc([128, 16], F32)
            TT(g2, g1, ee, ALU.mult)
            TT(oh1, oh1, g1.unsqueeze(2).broadcast_to([128, 16, 16]), ALU.mult)
            TT(oh2, oh2, g2.unsqueeze(2).broadcast_to([128, 16, 16]), ALU.mult)
            gb = S.alloc([128, 16, 16], BF16)
            TT(gb, oh1, oh2, ALU.add)
            if mode == "gates":
                k.dbg = (oh1, oh2)
            PT = P(5, [16, T], BF16)
            for i in range(16):
                TR(PT[:, i * 128:(i + 1) * 128], gb[:, i, :], identb)
            gatesT = S.alloc([16, T], BF16)
            CP(gatesT, PT)
            wg = [S.alloc([128, 8, 256], BF16) for _ in range(2)]
            wu = [S.alloc([128, 8, 256], BF16) for _ in range(2)]
            wd = [S.alloc([128, 2, 1024], BF16) for _ in range(2)]
            hT = S.alloc([128, 2, T], BF16)
            sgt = [S.alloc([128, 512], F32) for _ in range(2)]
            nblk = [0]
            ndn = [0]
            gpcur = [None]
            ngp = [0]

            def gateup_mm(e, c, ft):
                b = e % 2
                tc = slice(c * 512, (c + 1) * 512)
                bb = nblk[0] % 2
                nblk[0] += 1
                Pg = P(bb * 2, [128, 512])
                Pu = P(bb * 2 + 1, [128, 512])
                for kk in range(8):
                    MM(Pg, wg[b][:, kk, ft * 128:(ft + 1) * 128], xT[:, kk, tc], kk == 0, kk == 7)
                for kk in range(8):
                    MM(Pu, wu[b][:, kk, ft * 128:(ft + 1) * 128], xT[:, kk, tc], kk == 0, kk == 7)
                return (bb, Pg, Pu, ft, tc, gpcur[0])

            def gateup_ev(st):
                bb, Pg, Pu, ft, tc, Gp_ = st
                ACT(sgt[bb], Pg, AF.Silu)
                TT(sgt[bb], sgt[bb], Pu, ALU.mult)
                TT(hT[:, ft, tc], sgt[bb], Gp_, ALU.mult)

            def down(e, c, kks):
                b = e % 2
                tc = slice(c * 512, (c + 1) * 512)
                for kk in kks:
                    Pd = P(5 + ndn[0] % 2, [128, 512])
                    ndn[0] += 1
                    MM(Pd, wd[b][:, 0, kk * 128:(kk + 1) * 128], hT[:, 0, tc], True, False)
                    MM(Pd, wd[b][:, 1, kk * 128:(kk + 1) * 128], hT[:, 1, tc], False, True)
                    TT(ybuf[:, kk, tc], ybuf[:, kk, tc], Pd, ALU.add)

            prev = None
            for e in range(16):
                b = e % 2
                WLOAD(wg[b], dr["w_gate"][l, e])
                WLOAD(wu[b], dr["w_up"][l, e])
                WLOAD(wd[b], dr["w_down"][l, e])
                for c in range(4):
                    tc = slice(c * 512, (c + 1) * 512)
                    Gp = P(4 if ngp[0] % 2 == 0 else 7, [128, 512])
                    ngp[0] += 1
                    gpcur[0] = Gp
                    MM(Gp, selb[0:16, e, :], gatesT[0:16, tc])
                    for ft in range(2):
                        st = gateup_mm(e, c, ft)
                        if prev is not None:
                            down(prev[0], prev[1], range(4 * ft, 4 * ft + 4))
                        gateup_ev(st)
                    prev = (e, c)
            down(prev[0], prev[1], range(0, 8))
            S.release(m)

        def init_ybuf(ybuf):
            for kk in range(8):
                TS(ybuf[:, kk, :], xT[:, kk, :], ALPHA, None, ALU.mult)


        ones1 = S.alloc([128, 128], BF16)
        MEMSET(ones1, 1.0)
        ones128 = S.alloc([128, 128], BF16)
        MEMSET(ones128, 1.0 / 128.0)
        Ub = S.alloc([128, 128], BF16)
        Lcb = S.alloc([128, 128], BF16)
        S.dma("pool", Ub, dr["c_U"])
        S.dma("pool", Lcb, dr["c_L"])
        memT = S.alloc([128, 8, 256], BF16)

        def MMs(out, lhsT, rhs, start, stop):
            S.op("pe", lambda e: e.matmul(out, lhsT, rhs, start=start, stop=stop, skip_group_check=True), reads=[lhsT, rhs], writes=[out])

        def load_memT():
            m = S.mark()
            mt_ = S.alloc([128, 2, 1024], BF16)
            S.dma("pool", mt_, dr["mem"].rearrange("(a p) d -> p a d", p=128))
            for a in range(2):
                pt = P(2 + a, [128, 8, 128], BF16)
                for kk in range(8):
                    TR(pt[:, kk, :], mt_[:, a, kk * 128:(kk + 1) * 128], identb)
                CP(memT[:, :, a * 128:(a + 1) * 128], pt)
            S.release(m)

        def proj_fm(wsrc, tiles, dst_fn, func=None):
            m = S.mark()
            wb = [S.alloc([128, 8, 128], BF16) for _ in range(3)]
            for n, c0 in enumerate(tiles):
                w = wb[n % 3]
                WLOAD(w, wsrc[:, c0:c0 + 128])
                for c in range(4):
                    tc = slice(c * 512, (c + 1) * 512)
                    pp = P((n * 4 + c) % 2, [128, 512])
                    for kk in range(8):
                        MM(pp, w[:, kk, :], xT[:, kk, tc], kk == 0, kk == 7)
                    if func is not None and func(n) is not None:
                        ACT(dst_fn(n, c), pp, func(n))
                    else:
                        CP(dst_fn(n, c), pp, eng="act" if (n * 4 + c) % 2 else "dve")
            S.release(m)

        def mem_attn(l, qmT, mixedT):
            m = S.mark()
            wm = S.alloc([128, 8, 512], BF16)
            WLOAD(wm, dr["w_mem_kv"][l])
            kmT = S.alloc([128, 2, 256], BF16)
            vm = S.alloc([128, 2, 256], BF16)
            for j in range(2):
                pp = P(2 + j, [128, 256])
                for kk in range(8):
                    MM(pp, wm[:, kk, j * 128:(j + 1) * 128], memT[:, kk, :], kk == 0, kk == 7)
                CP(kmT[:, j, :], pp)
            for mt in range(2):
                pp = P(4 + mt, [128, 256])
                for kk in range(8):
                    MM(pp, memT[:, kk, mt * 128:(mt + 1) * 128], wm[:, kk, 256:512], kk == 0, kk == 7)
                CP(vm[:, mt, :], pp, eng="act")
            em = [S.alloc([128, 512], BF16) for _ in range(4)]
            rec = S.alloc([128, 512], F32)
            n = 0
            for j in range(2):
                for half in range(2):
                    pb = 64 * half
                    for c in range(4):
                        tc = slice(c * 512, (c + 1) * 512)
                        es_ = []
                        for mt in range(2):
                            zp = P(2 + (n % 2), [128, 512])
                            MM(zp, kmT[pb:pb + 64, j, mt * 128:(mt + 1) * 128], qmT[pb:pb + 64, j, tc])
                            eb = em[n % 4]
                            ACT(eb, zp, AF.Exp, scale=0.125)
                            es_.append(eb)
                            n += 1
                        pn = P(4, [128, 512])
                        pd = P(5, [128, 512])
                        for mt in range(2):
                            MM(pn, vm[:, mt, j * 128:(j + 1) * 128], es_[mt], mt == 0, mt == 1)
                        for mt in range(2):
                            MM(pd, ones1, es_[mt], mt == 0, mt == 1)
                        RECIP(rec[pb:pb + 64, :], pd[pb:pb + 64, :])
                        TT(mixedT[pb:pb + 64, 6 + j, tc], pn[pb:pb + 64, :], rec[pb:pb + 64, :], ALU.mult)
            S.release(m)

        def wo_stage(l, mixedT, ybuf):
            m = S.mark()
            wob = [S.alloc([128, 8, 256], BF16) for _ in range(2)]
            n = 0
            for q in range(4):
                w = wob[q % 2]
                WLOAD(w, dr["w_o"][l][:, q * 256:(q + 1) * 256])
                for kq in range(2):
                    kk = 2 * q + kq
                    for c in range(4):
                        tc = slice(c * 512, (c + 1) * 512)
                        pp = P(n % 2, [128, 512])
                        n += 1
                        for f in range(8):
                            MM(pp, w[:, f, kq * 128:(kq + 1) * 128], mixedT[:, f, tc], f == 0, f == 7)
                        STT(ybuf[:, kk, tc], xT[:, kk, tc], ALPHA, pp, ALU.mult, ALU.add)
            S.release(m)

        def kv_stage(kT, vtok):
            proj_fm(dr["w_kv_shared"], [j * 128 for j in range(6)], lambda n, c: kT[:, n, c * 512:(c + 1) * 512])
            m = S.mark()
            wv = S.alloc([128, 8, 768], BF16)
            WLOAD(wv, dr["w_kv_shared"][:, 768:1536])
            for i in range(16):
                ts_ = slice(i * 128, (i + 1) * 128)
                pa = P(2 + 2 * (i % 2), [128, 512])
                pb_ = P(3 + 2 * (i % 2), [128, 256])
                for kk in range(8):
                    MM(pa, xT[:, kk, ts_], wv[:, kk, 0:512], kk == 0, kk == 7)
                for kk in range(8):
                    MM(pb_, xT[:, kk, ts_], wv[:, kk, 512:768], kk == 0, kk == 7)
                CP(vtok[:, i, 0:512], pa, eng="act")
                CP(vtok[:, i, 512:768], pb_, eng="dve")
            S.release(m)

        def sb_attn(qT, kT, vtok, mixedT):
            m = S.mark()
            dm = S.alloc([128, 4, 512], BF16)
            dmf = S.alloc([128, 128], F32)
            S.dma("sp", dmf, dr["c_dmask"])
            for q in range(4):
                if q > 0:
                    MEMSET(dm[:, q, 0:q * 128], 0.0)
                CP(dm[:, q, q * 128:(q + 1) * 128], dmf)
                if q < 3:
                    MEMSET(dm[:, q, (q + 1) * 128:512], 1.0)
            yo = [32768]

            def ya(shape, dt):
                nb = int(np.prod(shape[1:])) * esz(dt)
                v = yview(yo[0], shape, dt)
                yo[0] += nb
                assert yo[0] <= 65536
                return v
            REF, RLF, RLB, RAB = 5, 3, 4, 3
            ef = [ya([128, 512], F32) for _ in range(REF)]
            Lf = [ya([128, 512], F32) for _ in range(RLF)]
            Lb = [ya([128, 512], BF16) for _ in range(RLB)]
            ab = [ya([128, 512], BF16) for _ in range(RAB)]
            zbanks = [0, 1, 6, 7]
            items = []
            for j in range(6):
                for c in range(4):
                    top = 4 * c + 3
                    for I in range(top, -1, -1):
                        for half in range(2):
                            items.append((j, half, c, I, half, I == top, I == 0))
            N = len(items)

            def Zb(n):
                return P(zbanks[n % 4], [128, 512])

            def accb(n):
                return P(2 + items[n][4], [128, 512])

            def Ob(n):
                return P(4 + items[n][4], [128, 512])

            def s0(n):
                j, half, c, I, _, _, _ = items[n]
                pb = 64 * half
                MM(Zb(n), kT[pb:pb + 64, j, I * 128:(I + 1) * 128], qT[pb:pb + 64, j, c * 512:(c + 1) * 512])

            def s1(n):
                ACT(ef[n % REF], Zb(n), AF.Exp, scale=0.125)
                ACT(Lf[n % RLF], ef[n % REF], AF.Ln, bias=1.0)

            def s2(n):
                j, half, c, I, _, _, _ = items[n]
                if I >= 4 * c:
                    TT(Lb[n % RLB], Lf[n % RLF], dm[:, I - 4 * c, :], ALU.mult)
                else:
                    CP(Lb[n % RLB], Lf[n % RLF], eng="pool")
                STT(ef[n % REF], Zb(n), 0.125, Lf[n % RLF], ALU.mult, ALU.subtract)

            def s3a(n):
                MMs(accb(n), Ub, Lb[n % RLB], items[n][5], True)

            def s3b(n):
                TT(ef[n % REF], ef[n % REF], accb(n), ALU.subtract)

            def s3c(n):
                MMs(accb(n), Lcb, Lb[n % RLB], False, True)

            def s4(n):
                j, half, c, I, _, _, _ = items[n]
                ACT(ab[n % RAB], ef[n % REF], AF.Exp)
                if I >= 4 * c:
                    TT(ab[n % RAB], ab[n % RAB], dm[:, I - 4 * c, :], ALU.mult)

            def s5(n):
                j, half, c, I, _, first, last = items[n]
                MMs(Ob(n), vtok[:, I, j * 128:(j + 1) * 128], ab[n % RAB], first, last)
                if last:
                    pb = 64 * half
                    CP(mixedT[pb:pb + 64, j, c * 512:(c + 1) * 512], Ob(n)[pb:pb + 64, :], eng="act")

            def ok(n):
                return 0 <= n < N
            for t in range(N + 7):
                if ok(t - 5):
                    s4(t - 5)
                if ok(t):
                    s0(t)
                if ok(t - 1):
                    s1(t - 1)
                if ok(t - 2):
                    s2(t - 2)
                if ok(t - 5):
                    s3c(t - 5)
                if ok(t - 3):
                    s3a(t - 3)
                if ok(t - 4):
                    s3b(t - 4)
                if ok(t - 6):
                    s5(t - 6)
            S.release(m)

        def yview(off, shape, dt):
            return S.view(k.ybuf_off + off, shape, dt)

        def b_mix(l, kT, vtok, ybuf):
            m = S.mark()
            qT = yview(0, [128, 6, T], BF16)
            qmT = yview(6 * T * 2, [128, 2, T], BF16)
            proj_fm(dr["b_w_in"][l - 2], [j * 128 for j in range(8)],
                    lambda n, c: (qT[:, n, c * 512:(c + 1) * 512] if n < 6 else qmT[:, n - 6, c * 512:(c + 1) * 512]))
            mixedT = S.alloc([128, 8, T], BF16)
            mem_attn(l, qmT, mixedT)
            sb_attn(qT, kT, vtok, mixedT)
            if mode == "Bmix":
                k.dbg_mixedT = mixedT
                return
            wo_stage(l, mixedT, ybuf)
            S.release(m)

        def a_mix(l, ybuf):
            import os
            m = S.mark()
            qT = yview(0, [128, 6, T], BF16)
            gT = yview(6 * T * 2, [128, 6, T], BF16)
            qmT = yview(12 * T * 2, [128, 2, T], BF16)
            tiles = [j * 128 for j in range(6)] + [2304 + j * 128 for j in range(6)] + [3072, 3200]

            def dst(n, c):
                tc = slice(c * 512, (c + 1) * 512)
                if n < 6:
                    return qT[:, n, tc]
                if n < 12:
                    return gT[:, n - 6, tc]
                return qmT[:, n - 12, tc]
            proj_fm(dr["a_w_in"][l], tiles, dst, func=lambda n: AF.Silu if n < 12 else None)
            mixedT = S.alloc([128, 8, T], BF16)
            mem_attn(l, qmT, mixedT)
            Wfi = S.alloc([128, 8, 1536], BF16)
            if os.environ.get("NOWFI"):
                MEMSET(Wfi, 0.01)
            for ch_ in range(0 if os.environ.get("NOWFI") else 6):
                WLOAD(Wfi[:, :, ch_ * 256:(ch_ + 1) * 256], dr["a_w_in"][l][:, 768 + ch_ * 256:768 + (ch_ + 1) * 256])
            c1bc = S.alloc([128, 768], F32)
            c1fm = S.alloc([128, 6], F32)
            gnfm = S.alloc([128, 6], F32)
            S.dma("sp", gnfm, dr["a_gnorm"][l].rearrange("(h p) -> p h", p=128), allow_slow_non_contiguous=True)
            if l == 0:
                MEMSET(c1bc, 1.0)
                MEMSET(c1fm, 1.0)
            else:
                mk_ = S.mark()
                a1bc = S.alloc([128, 768], F32)
                S.release(mk_)
                S.dma("sp", c1bc, dr["a_lower_bounds"][0:1, :].broadcast_to([128, 768]))
                S.dma("sp", a1bc, dr["a_lower_bounds"][1:2, :].broadcast_to([128, 768]))
                TT(c1bc, c1bc, a1bc, ALU.subtract)
                ACT(c1bc, c1bc, AF.Sigmoid)
                a1fm = S.alloc([128, 6], F32)
                S.dma("sp", c1fm, dr["a_lower_bounds"][0].rearrange("(h p) -> p h", p=128), allow_slow_non_contiguous=True)
                S.dma("sp", a1fm, dr["a_lower_bounds"][1].rearrange("(h p) -> p h", p=128), allow_slow_non_contiguous=True)
                TT(c1fm, c1fm, a1fm, ALU.subtract)
                ACT(c1fm, c1fm, AF.Sigmoid)
            Mf = S.alloc([128, 132], F32)
            S.dma("sp", Mf, dr["c_M"])
            hmf = S.alloc([128, 128], F32)
            S.dma("sp", hmf, dr["c_hmask"])
            Sst = S.alloc([128, 6, 128], F32)
            MEMSET(Sst, 0.0)
            sneg = S.alloc([128, 768], F32)
            wtmp = S.alloc([128, 768], F32)
            einv = wtmp
            logf2 = [S.alloc([128, 768], F32) for _ in range(2)]
            ktok2 = [S.alloc([128, 768], BF16) for _ in range(2)]
            vtk2 = [S.alloc([128, 768], BF16) for _ in range(2)]
            E = [S.alloc([128, 132], F32) for _ in range(2)]
            qt = [S.alloc([128, 128], BF16) for _ in range(2)]
            kt = [S.alloc([128, 128], BF16) for _ in range(2)]
            sc = [S.alloc([128, 128], BF16) for _ in range(2)]
            SpA = [S.alloc([128, 128], BF16) for _ in range(2)]
            SpB = [S.alloc([128, 128], BF16) for _ in range(2)]
            SA = S.alloc([128, 128], F32)
            ptmp = S.alloc([128, 128], F32)
            sAB = S.alloc([128, 2], F32)
            osq = [S.alloc([128, 128], BF16) for _ in range(2)]
            rs = [S.alloc([128, 128], F32) for _ in range(2)]
            o1 = S.alloc([128, 128], F32)
            nn = [0]

            def TMs(i, part):
                ts_ = slice(i * 128, (i + 1) * 128)
                logf, ktok, vtk = logf2[i % 2], ktok2[i % 2], vtk2[i % 2]
                pf = [P(0, [128, 512]), P(1, [128, 512]), P(2, [128, 512])]
                pdk = [P(3, [128, 512]), P(4, [128, 256])]
                if part == 0:
                    for ch in range(3):
                        for kk in range(8):
                            MM(pf[ch], xT[:, kk, ts_], Wfi[:, kk, ch * 512:(ch + 1) * 512], kk == 0, kk == 7)
                elif part == 1:
                    ACT(sneg[:, 0:512], pf[0], AF.Exp)
                    ACT(sneg[:, 512:768], pf[1][:, 0:256], AF.Exp)
                    ACT(sneg, sneg, AF.Ln, bias=1.0)
                    ACT(sneg, sneg, AF.Exp, scale=-1.0)
                    CP(vtk[:, 0:256], pf[1][:, 256:512])
                    CP(vtk[:, 256:768], pf[2])
                elif part == 2:
                    TT(wtmp, sneg, c1bc, ALU.mult)
                    ACT(logf, wtmp, AF.Ln, scale=-1.0, bias=1.0)
                elif part == 3:
                    MM(pdk[0][:, 0:256], Mf[:, 0:128], logf[:, 0:256])
                    MM(pdk[0][:, 256:512], Mf[:, 0:128], logf[:, 256:512])
                    MM(pdk[1], Mf[:, 0:128], logf[:, 512:768])
                else:
                    ACT(einv[:, 0:512], pdk[0], AF.Exp, scale=-1.0)
                    ACT(einv[:, 512:768], pdk[1], AF.Exp, scale=-1.0)
                    TT(ktok, sneg, einv, ALU.mult)

            def HDs(i, h):
                ts_ = slice(i * 128, (i + 1) * 128)
                logf, ktok, vtk = logf2[i % 2], ktok2[i % 2], vtk2[i % 2]
                if True:
                    r = nn[0] % 2
                    nn[0] += 1
                    hs = slice(h * 128, (h + 1) * 128)
                    PD = P(5, [128, 132])
                    MM(PD, logf[:, hs], Mf)
                    ACT(E[r], PD, AF.Exp)
                    STT(qt[r], qT[:, h, ts_], 128.0 ** -0.5, E[r][:, 0:128], ALU.mult, ALU.mult)
                    PK = P(6, [128, 128], BF16)
                    TR(PK, ktok[:, hs], identb)
                    ACT(kt[r], PK, AF.Identity, scale=c1fm[:, h:h + 1])
                    PS_ = P(6, [128, 128], F32, off=512)
                    MM(PS_, kt[r], qt[r])
                    TT(sc[r], PS_, hmf, ALU.mult)
                    TS(sAB[:, 0:1], E[r][:, 63:64], c1fm[:, h:h + 1], None, ALU.mult)
                    TS(sAB[:, 1:2], E[r][:, 127:128], c1fm[:, h:h + 1], None, ALU.mult)
                    TS(SpA[r], Sst[:, h, :], E[r][:, 128:129], None, ALU.mult)
                    PA = P(7, [128, 128], F32)
                    MM(PA, ktok[0:64, hs], vtk[0:64, hs])
                    TS(ptmp, PA, sAB[:, 0:1], None, ALU.mult)
                    STT(SA, Sst[:, h, :], E[r][:, 129:130], ptmp, ALU.mult, ALU.add)
                    TS(SpB[r], SA, E[r][:, 130:131], None, ALU.mult)
                    PB = P(7, [128, 128], F32, off=512)
                    MM(PB, ktok[64:128, hs], vtk[64:128, hs])
                    TS(ptmp, PB, sAB[:, 1:2], None, ALU.mult)
                    STT(Sst[:, h, :], SA, E[r][:, 131:132], ptmp, ALU.mult, ALU.add)
                    PO = P(6, [128, 128], F32, off=1024)
                    MMs(PO, vtk[:, hs], sc[r], True, True)
                    MMs(PO[:, 0:64], SpA[r], qt[r][:, 0:64], False, True)
                    MMs(PO[:, 64:128], SpB[r], qt[r][:, 64:128], False, True)
                    ACT(osq[r], PO, AF.Square)
                    PM = P(7, [128, 128], F32, off=1024)
                    MM(PM, ones128, osq[r])
                    ACT(rs[r], PM, AF.Ln, bias=1e-6)
                    ACT(rs[r], rs[r], AF.Exp, scale=-0.5)
                    STT(o1, PO, gnfm[:, h:h + 1], rs[r], ALU.mult, ALU.mult)
                    TT(mixedT[:, h, ts_], o1, gT[:, h, ts_], ALU.mult)

            for part in range(5):
                TMs(0, part)
            for i in range(16):
                for h in range(6):
                    if i + 1 < 16 and h < 5:
                        TMs(i + 1, h)
                    HDs(i, h)
            if mode == "Amix":
                k.dbg_mixedT = mixedT
                return
            wo_stage(l, mixedT, ybuf)
            S.release(m)

        def dump_fm(src):
            m = S.mark()
            ot = [S.alloc([128, 1024], F32) for _ in range(2)]
            for tt in range(16):
                o = ot[tt % 2]
                pp = P(tt % 2, [128, 8, 128], BF16)
                for kk in range(8):
                    TR(pp[:, kk, :], src[:, kk, tt * 128:(tt + 1) * 128], identb)
                CP(o, pp.rearrange("p a b -> p (a b)"))
                k.outd.append(S.dma("sp", y_d[tt * 128:(tt + 1) * 128, :], o))
            S.release(m)

        k.outd = []
        load_xT()
        load_memT()
        k.ybuf_off = S.mark()
        ybuf = S.alloc([128, 8, T], F32)
        if mode == "moe0":
            init_ybuf(ybuf)
            moe_stage(0, ybuf)
            ln_stage(ybuf, 2, 0, final=True)
        elif mode == "ln0":
            init_ybuf(ybuf)
            ln_stage(ybuf, 2, 0, final=True)
        elif mode == "Bmix":
            kT = S.alloc([128, 6, T], BF16)
            vtok = S.alloc([128, 16, 768], BF16)
            kv_stage(kT, vtok)
            b_mix(2, kT, vtok, ybuf)
            dump_fm(k.dbg_mixedT)
        elif mode == "Amix":
            a_mix(0, ybuf)
            dump_fm(k.dbg_mixedT)
        else:
            kT = vtok = None
            nl = 4 if mode == "full" else int(mode[1:])
            for l in range(nl):
                if l < 2:
                    a_mix(l, ybuf)
                else:
                    if l == 2:
                        kT = S.alloc([128, 6, T], BF16)
                        vtok = S.alloc([128, 16, 768], BF16)
                        kv_stage(kT, vtok)
                    b_mix(l, kT, vtok, ybuf)
                ln_stage(ybuf, 0, l)
                init_ybuf(ybuf)
                moe_stage(l, ybuf)
                ln_stage(ybuf, 2, l, final=(l == nl - 1))
        S.final_wait("sp", k.outd)
        S.emit(sems)
        k.nops = {e: len(S.ops[e]) for e in S.ENG}
    return nc, cs, k


_CACHE = {}


def kernel(**inputs):
    if "nc" not in _CACHE:
        _CACHE["nc"] = build("full")
    nc, cs, _k = _CACHE["nc"]
    x = np.ascontiguousarray(inputs["x"], dtype=np.float32)
    mem = np.ascontiguousarray(inputs["mem"], dtype=np.float32)
    in_maps = []
    for b in range(8):
        m = {"x": x[b], "mem": mem[b]}
        for nm, _shp in WSPEC:
            m[nm] = np.ascontiguousarray(inputs[nm], dtype=np.float32)
        m.update(cs)
        in_maps.append(m)
    res = run_bass_kernel_spmd(nc, in_maps, core_ids=list(range(8)))
    return np.stack([np.asarray(r["y"], dtype=np.float32) for r in res.results], axis=0)
```

```python
import numpy as np
import concourse.bass as bass
import concourse.mybir as mybir

F32 = mybir.dt.float32
BF16 = mybir.dt.bfloat16
U8 = mybir.dt.uint8
I32 = mybir.dt.int32
ALU = mybir.AluOpType
AF = mybir.ActivationFunctionType
AX = mybir.AxisListType

_ESZ = {F32: 4, BF16: 2, U8: 1, I32: 4}

SB_BYTES = 207 * 1024
N_DSEM = 32
BUCKET = 2048


def esz(dt):
    return _ESZ[dt]


class Sched:
    ENG = ("pe", "act", "dve", "pool", "sp")

    def __init__(self, nc, big, pbig):
        self.nc = nc
        self.big = big
        self.pbig = pbig
        self.ops = {e: [] for e in self.ENG}
        self.keys = list(self.ENG[:4]) + ["d%d" % i for i in range(N_DSEM)]
        self.kidx = {k: i for i, k in enumerate(self.keys)}
        self.nk = len(self.keys)
        self.know = {e: [0] * self.nk for e in self.ENG}
        self.clock = {}
        self.seq = {k: 0 for k in self.keys}
        self.hist = {"sb": {}, "ps": {}, "dr": {}}
        self.ndma = 0
        self.ndma_sw = 0
        self.sb_top = 0
        self.marked = set()
        self.out_deps = []

    def alloc(self, shape, dt, name=None):
        n = int(np.prod(shape[1:])) * esz(dt)
        n = (n + 63) // 64 * 64
        off = self.sb_top
        self.sb_top += n
        assert self.sb_top <= SB_BYTES, ("SBUF overflow", name, self.sb_top)
        return self.view(off, shape, dt)

    def view(self, off, shape, dt):
        nb = int(np.prod(shape[1:])) * esz(dt)
        v = self.big[0:shape[0], off:off + nb]
        if dt != U8:
            v = v.bitcast(dt)
        if len(shape) == 3:
            v = v.rearrange("p (a b) -> p a b", a=shape[1])
        elif len(shape) == 4:
            v = v.rearrange("p (a b c) -> p a b c", a=shape[1], b=shape[2])
        return v

    def mark(self):
        return self.sb_top

    def release(self, m):
        self.sb_top = m

    def psum(self, bank, shape, dt=F32, off=0):
        nb = int(np.prod(shape[1:])) * esz(dt)
        assert off + nb <= 2048 * 8
        e0 = (bank * 2048 + off) // 4
        v = self.pbig[0:shape[0], e0:e0 + (nb + 3) // 4]
        if dt != F32:
            v = v.bitcast(dt)
        if len(shape) == 3:
            v = v.rearrange("p (a b) -> p a b", a=shape[1])
        return v

    def rect(self, ap):
        t = ap.tensor
        nm = t.name
        e = esz(ap.dtype)
        a = ap.ap
        off = int(ap.offset)
        if nm == "big":
            sp = "sb"
        elif nm == "pbig":
            sp = "ps"
        else:
            return ("dr", nm, 0, 1, 0, 1)
        pstep, pn = a[0]
        if pstep == 0:
            pstep = 1 << 40
        p0 = off // pstep if pstep < (1 << 40) else 0
        fo = off - p0 * pstep if pstep < (1 << 40) else off
        span = 1
        for st, cn in a[1:]:
            span += abs(st) * (cn - 1)
        b0 = fo * e
        b1 = (fo + span) * e
        p1 = p0 + pn
        if sp == "ps":
            b0 = (b0 // 2048) * 2048
            b1 = ((b1 + 2047) // 2048) * 2048
            p0 = (p0 // 32) * 32
            p1 = ((p1 + 31) // 32) * 32
        return (sp, None, p0, p1, b0, b1)

    def _deps_for(self, accesses, own=None):
        deps = {}
        regs = []
        for r, kind in accesses:
            sp, nm, p0, p1, b0, b1 = r
            h = self.hist[sp]
            if sp == "dr":
                bks = [nm]
            else:
                bks = range(b0 // BUCKET, (b1 - 1) // BUCKET + 1)
            for bk in bks:
                d = h.get(bk)
                if d is None:
                    d = {}
                    h[bk] = d
                dead = []
                for (k2, kind2, q0, q1, c0, c1), s2 in d.items():
                    if q0 < p1 and p0 < q1 and c0 < b1 and b0 < c1:
                        rr_ps = (sp == "ps" and kind == "r" and kind2 == "r" and k2 != own)
                        if kind == "w" or kind2 == "w" or rr_ps:
                            skip = False
                            if k2 == own:
                                if own == "pe" or not (kind == "r" and kind2 == "w"):
                                    skip = True
                            if not skip and deps.get(k2, 0) < s2:
                                deps[k2] = s2
                        if kind == "w" and p0 <= q0 and q1 <= p1 and b0 <= c0 and c1 <= b1:
                            dead.append((k2, kind2, q0, q1, c0, c1))
                for kk in dead:
                    del d[kk]
                regs.append((d, kind, p0, p1, b0, b1))
        return deps, regs

    def _register(self, regs, key, seq):
        for d, kind, p0, p1, b0, b1 in regs:
            d[(key, kind, p0, p1, b0, b1)] = seq

    def op(self, eng, fn, reads=(), writes=()):
        acc = [(self.rect(a), "r") for a in reads] + [(self.rect(a), "w") for a in writes]
        key = eng
        deps, regs = self._deps_for(acc, own=key)
        self.seq[key] += 1
        seq = self.seq[key]
        waits = self._resolve(eng, deps, own=key)
        clk = list(self.know[eng])
        clk[self.kidx[key]] = seq
        self.clock[(key, seq)] = clk
        self._register(regs, key, seq)
        self.ops[eng].append({"fn": fn, "waits": waits, "inc": (key, seq)})

    def _resolve(self, eng, deps, own=None):
        know = list(self.know[eng])
        waits = []
        items = sorted(deps.items(), key=lambda kv: -kv[1])
        for k, s in items:
            if k == own:
                pass
            i = self.kidx[k]
            if know[i] >= s:
                continue
            waits.append((k, s))
            self.marked.add((k, s))
            c = self.clock[(k, s)]
            know = [a if a >= b else b for a, b in zip(know, c)]
        self.know[eng] = know
        return waits

    def dma(self, eng, out, in_, reads=None, writes=None, **kw):
        rd = [in_] if reads is None else reads
        wr = [out] if writes is None else writes
        acc = [(self.rect(a), "r") for a in rd] + [(self.rect(a), "w") for a in wr]
        deps, regs = self._deps_for(acc)
        half = N_DSEM // 2
        if eng == "pool":
            k = "d%d" % (half + self.ndma_sw % half)
            self.ndma_sw += 1
        else:
            k = "d%d" % (self.ndma % half)
            self.ndma += 1
        prev = self.seq[k]
        if prev > 0:
            deps[k] = max(deps.get(k, 0), prev)
        self.seq[k] += 1
        seq = self.seq[k]
        waits = self._resolve(eng, deps)
        clk = list(self.know[eng])
        clk[self.kidx[k]] = seq
        self.clock[(k, seq)] = clk
        self._register(regs, k, seq)
        self.ops[eng].append({"dma": (out, in_), "kw": kw, "waits": waits, "inc": (k, seq)})
        return (k, seq)

    def final_wait(self, eng, deps):
        waits = self._resolve(eng, dict(deps))
        self.ops[eng].append({"waits": waits})

    def emit(self, sems):
        rank = {}
        for k in self.ENG[:4]:
            ms = sorted(s for (kk, s) in self.marked if kk == k)
            rank[k] = {s: i + 1 for i, s in enumerate(ms)}

        def val(k, s):
            if k[0] == "d" and k[1:].isdigit():
                return 16 * s
            return rank[k][s]

        nc = self.nc
        engobj = {"pe": "tensor", "act": "scalar", "dve": "vector", "pool": "gpsimd", "sp": "sync"}
        with nc.Block() as block:
            for e in self.ENG:
                ops = self.ops[e]

                def body(engine, ops=ops, e=e):
                    for o in ops:
                        for (k, s) in o["waits"]:
                            engine.wait_ge(sems[k], val(k, s))
                        if "fn" in o:
                            ins = o["fn"](engine)
                            k, s = o["inc"]
                            if (k, s) in self.marked:
                                ins.then_inc(sems[k], 1)
                        elif "dma" in o:
                            out, in_ = o["dma"]
                            k, s = o["inc"]
                            engine.dma_start(out=out, in_=in_, **o["kw"]).then_inc(sems[k], 16)

                getattr(block, engobj[e])(body)
from contextlib import ExitStack
from concourse.bass_utils import run_bass_kernel_spmd

T = 2048
D = 1024
ALPHA = 8 ** 0.25
WSPEC = [("a_w_in", [2, 1024, 3328]), ("a_lower_bounds", [2, 768]), ("a_gnorm", [2, 768]),
         ("b_w_in", [2, 1024, 1024]), ("w_kv_shared", [1024, 1536]), ("w_mem_kv", [4, 1024, 512]),
         ("w_o", [4, 1024, 1024]), ("ln_mix_g", [4, 1024]), ("ln_mix_b", [4, 1024]),
         ("ln_ffn_g", [4, 1024]), ("ln_ffn_b", [4, 1024]), ("w_group", [4, 1024, 4]),
         ("b_group", [4, 4]), ("w_router", [4, 1024, 16]), ("b_router", [4, 16]),
         ("w_gate", [4, 16, 1024, 256]), ("w_up", [4, 16, 1024, 256]), ("w_down", [4, 16, 256, 1024])]


def make_consts():
    c = {}
    c["c_ident"] = np.eye(128, dtype=np.float32)
    sel = np.zeros((16, 16, 128), np.float32)
    for e in range(16):
        sel[e, e, :] = 1.0
    c["c_sel"] = sel
    s = np.arange(128)
    c["c_hmask"] = ((s[:, None] // 64 == s[None, :] // 64) & (s[None, :] >= s[:, None])).astype(np.float32)
    M = np.zeros((128, 132), np.float32)
    for t in range(128):
        ch = t // 64
        ref = ch * 64 + 31
        for sp in range(ch * 64, ch * 64 + 64):
            M[sp, t] = float(sp <= t) - float(sp <= ref)
    for ch in range(2):
        for sp in range(ch * 64, ch * 64 + 64):
            M[sp, 128 + 2 * ch] = float(sp <= ch * 64 + 31)
            M[sp, 129 + 2 * ch] = 1.0
    c["c_M"] = M
    c["c_U"] = (s[:, None] > s[None, :]).astype(np.float32)
    c["c_L"] = (s[:, None] <= s[None, :]).astype(np.float32)
    c["c_dmask"] = (s[:, None] < s[None, :]).astype(np.float32)
    oz = np.zeros((128, 2, 128), np.float32)
    oz[:, 0, 0:64] = 1.0
    oz[:, 1, 64:128] = 1.0
    c["c_onesz"] = oz
    return c


class K:
    pass


def build(mode="full"):
    nc = bass.Bass("TRN2", target_bir_lowering=False)
    k = K()
    dr = {}
    dr["x"] = nc.dram_tensor("x", [T, D], F32, kind="ExternalInput").ap()
    dr["mem"] = nc.dram_tensor("mem", [256, D], F32, kind="ExternalInput").ap()
    for nm, shp in WSPEC:
        dr[nm] = nc.dram_tensor(nm, shp, F32, kind="ExternalInput").ap()
    cs = make_consts()
    for nm, arr in cs.items():
        dr[nm] = nc.dram_tensor(nm, list(arr.shape), F32, kind="ExternalInput").ap()
    y_d = nc.dram_tensor("y", [T, D], F32, kind="ExternalOutput").ap()
    with ExitStack() as es:
        big = es.enter_context(nc.sbuf_tensor("big", [128, SB_BYTES], U8))
        pbig = es.enter_context(nc.psum_tensor("pbig", [128, 4096], F32))
        S = Sched(nc, big, pbig)
        sems = {kk: es.enter_context(nc.semaphore("q%d" % i)) for i, kk in enumerate(S.keys)}
        P = S.psum

        def aps(*xs):
            return [a for a in xs if a is not None and not isinstance(a, (int, float))]

        def MM(out, lhsT, rhs, start=True, stop=True):
            S.op("pe", lambda e: e.matmul(out, lhsT, rhs, start=start, stop=stop), reads=[lhsT, rhs], writes=[out])

        def TR(out, in_, ident):
            S.op("pe", lambda e: e.transpose(out, in_, ident), reads=[in_, ident], writes=[out])

        def ACT(out, in_, func, scale=None, bias=None, accum=None):
            kw = {}
            if scale is not None:
                kw["scale"] = scale
            if bias is not None:
                kw["bias"] = bias
            if accum is not None:
                kw["accum_out"] = accum
            S.op("act", lambda e: e.activation(out, in_, func, **kw), reads=aps(in_, scale, bias), writes=aps(out, accum))

        def TT(out, a, b, op, eng="dve"):
            S.op(eng, lambda e: e.tensor_tensor(out, a, b, op), reads=[a, b], writes=[out])

        def TS(out, a, s1, s2, op0, op1=None, eng="dve"):
            if op1 is None:
                S.op(eng, lambda e: e.tensor_scalar(out, a, s1, None, op0), reads=aps(a, s1), writes=[out])
            else:
                S.op(eng, lambda e: e.tensor_scalar(out, a, s1, s2, op0, op1), reads=aps(a, s1, s2), writes=[out])

        def STT(out, in0, sc, in1, op0, op1, eng="dve"):
            S.op(eng, lambda e: e.scalar_tensor_tensor(out, in0, sc, in1, op0, op1), reads=aps(in0, sc, in1), writes=[out])

        def CP(out, in_, eng="dve"):
            if eng == "act":
                S.op("act", lambda e: e.copy(out, in_), reads=[in_], writes=[out])
            else:
                S.op(eng, lambda e: e.tensor_copy(out, in_), reads=[in_], writes=[out])

        def RED(out, in_, op):
            S.op("dve", lambda e: e.tensor_reduce(out, in_, AX.X, op), reads=[in_], writes=[out])

        def RECIP(out, in_):
            S.op("dve", lambda e: e.reciprocal(out, in_), reads=[in_], writes=[out])

        def MEMSET(out, v, eng="dve"):
            S.op(eng, lambda e: e.memset(out, v), writes=[out])

        def WLOAD(dst, src):
            S.dma("pool", dst, src.rearrange("(k p) n -> p k n", p=128))

        xT = S.alloc([128, 8, T], BF16)
        identf = S.alloc([128, 128], F32)
        identb = S.alloc([128, 128], BF16)
        onesD = S.alloc([128, 128], BF16)
        selb = S.alloc([16, 16, 128], BF16)
        lnp = S.alloc([128, 4, 4, 8], F32)
        S.dma("sp", identf, dr["c_ident"])
        S.dma("pool", identb, dr["c_ident"])
        S.dma("pool", selb, dr["c_sel"])
        MEMSET(onesD, 1.0 / 1024.0)
        for wi, nm in enumerate(["ln_mix_g", "ln_mix_b", "ln_ffn_g", "ln_ffn_b"]):
            for l in range(4):
                S.dma("sp", lnp[:, wi, l, :], dr[nm][l].rearrange("(k p) -> p k", p=128), allow_slow_non_contiguous=True)

        def load_xT():
            m = S.mark()
            xt = [S.alloc([128, 1024], BF16) for _ in range(2)]
            for i in range(16):
                b = xt[i % 2]
                S.dma("pool", b, dr["x"][i * 128:(i + 1) * 128, :])
                pt = P(i % 2, [128, 8, 128], BF16)
                for kk in range(8):
                    TR(pt[:, kk, :], b[:, kk * 128:(kk + 1) * 128], identb)
                CP(xT[:, :, i * 128:(i + 1) * 128], pt, eng="act" if i % 2 else "dve")
            S.release(m)

        def ln_stage(ybuf, wi, l, final=False):
            m = S.mark()
            ysq = S.alloc([128, 8, 512], BF16)
            yb = S.alloc([128, 8, 512], BF16)
            mean_sb = S.alloc([128, 512], F32)
            m2 = S.alloc([128, 512], F32)
            rstd = S.alloc([128, 512], F32)
            otile = [S.alloc([128, 1024], F32) for _ in range(2)] if final else None
            for c in range(4):
                tc = slice(c * 512, (c + 1) * 512)
                for kk in range(8):
                    ACT(ysq[:, kk, :], ybuf[:, kk, tc], AF.Square)
                    CP(yb[:, kk, :], ybuf[:, kk, tc], eng="dve")
                mps = P(0, [128, 512])
                sps = P(1, [128, 512])
                for kk in range(8):
                    MM(mps, onesD, yb[:, kk, :], kk == 0, kk == 7)
                for kk in range(8):
                    MM(sps, onesD, ysq[:, kk, :], kk == 0, kk == 7)
                CP(mean_sb, mps, eng="act")
                ACT(m2, mps, AF.Square)
                TT(m2, sps, m2, ALU.subtract)
                ACT(m2, m2, AF.Ln, bias=1e-5)
                ACT(rstd, m2, AF.Exp, scale=-0.5)
                for kk in range(8):
                    yk = ybuf[:, kk, tc]
                    TT(yk, yk, mean_sb, ALU.subtract)
                    TT(yk, yk, rstd, ALU.mult)
                    if final:
                        ACT(yk, yk, AF.Identity, scale=lnp[:, wi, l, kk:kk + 1], bias=lnp[:, wi + 1, l, kk:kk + 1])
                    else:
                        ACT(xT[:, kk, tc], yk, AF.Identity, scale=lnp[:, wi, l, kk:kk + 1], bias=lnp[:, wi + 1, l, kk:kk + 1])
                if final:
                    for j in range(4):
                        tt = c * 4 + j
                        ot = otile[tt % 2]
                        for half in range(2):
                            pp = P(2 + half, [128, 4, 128])
                            for q in range(4):
                                kk = half * 4 + q
                                TR(pp[:, q, :], ybuf[:, kk, tt * 128:(tt + 1) * 128], identf)
                            CP(ot[:, half * 512:(half + 1) * 512], pp.rearrange("p a b -> p (a b)"), eng="act" if half else "dve")
                        k.outd.append(S.dma("sp", y_d[tt * 128:(tt + 1) * 128, :], ot))
            S.release(m)

        def moe_stage(l, ybuf):
            m = S.mark()
            wr = S.alloc([128, 8, 20], BF16)
            S.dma("pool", wr[:, :, 0:4], dr["w_group"][l].rearrange("(k p) n -> p k n", p=128))
            S.dma("pool", wr[:, :, 4:20], dr["w_router"][l].rearrange("(k p) n -> p k n", p=128))
            bias = S.alloc([128, 20], F32)
            S.dma("sp", bias[:, 0:4], dr["b_group"][l:l + 1, :].broadcast_to([128, 4]))
            S.dma("sp", bias[:, 4:20], dr["b_router"][l:l + 1, :].broadcast_to([128, 16]))
            PR = P(7, [128, 16, 20])
            for i in range(16):
                for kk in range(8):
                    MM(PR[:, i, :], xT[:, kk, i * 128:(i + 1) * 128], wr[:, kk, :], kk == 0, kk == 7)
            L = S.alloc([128, 16, 20], F32)
            TT(L, PR, bias.unsqueeze(1).broadcast_to([128, 16, 20]), ALU.add)
            Lg = L[:, :, 0:4]
            Le = L[:, :, 4:20]
            gmax = S.alloc([128, 16], F32)
            RED(gmax, Lg, ALU.max)
            gb3 = gmax.unsqueeze(2).broadcast_to([128, 16, 4])
            dg = S.alloc([128, 16, 4], F32)
            TT(dg, Lg, gb3, ALU.subtract)
            ACT(dg, dg, AF.Exp)
            sg = S.alloc([128, 16], F32)
            RED(sg, dg, ALU.add)
            ptop = S.alloc([128, 16], F32)
            RECIP(ptop, sg)
            ohg = S.alloc([128, 16, 4], F32)
            TT(ohg, Lg, gb3, ALU.is_equal)
            TS(ohg, ohg, -1.0, 1.0e4, ALU.add, ALU.mult)
            Lm = S.alloc([128, 16, 16], F32)
            Lm4 = Lm.rearrange("p a (g e) -> p a g e", g=4)
            TT(Lm4, Le.rearrange("p a (g e) -> p a g e", g=4), ohg.unsqueeze(3).broadcast_to([128, 16, 4, 4]), ALU.add)
            m1 = S.alloc([128, 16], F32)
            RED(m1, Lm, ALU.max)
            oh1 = S.alloc([128, 16, 16], F32)
            TT(oh1, Lm, m1.unsqueeze(2).broadcast_to([128, 16, 16]), ALU.is_equal)
            Lm2 = S.alloc([128, 16, 16], F32)
            STT(Lm2, oh1, -1.0e4, Lm, ALU.mult, ALU.add)
            mm2 = S.alloc([128, 16], F32)
            RED(mm2, Lm2, ALU.max)
            oh2 = S.alloc([128, 16, 16], F32)
            TT(oh2, Lm2, mm2.unsqueeze(2).broadcast_to([128, 16, 16]), ALU.is_equal)
            ee = S.alloc([128, 16], F32)
            TT(ee, mm2, m1, ALU.subtract)
            ACT(ee, ee, AF.Exp)
            den = S.alloc([128, 16], F32)
            TS(den, ee, 1.0, None, ALU.add)
            RECIP(den, den)
            g1 = S.alloc([128, 16], F32)
            TT(g1, den, ptop, ALU.mult)
            g2 = S.alloc([128, 16], F32)
            TT(g2, g1, ee, ALU.mult)
            TT(oh1, oh1, g1.unsqueeze(2).broadcast_to([128, 16, 16]), ALU.mult)
            TT(oh2, oh2, g2.unsqueeze(2).broadcast_to([128, 16, 16]), ALU.mult)
            gb = S.alloc([128, 16, 16], BF16)
            TT(gb, oh1, oh2, ALU.add)
            if mode == "gates":
                k.dbg = (oh1, oh2)
            PT = P(5, [16, T], BF16)
            for i in range(16):
                TR(PT[:, i * 128:(i + 1) * 128], gb[:, i, :], identb)
            gatesT = S.alloc([16, T], BF16)
            CP(gatesT, PT)
            wg = [S.alloc([128, 8, 256], BF16) for _ in range(2)]
            wu = [S.alloc([128, 8, 256], BF16) for _ in range(2)]
            wd = [S.alloc([128, 2, 1024], BF16) for _ in range(2)]
            hT = S.alloc([128, 2, T], BF16)
            sgt = [S.alloc([128, 512], F32) for _ in range(2)]
            nblk = [0]
            ndn = [0]
            gpcur = [None]
            ngp = [0]

            def gateup_mm(e, c, ft):
                b = e % 2
                tc = slice(c * 512, (c + 1) * 512)
                bb = nblk[0] % 2
                nblk[0] += 1
                Pg = P(bb * 2, [128, 512])
                Pu = P(bb * 2 + 1, [128, 512])
                for kk in range(8):
                    MM(Pg, wg[b][:, kk, ft * 128:(ft + 1) * 128], xT[:, kk, tc], kk == 0, kk == 7)
                for kk in range(8):
                    MM(Pu, wu[b][:, kk, ft * 128:(ft + 1) * 128], xT[:, kk, tc], kk == 0, kk == 7)
                return (bb, Pg, Pu, ft, tc, gpcur[0])

            def gateup_ev(st):
                bb, Pg, Pu, ft, tc, Gp_ = st
                ACT(sgt[bb], Pg, AF.Silu)
                TT(sgt[bb], sgt[bb], Pu, ALU.mult)
                TT(hT[:, ft, tc], sgt[bb], Gp_, ALU.mult)

            def down(e, c, kks):
                b = e % 2
                tc = slice(c * 512, (c + 1) * 512)
                for kk in kks:
                    Pd = P(5 + ndn[0] % 2, [128, 512])
                    ndn[0] += 1
                    MM(Pd, wd[b][:, 0, kk * 128:(kk + 1) * 128], hT[:, 0, tc], True, False)
                    MM(Pd, wd[b][:, 1, kk * 128:(kk + 1) * 128], hT[:, 1, tc], False, True)
                    TT(ybuf[:, kk, tc], ybuf[:, kk, tc], Pd, ALU.add)

            prev = None
            for e in range(16):
                b = e % 2
                WLOAD(wg[b], dr["w_gate"][l, e])
                WLOAD(wu[b], dr["w_up"][l, e])
                WLOAD(wd[b], dr["w_down"][l, e])
                for c in range(4):
                    tc = slice(c * 512, (c + 1) * 512)
                    Gp = P(4 if ngp[0] % 2 == 0 else 7, [128, 512])
                    ngp[0] += 1
                    gpcur[0] = Gp
                    MM(Gp, selb[0:16, e, :], gatesT[0:16, tc])
                    for ft in range(2):
                        st = gateup_mm(e, c, ft)
                        if prev is not None:
                            down(prev[0], prev[1], range(4 * ft, 4 * ft + 4))
                        gateup_ev(st)
                    prev = (e, c)
            down(prev[0], prev[1], range(0, 8))
            S.release(m)

        def init_ybuf(ybuf):
            for kk in range(8):
                TS(ybuf[:, kk, :], xT[:, kk, :], ALPHA, None, ALU.mult)


        ones1 = S.alloc([128, 128], BF16)
        MEMSET(ones1, 1.0)
        ones128 = S.alloc([128, 128], BF16)
        MEMSET(ones128, 1.0 / 128.0)
        Ub = S.alloc([128, 128], BF16)
        Lcb = S.alloc([128, 128], BF16)
        S.dma("pool", Ub, dr["c_U"])
        S.dma("pool", Lcb, dr["c_L"])
        memT = S.alloc([128, 8, 256], BF16)

        def MMs(out, lhsT, rhs, start, stop):
            S.op("pe", lambda e: e.matmul(out, lhsT, rhs, start=start, stop=stop, skip_group_check=True), reads=[lhsT, rhs], writes=[out])

        def load_memT():
            m = S.mark()
            mt_ = S.alloc([128, 2, 1024], BF16)
            S.dma("pool", mt_, dr["mem"].rearrange("(a p) d -> p a d", p=128))
            for a in range(2):
                pt = P(2 + a, [128, 8, 128], BF16)
                for kk in range(8):
                    TR(pt[:, kk, :], mt_[:, a, kk * 128:(kk + 1) * 128], identb)
                CP(memT[:, :, a * 128:(a + 1) * 128], pt)
            S.release(m)

        def proj_fm(wsrc, tiles, dst_fn, func=None):
            m = S.mark()
            wb = [S.alloc([128, 8, 128], BF16) for _ in range(3)]
            for n, c0 in enumerate(tiles):
                w = wb[n % 3]
                WLOAD(w, wsrc[:, c0:c0 + 128])
                for c in range(4):
                    tc = slice(c * 512, (c + 1) * 512)
                    pp = P((n * 4 + c) % 2, [128, 512])
                    for kk in range(8):
                        MM(pp, w[:, kk, :], xT[:, kk, tc], kk == 0, kk == 7)
                    if func is not None and func(n) is not None:
                        ACT(dst_fn(n, c), pp, func(n))
                    else:
                        CP(dst_fn(n, c), pp, eng="act" if (n * 4 + c) % 2 else "dve")
            S.release(m)

        def mem_attn(l, qmT, mixedT):
            m = S.mark()
            wm = S.alloc([128, 8, 512], BF16)
            WLOAD(wm, dr["w_mem_kv"][l])
            kmT = S.alloc([128, 2, 256], BF16)
            vm = S.alloc([128, 2, 256], BF16)
            for j in range(2):
                pp = P(2 + j, [128, 256])
                for kk in range(8):
                    MM(pp, wm[:, kk, j * 128:(j + 1) * 128], memT[:, kk, :], kk == 0, kk == 7)
                CP(kmT[:, j, :], pp)
            for mt in range(2):
                pp = P(4 + mt, [128, 256])
                for kk in range(8):
                    MM(pp, memT[:, kk, mt * 128:(mt + 1) * 128], wm[:, kk, 256:512], kk == 0, kk == 7)
                CP(vm[:, mt, :], pp, eng="act")
            em = [S.alloc([128, 512], BF16) for _ in range(4)]
            rec = S.alloc([128, 512], F32)
            n = 0
            for j in range(2):
                for half in range(2):
                    pb = 64 * half
                    for c in range(4):
                        tc = slice(c * 512, (c + 1) * 512)
                        es_ = []
                        for mt in range(2):
                            zp = P(2 + (n % 2), [128, 512])
                            MM(zp, kmT[pb:pb + 64, j, mt * 128:(mt + 1) * 128], qmT[pb:pb + 64, j, tc])
                            eb = em[n % 4]
                            ACT(eb, zp, AF.Exp, scale=0.125)
                            es_.append(eb)
                            n += 1
                        pn = P(4, [128, 512])
                        pd = P(5, [128, 512])
                        for mt in range(2):
                            MM(pn, vm[:, mt, j * 128:(j + 1) * 128], es_[mt], mt == 0, mt == 1)
                        for mt in range(2):
                            MM(pd, ones1, es_[mt], mt == 0, mt == 1)
                        RECIP(rec[pb:pb + 64, :], pd[pb:pb + 64, :])
                        TT(mixedT[pb:pb + 64, 6 + j, tc], pn[pb:pb + 64, :], rec[pb:pb + 64, :], ALU.mult)
            S.release(m)

        def wo_stage(l, mixedT, ybuf):
            m = S.mark()
            wob = [S.alloc([128, 8, 256], BF16) for _ in range(2)]
            n = 0
            for q in range(4):
                w = wob[q % 2]
                WLOAD(w, dr["w_o"][l][:, q * 256:(q + 1) * 256])
                for kq in range(2):
                    kk = 2 * q + kq
                    for c in range(4):
                        tc = slice(c * 512, (c + 1) * 512)
                        pp = P(n % 2, [128, 512])
                        n += 1
                        for f in range(8):
                            MM(pp, w[:, f, kq * 128:(kq + 1) * 128], mixedT[:, f, tc], f == 0, f == 7)
                        STT(ybuf[:, kk, tc], xT[:, kk, tc], ALPHA, pp, ALU.mult, ALU.add)
            S.release(m)

        def kv_stage(kT, vtok):
            proj_fm(dr["w_kv_shared"], [j * 128 for j in range(6)], lambda n, c: kT[:, n, c * 512:(c + 1) * 512])
            m = S.mark()
            wv = S.alloc([128, 8, 768], BF16)
            WLOAD(wv, dr["w_kv_shared"][:, 768:1536])
            for i in range(16):
                ts_ = slice(i * 128, (i + 1) * 128)
                pa = P(2 + 2 * (i % 2), [128, 512])
                pb_ = P(3 + 2 * (i % 2), [128, 256])
                for kk in range(8):
                    MM(pa, xT[:, kk, ts_], wv[:, kk, 0:512], kk == 0, kk == 7)
                for kk in range(8):
                    MM(pb_, xT[:, kk, ts_], wv[:, kk, 512:768], kk == 0, kk == 7)
                CP(vtok[:, i, 0:512], pa, eng="act")
                CP(vtok[:, i, 512:768], pb_, eng="dve")
            S.release(m)

        def sb_attn(qT, kT, vtok, mixedT):
            m = S.mark()
            dm = S.alloc([128, 4, 512], BF16)
            dmf = S.alloc([128, 128], F32)
            S.dma("sp", dmf, dr["c_dmask"])
            for q in range(4):
                if q > 0:
                    MEMSET(dm[:, q, 0:q * 128], 0.0)
                CP(dm[:, q, q * 128:(q + 1) * 128], dmf)
                if q < 3:
                    MEMSET(dm[:, q, (q + 1) * 128:512], 1.0)
            yo = [32768]

            def ya(shape, dt):
                nb = int(np.prod(shape[1:])) * esz(dt)
                v = yview(yo[0], shape, dt)
                yo[0] += nb
                assert yo[0] <= 65536
                return v
            REF, RLF, RLB, RAB = 5, 3, 4, 3
            ef = [ya([128, 512], F32) for _ in range(REF)]
            Lf = [ya([128, 512], F32) for _ in range(RLF)]
            Lb = [ya([128, 512], BF16) for _ in range(RLB)]
            ab = [ya([128, 512], BF16) for _ in range(RAB)]
            zbanks = [0, 1, 6, 7]
            items = []
            for j in range(6):
                for c in range(4):
                    top = 4 * c + 3
                    for I in range(top, -1, -1):
                        for half in range(2):
                            items.append((j, half, c, I, half, I == top, I == 0))
            N = len(items)

            def Zb(n):
                return P(zbanks[n % 4], [128, 512])

            def accb(n):
                return P(2 + items[n][4], [128, 512])

            def Ob(n):
                return P(4 + items[n][4], [128, 512])

            def s0(n):
                j, half, c, I, _, _, _ = items[n]
                pb = 64 * half
                MM(Zb(n), kT[pb:pb + 64, j, I * 128:(I + 1) * 128], qT[pb:pb + 64, j, c * 512:(c + 1) * 512])

            def s1(n):
                ACT(ef[n % REF], Zb(n), AF.Exp, scale=0.125)
                ACT(Lf[n % RLF], ef[n % REF], AF.Ln, bias=1.0)

            def s2(n):
                j, half, c, I, _, _, _ = items[n]
                if I >= 4 * c:
                    TT(Lb[n % RLB], Lf[n % RLF], dm[:, I - 4 * c, :], ALU.mult)
                else:
                    CP(Lb[n % RLB], Lf[n % RLF], eng="pool")
                STT(ef[n % REF], Zb(n), 0.125, Lf[n % RLF], ALU.mult, ALU.subtract)

            def s3a(n):
                MMs(accb(n), Ub, Lb[n % RLB], items[n][5], True)

            def s3b(n):
                TT(ef[n % REF], ef[n % REF], accb(n), ALU.subtract)

            def s3c(n):
                MMs(accb(n), Lcb, Lb[n % RLB], False, True)

            def s4(n):
                j, half, c, I, _, _, _ = items[n]
                ACT(ab[n % RAB], ef[n % REF], AF.Exp)
                if I >= 4 * c:
                    TT(ab[n % RAB], ab[n % RAB], dm[:, I - 4 * c, :], ALU.mult)

            def s5(n):
                j, half, c, I, _, first, last = items[n]
                MMs(Ob(n), vtok[:, I, j * 128:(j + 1) * 128], ab[n % RAB], first, last)
                if last:
                    pb = 64 * half
                    CP(mixedT[pb:pb + 64, j, c * 512:(c + 1) * 512], Ob(n)[pb:pb + 64, :], eng="act")

            def ok(n):
                return 0 <= n < N
            for t in range(N + 7):
                if ok(t - 5):
                    s4(t - 5)
                if ok(t):
                    s0(t)
                if ok(t - 1):
                    s1(t - 1)
                if ok(t - 2):
                    s2(t - 2)
                if ok(t - 5):
                    s3c(t - 5)
                if ok(t - 3):
                    s3a(t - 3)
                if ok(t - 4):
                    s3b(t - 4)
                if ok(t - 6):
                    s5(t - 6)
            S.release(m)

        def yview(off, shape, dt):
            return S.view(k.ybuf_off + off, shape, dt)

        def b_mix(l, kT, vtok, ybuf):
            m = S.mark()
            qT = yview(0, [128, 6, T], BF16)
            qmT = yview(6 * T * 2, [128, 2, T], BF16)
            proj_fm(dr["b_w_in"][l - 2], [j * 128 for j in range(8)],
                    lambda n, c: (qT[:, n, c * 512:(c + 1) * 512] if n < 6 else qmT[:, n - 6, c * 512:(c + 1) * 512]))
            mixedT = S.alloc([128, 8, T], BF16)
            mem_attn(l, qmT, mixedT)
            sb_attn(qT, kT, vtok, mixedT)
            if mode == "Bmix":
                k.dbg_mixedT = mixedT
                return
            wo_stage(l, mixedT, ybuf)
            S.release(m)

        def a_mix(l, ybuf):
            import os
            m = S.mark()
            qT = yview(0, [128, 6, T], BF16)
            gT = yview(6 * T * 2, [128, 6, T], BF16)
            qmT = yview(12 * T * 2, [128, 2, T], BF16)
            tiles = [j * 128 for j in range(6)] + [2304 + j * 128 for j in range(6)] + [3072, 3200]

            def dst(n, c):
                tc = slice(c * 512, (c + 1) * 512)
                if n < 6:
                    return qT[:, n, tc]
                if n < 12:
                    return gT[:, n - 6, tc]
                return qmT[:, n - 12, tc]
            proj_fm(dr["a_w_in"][l], tiles, dst, func=lambda n: AF.Silu if n < 12 else None)
            mixedT = S.alloc([128, 8, T], BF16)
            mem_attn(l, qmT, mixedT)
            Wfi = S.alloc([128, 8, 1536], BF16)
            if os.environ.get("NOWFI"):
                MEMSET(Wfi, 0.01)
            for ch_ in range(0 if os.environ.get("NOWFI") else 6):
                WLOAD(Wfi[:, :, ch_ * 256:(ch_ + 1) * 256], dr["a_w_in"][l][:, 768 + ch_ * 256:768 + (ch_ + 1) * 256])
            c1bc = S.alloc([128, 768], F32)
            c1fm = S.alloc([128, 6], F32)
            gnfm = S.alloc([128, 6], F32)
            S.dma("sp", gnfm, dr["a_gnorm"][l].rearrange("(h p) -> p h", p=128), allow_slow_non_contiguous=True)
            if l == 0:
                MEMSET(c1bc, 1.0)
                MEMSET(c1fm, 1.0)
            else:
                mk_ = S.mark()
                a1bc = S.alloc([128, 768], F32)
                S.release(mk_)
                S.dma("sp", c1bc, dr["a_lower_bounds"][0:1, :].broadcast_to([128, 768]))
                S.dma("sp", a1bc, dr["a_lower_bounds"][1:2, :].broadcast_to([128, 768]))
                TT(c1bc, c1bc, a1bc, ALU.subtract)
                ACT(c1bc, c1bc, AF.Sigmoid)
                a1fm = S.alloc([128, 6], F32)
                S.dma("sp", c1fm, dr["a_lower_bounds"][0].rearrange("(h p) -> p h", p=128), allow_slow_non_contiguous=True)
                S.dma("sp", a1fm, dr["a_lower_bounds"][1].rearrange("(h p) -> p h", p=128), allow_slow_non_contiguous=True)
                TT(c1fm, c1fm, a1fm, ALU.subtract)
                ACT(c1fm, c1fm, AF.Sigmoid)
            Mf = S.alloc([128, 132], F32)
            S.dma("sp", Mf, dr["c_M"])
            hmf = S.alloc([128, 128], F32)
            S.dma("sp", hmf, dr["c_hmask"])
            Sst = S.alloc([128, 6, 128], F32)
            MEMSET(Sst, 0.0)
            sneg = S.alloc([128, 768], F32)
            wtmp = S.alloc([128, 768], F32)
            einv = wtmp
            logf2 = [S.alloc([128, 768], F32) for _ in range(2)]
            ktok2 = [S.alloc([128, 768], BF16) for _ in range(2)]
            vtk2 = [S.alloc([128, 768], BF16) for _ in range(2)]
            E = [S.alloc([128, 132], F32) for _ in range(2)]
            qt = [S.alloc([128, 128], BF16) for _ in range(2)]
            kt = [S.alloc([128, 128], BF16) for _ in range(2)]
            sc = [S.alloc([128, 128], BF16) for _ in range(2)]
            SpA = [S.alloc([128, 128], BF16) for _ in range(2)]
            SpB = [S.alloc([128, 128], BF16) for _ in range(2)]
            SA2 = [S.alloc([128, 128], F32) for _ in range(2)]
            ptmp2 = [S.alloc([128, 128], F32) for _ in range(2)]
            sAB2 = [S.alloc([128, 2], F32) for _ in range(2)]
            osq = [S.alloc([128, 128], BF16) for _ in range(2)]
            rs = [S.alloc([128, 128], F32) for _ in range(2)]
            o12 = [S.alloc([128, 128], F32) for _ in range(2)]

            def TM(i, part):
                ts_ = slice(i * 128, (i + 1) * 128)
                logf, ktok, vtk = logf2[i % 2], ktok2[i % 2], vtk2[i % 2]
                pf = [P(0, [128, 512]), P(1, [128, 512]), P(2, [128, 512])]
                if part == 0:
                    for ch in range(3):
                        for kk in range(8):
                            MM(pf[ch], xT[:, kk, ts_], Wfi[:, kk, ch * 512:(ch + 1) * 512], kk == 0, kk == 7)
                elif part == 1:
                    ACT(sneg[:, 0:512], pf[0], AF.Exp)
                    ACT(sneg[:, 512:768], pf[1][:, 0:256], AF.Exp)
                    CP(vtk[:, 0:256], pf[1][:, 256:512])
                    CP(vtk[:, 256:768], pf[2])
                    ACT(sneg, sneg, AF.Ln, bias=1.0)
                    ACT(sneg, sneg, AF.Exp, scale=-1.0)
                elif part == 2:
                    TT(wtmp, sneg, c1bc, ALU.mult)
                    ACT(logf, wtmp, AF.Ln, scale=-1.0, bias=1.0)
                elif part == 3:
                    pdk = [P(0, [128, 512]), P(1, [128, 256])]
                    MM(pdk[0][:, 0:256], Mf[:, 0:128], logf[:, 0:256])
                    MM(pdk[0][:, 256:512], Mf[:, 0:128], logf[:, 256:512])
                    MM(pdk[1], Mf[:, 0:128], logf[:, 512:768])
                else:
                    pdk = [P(0, [128, 512]), P(1, [128, 256])]
                    ACT(einv[:, 0:512], pdk[0], AF.Exp, scale=-1.0)
                    ACT(einv[:, 512:768], pdk[1], AF.Exp, scale=-1.0)
                    TT(ktok, sneg, einv, ALU.mult)

            def hp(n):
                i, h = divmod(n, 6)
                r = n % 2
                X = 3 + 2 * r
                Y = 4 + 2 * r
                d = dict(i=i, h=h, r=r, ts=slice(i * 128, (i + 1) * 128), hs=slice(h * 128, (h + 1) * 128),
                         logf=logf2[i % 2], ktok=ktok2[i % 2], vtk=vtk2[i % 2],
                         PD=P(X, [128, 132]), PK=P(X, [128, 128], BF16, off=768),
                         PO=P(X, [128, 128], F32, off=1024), PM=P(X, [128, 128], F32, off=1536),
                         PS=P(Y, [128, 128], F32), PA=P(Y, [128, 128], F32, off=512), PB=P(Y, [128, 128], F32, off=1024))
                return d

            def A1(n):
                d = hp(n)
                MM(d["PA"], d["ktok"][0:64, d["hs"]], d["vtk"][0:64, d["hs"]])
                MM(d["PD"], d["logf"][:, d["hs"]], Mf)
                MM(d["PB"], d["ktok"][64:128, d["hs"]], d["vtk"][64:128, d["hs"]])
                TR(d["PK"], d["ktok"][:, d["hs"]], identb)

            def A2(n):
                d = hp(n)
                r, h = d["r"], d["h"]
                ACT(E[r], d["PD"], AF.Exp)
                ACT(kt[r], d["PK"], AF.Identity, scale=c1fm[:, h:h + 1])
                STT(qt[r], qT[:, h, d["ts"]], 128.0 ** -0.5, E[r][:, 0:128], ALU.mult, ALU.mult)
                TS(sAB2[r][:, 0:1], E[r][:, 63:64], c1fm[:, h:h + 1], None, ALU.mult)
                TS(sAB2[r][:, 1:2], E[r][:, 127:128], c1fm[:, h:h + 1], None, ALU.mult)
                MM(d["PS"], kt[r], qt[r])

            def B1(n):
                d = hp(n)
                r, h = d["r"], d["h"]
                TT(sc[r], d["PS"], hmf, ALU.mult)
                TS(SpA[r], Sst[:, h, :], E[r][:, 128:129], None, ALU.mult)
                TS(ptmp2[r], d["PA"], sAB2[r][:, 0:1], None, ALU.mult)
                STT(SA2[r], Sst[:, h, :], E[r][:, 129:130], ptmp2[r], ALU.mult, ALU.add)
                TS(SpB[r], SA2[r], E[r][:, 130:131], None, ALU.mult)
                TS(ptmp2[r], d["PB"], sAB2[r][:, 1:2], None, ALU.mult)
                STT(Sst[:, h, :], SA2[r], E[r][:, 131:132], ptmp2[r], ALU.mult, ALU.add)
                MMs(d["PO"], d["vtk"][:, d["hs"]], sc[r], True, True)
                MMs(d["PO"][:, 0:64], SpA[r], qt[r][:, 0:64], False, True)
                MMs(d["PO"][:, 64:128], SpB[r], qt[r][:, 64:128], False, True)
                ACT(osq[r], d["PO"], AF.Square)

            def B2(n):
                d = hp(n)
                r, h = d["r"], d["h"]
                MM(d["PM"], ones128, osq[r])
                ACT(rs[r], d["PM"], AF.Ln, bias=1e-6)
                ACT(rs[r], rs[r], AF.Exp, scale=-0.5)
                STT(o12[r], d["PO"], gnfm[:, h:h + 1], rs[r], ALU.mult, ALU.mult)
                TT(mixedT[:, h, d["ts"]], o12[r], gT[:, h, d["ts"]], ALU.mult)

            for part in range(5):
                TM(0, part)
            NH = 96
            A1(0)
            A2(0)
            for n in range(NH):
                i, h = divmod(n, 6)
                if n + 1 < NH and (n + 1) % 6 != 0:
                    A1(n + 1)
                if i + 1 < 16 and h < 5:
                    TM(i + 1, h)
                if n + 1 < NH and (n + 1) % 6 == 0:
                    A1(n + 1)
                B1(n)
                if n + 1 < NH:
                    A2(n + 1)
                B2(n)
            if mode == "Amix":
                k.dbg_mixedT = mixedT
                return
            wo_stage(l, mixedT, ybuf)
            S.release(m)

        def dump_fm(src):
            m = S.mark()
            ot = [S.alloc([128, 1024], F32) for _ in range(2)]
            for tt in range(16):
                o = ot[tt % 2]
                pp = P(tt % 2, [128, 8, 128], BF16)
                for kk in range(8):
                    TR(pp[:, kk, :], src[:, kk, tt * 128:(tt + 1) * 128], identb)
                CP(o, pp.rearrange("p a b -> p (a b)"))
                k.outd.append(S.dma("sp", y_d[tt * 128:(tt + 1) * 128, :], o))
            S.release(m)

        k.outd = []
        load_xT()
        load_memT()
        k.ybuf_off = S.mark()
        ybuf = S.alloc([128, 8, T], F32)
        if mode == "moe0":
            init_ybuf(ybuf)
            moe_stage(0, ybuf)
            ln_stage(ybuf, 2, 0, final=True)
        elif mode == "ln0":
            init_ybuf(ybuf)
            ln_stage(ybuf, 2, 0, final=True)
        elif mode == "Bmix":
            kT = S.alloc([128, 6, T], BF16)
            vtok = S.alloc([128, 16, 768], BF16)
            kv_stage(kT, vtok)
            b_mix(2, kT, vtok, ybuf)
            dump_fm(k.dbg_mixedT)
        elif mode == "Amix":
            a_mix(0, ybuf)
            dump_fm(k.dbg_mixedT)
        else:
            kT = vtok = None
            nl = 4 if mode == "full" else int(mode[1:])
            for l in range(nl):
                if l < 2:
                    a_mix(l, ybuf)
                else:
                    if l == 2:
                        kT = S.alloc([128, 6, T], BF16)
                        vtok = S.alloc([128, 16, 768], BF16)
                        kv_stage(kT, vtok)
                    b_mix(l, kT, vtok, ybuf)
                ln_stage(ybuf, 0, l)
                init_ybuf(ybuf)
                moe_stage(l, ybuf)
                ln_stage(ybuf, 2, l, final=(l == nl - 1))
        S.final_wait("sp", k.outd)
        S.emit(sems)
        k.nops = {e: len(S.ops[e]) for e in S.ENG}
    return nc, cs, k


_CACHE = {}


def kernel(**inputs):
    if "nc" not in _CACHE:
        _CACHE["nc"] = build("full")
    nc, cs, _k = _CACHE["nc"]
    x = np.ascontiguousarray(inputs["x"], dtype=np.float32)
    mem = np.ascontiguousarray(inputs["mem"], dtype=np.float32)
    in_maps = []
    for b in range(8):
        m = {"x": x[b], "mem": mem[b]}
        for nm, _shp in WSPEC:
            m[nm] = np.ascontiguousarray(inputs[nm], dtype=np.float32)
        m.update(cs)
        in_maps.append(m)
    res = run_bass_kernel_spmd(nc, in_maps, core_ids=list(range(8)))
    return np.stack([np.asarray(r["y"], dtype=np.float32) for r in res.results], axis=0)
```

```python
import numpy as np
import concourse.bass as bass
import concourse.mybir as mybir

F32 = mybir.dt.float32
BF16 = mybir.dt.bfloat16
U8 = mybir.dt.uint8
I32 = mybir.dt.int32
ALU = mybir.AluOpType
AF = mybir.ActivationFunctionType
AX = mybir.AxisListType

_ESZ = {F32: 4, BF16: 2, U8: 1, I32: 4}

SB_BYTES = 207 * 1024
N_DSEM = 32
BUCKET = 2048


def esz(dt):
    return _ESZ[dt]


class Sched:
    ENG = ("pe", "act", "dve", "pool", "sp")

    def __init__(self, nc, big, pbig):
        self.nc = nc
        self.big = big
        self.pbig = pbig
        self.ops = {e: [] for e in self.ENG}
        self.keys = list(self.ENG[:4]) + ["d%d" % i for i in range(N_DSEM)]
        self.kidx = {k: i for i, k in enumerate(self.keys)}
        self.nk = len(self.keys)
        self.know = {e: [0] * self.nk for e in self.ENG}
        self.clock = {}
        self.seq = {k: 0 for k in self.keys}
        self.hist = {"sb": {}, "ps": {}, "dr": {}}
        self.ndma = 0
        self.ndma_sw = 0
        self.sb_top = 0
        self.marked = set()
        self.out_deps = []

    def alloc(self, shape, dt, name=None):
        n = int(np.prod(shape[1:])) * esz(dt)
        n = (n + 63) // 64 * 64
        off = self.sb_top
        self.sb_top += n
        assert self.sb_top <= SB_BYTES, ("SBUF overflow", name, self.sb_top)
        return self.view(off, shape, dt)

    def view(self, off, shape, dt):
        nb = int(np.prod(shape[1:])) * esz(dt)
        v = self.big[0:shape[0], off:off + nb]
        if dt != U8:
            v = v.bitcast(dt)
        if len(shape) == 3:
            v = v.rearrange("p (a b) -> p a b", a=shape[1])
        elif len(shape) == 4:
            v = v.rearrange("p (a b c) -> p a b c", a=shape[1], b=shape[2])
        return v

    def mark(self):
        return self.sb_top

    def release(self, m):
        self.sb_top = m

    def psum(self, bank, shape, dt=F32, off=0):
        nb = int(np.prod(shape[1:])) * esz(dt)
        assert off + nb <= 2048 * 8
        e0 = (bank * 2048 + off) // 4
        v = self.pbig[0:shape[0], e0:e0 + (nb + 3) // 4]
        if dt != F32:
            v = v.bitcast(dt)
        if len(shape) == 3:
            v = v.rearrange("p (a b) -> p a b", a=shape[1])
        return v

    def rect(self, ap):
        t = ap.tensor
        nm = t.name
        e = esz(ap.dtype)
        a = ap.ap
        off = int(ap.offset)
        if nm == "big":
            sp = "sb"
        elif nm == "pbig":
            sp = "ps"
        else:
            return ("dr", nm, 0, 1, 0, 1)
        pstep, pn = a[0]
        if pstep == 0:
            pstep = 1 << 40
        p0 = off // pstep if pstep < (1 << 40) else 0
        fo = off - p0 * pstep if pstep < (1 << 40) else off
        span = 1
        for st, cn in a[1:]:
            span += abs(st) * (cn - 1)
        b0 = fo * e
        b1 = (fo + span) * e
        p1 = p0 + pn
        if sp == "ps":
            b0 = (b0 // 2048) * 2048
            b1 = ((b1 + 2047) // 2048) * 2048
            p0 = (p0 // 32) * 32
            p1 = ((p1 + 31) // 32) * 32
        return (sp, None, p0, p1, b0, b1)

    def _deps_for(self, accesses, own=None):
        deps = {}
        regs = []
        for r, kind in accesses:
            sp, nm, p0, p1, b0, b1 = r
            h = self.hist[sp]
            if sp == "dr":
                bks = [nm]
            else:
                bks = range(b0 // BUCKET, (b1 - 1) // BUCKET + 1)
            for bk in bks:
                d = h.get(bk)
                if d is None:
                    d = {}
                    h[bk] = d
                dead = []
                for (k2, kind2, q0, q1, c0, c1), s2 in d.items():
                    if q0 < p1 and p0 < q1 and c0 < b1 and b0 < c1:
                        rr_ps = (sp == "ps" and kind == "r" and kind2 == "r" and k2 != own)
                        if kind == "w" or kind2 == "w" or rr_ps:
                            skip = False
                            if k2 == own:
                                if own == "pe" or not (kind == "r" and kind2 == "w"):
                                    skip = True
                            if not skip and deps.get(k2, 0) < s2:
                                deps[k2] = s2
                        if kind == "w" and p0 <= q0 and q1 <= p1 and b0 <= c0 and c1 <= b1:
                            dead.append((k2, kind2, q0, q1, c0, c1))
                for kk in dead:
                    del d[kk]
                regs.append((d, kind, p0, p1, b0, b1))
        return deps, regs

    def _register(self, regs, key, seq):
        for d, kind, p0, p1, b0, b1 in regs:
            d[(key, kind, p0, p1, b0, b1)] = seq

    def op(self, eng, fn, reads=(), writes=()):
        acc = [(self.rect(a), "r") for a in reads] + [(self.rect(a), "w") for a in writes]
        key = eng
        deps, regs = self._deps_for(acc, own=key)
        self.seq[key] += 1
        seq = self.seq[key]
        waits = self._resolve(eng, deps, own=key)
        clk = list(self.know[eng])
        clk[self.kidx[key]] = seq
        self.clock[(key, seq)] = clk
        self._register(regs, key, seq)
        self.ops[eng].append({"fn": fn, "waits": waits, "inc": (key, seq)})

    def _resolve(self, eng, deps, own=None):
        know = list(self.know[eng])
        waits = []
        items = sorted(deps.items(), key=lambda kv: -kv[1])
        for k, s in items:
            if k == own:
                pass
            i = self.kidx[k]
            if know[i] >= s:
                continue
            waits.append((k, s))
            self.marked.add((k, s))
            c = self.clock[(k, s)]
            know = [a if a >= b else b for a, b in zip(know, c)]
        self.know[eng] = know
        return waits

    def dma(self, eng, out, in_, reads=None, writes=None, **kw):
        rd = [in_] if reads is None else reads
        wr = [out] if writes is None else writes
        acc = [(self.rect(a), "r") for a in rd] + [(self.rect(a), "w") for a in wr]
        deps, regs = self._deps_for(acc)
        half = N_DSEM // 2
        if eng == "pool":
            k = "d%d" % (half + self.ndma_sw % half)
            self.ndma_sw += 1
        else:
            k = "d%d" % (self.ndma % half)
            self.ndma += 1
        prev = self.seq[k]
        if prev > 0:
            deps[k] = max(deps.get(k, 0), prev)
        self.seq[k] += 1
        seq = self.seq[k]
        waits = self._resolve(eng, deps)
        clk = list(self.know[eng])
        clk[self.kidx[k]] = seq
        self.clock[(k, seq)] = clk
        self._register(regs, k, seq)
        self.ops[eng].append({"dma": (out, in_), "kw": kw, "waits": waits, "inc": (k, seq)})
        return (k, seq)

    def final_wait(self, eng, deps):
        waits = self._resolve(eng, dict(deps))
        self.ops[eng].append({"waits": waits})

    def emit(self, sems):
        rank = {}
        for k in self.ENG[:4]:
            ms = sorted(s for (kk, s) in self.marked if kk == k)
            rank[k] = {s: i + 1 for i, s in enumerate(ms)}

        def val(k, s):
            if k[0] == "d" and k[1:].isdigit():
                return 16 * s
            return rank[k][s]

        nc = self.nc
        engobj = {"pe": "tensor", "act": "scalar", "dve": "vector", "pool": "gpsimd", "sp": "sync"}
        with nc.Block() as block:
            for e in self.ENG:
                ops = self.ops[e]

                def body(engine, ops=ops, e=e):
                    for o in ops:
                        for (k, s) in o["waits"]:
                            engine.wait_ge(sems[k], val(k, s))
                        if "fn" in o:
                            ins = o["fn"](engine)
                            k, s = o["inc"]
                            if (k, s) in self.marked:
                                ins.then_inc(sems[k], 1)
                        elif "dma" in o:
                            out, in_ = o["dma"]
                            k, s = o["inc"]
                            engine.dma_start(out=out, in_=in_, **o["kw"]).then_inc(sems[k], 16)

                getattr(block, engobj[e])(body)
from contextlib import ExitStack
from concourse.bass_utils import run_bass_kernel_spmd

T = 2048
D = 1024
ALPHA = 8 ** 0.25
WSPEC = [("a_w_in", [2, 1024, 3328]), ("a_lower_bounds", [2, 768]), ("a_gnorm", [2, 768]),
         ("b_w_in", [2, 1024, 1024]), ("w_kv_shared", [1024, 1536]), ("w_mem_kv", [4, 1024, 512]),
         ("w_o", [4, 1024, 1024]), ("ln_mix_g", [4, 1024]), ("ln_mix_b", [4, 1024]),
         ("ln_ffn_g", [4, 1024]), ("ln_ffn_b", [4, 1024]), ("w_group", [4, 1024, 4]),
         ("b_group", [4, 4]), ("w_router", [4, 1024, 16]), ("b_router", [4, 16]),
         ("w_gate", [4, 16, 1024, 256]), ("w_up", [4, 16, 1024, 256]), ("w_down", [4, 16, 256, 1024])]


def make_consts():
    c = {}
    c["c_ident"] = np.eye(128, dtype=np.float32)
    sel = np.zeros((16, 16, 128), np.float32)
    for e in range(16):
        sel[e, e, :] = 1.0
    c["c_sel"] = sel
    s = np.arange(128)
    c["c_hmask"] = ((s[:, None] // 64 == s[None, :] // 64) & (s[None, :] >= s[:, None])).astype(np.float32)
    M = np.zeros((128, 132), np.float32)
    for t in range(128):
        ch = t // 64
        ref = ch * 64 + 31
        for sp in range(ch * 64, ch * 64 + 64):
            M[sp, t] = float(sp <= t) - float(sp <= ref)
    for ch in range(2):
        for sp in range(ch * 64, ch * 64 + 64):
            M[sp, 128 + 2 * ch] = float(sp <= ch * 64 + 31)
            M[sp, 129 + 2 * ch] = 1.0
    c["c_M"] = M
    c["c_U"] = (s[:, None] > s[None, :]).astype(np.float32)
    c["c_L"] = (s[:, None] <= s[None, :]).astype(np.float32)
    c["c_dmask"] = (s[:, None] < s[None, :]).astype(np.float32)
    oz = np.zeros((128, 2, 128), np.float32)
    oz[:, 0, 0:64] = 1.0
    oz[:, 1, 64:128] = 1.0
    c["c_onesz"] = oz
    return c


class K:
    pass


def build(mode="full"):
    nc = bass.Bass("TRN2", target_bir_lowering=False)
    k = K()
    dr = {}
    dr["x"] = nc.dram_tensor("x", [T, D], F32, kind="ExternalInput").ap()
    dr["mem"] = nc.dram_tensor("mem", [256, D], F32, kind="ExternalInput").ap()
    for nm, shp in WSPEC:
        dr[nm] = nc.dram_tensor(nm, shp, F32, kind="ExternalInput").ap()
    cs = make_consts()
    for nm, arr in cs.items():
        dr[nm] = nc.dram_tensor(nm, list(arr.shape), F32, kind="ExternalInput").ap()
    y_d = nc.dram_tensor("y", [T, D], F32, kind="ExternalOutput").ap()
    with ExitStack() as es:
        big = es.enter_context(nc.sbuf_tensor("big", [128, SB_BYTES], U8))
        pbig = es.enter_context(nc.psum_tensor("pbig", [128, 4096], F32))
        S = Sched(nc, big, pbig)
        sems = {kk: es.enter_context(nc.semaphore("q%d" % i)) for i, kk in enumerate(S.keys)}
        P = S.psum

        def aps(*xs):
            return [a for a in xs if a is not None and not isinstance(a, (int, float))]

        def MM(out, lhsT, rhs, start=True, stop=True):
            S.op("pe", lambda e: e.matmul(out, lhsT, rhs, start=start, stop=stop), reads=[lhsT, rhs], writes=[out])

        def TR(out, in_, ident):
            S.op("pe", lambda e: e.transpose(out, in_, ident), reads=[in_, ident], writes=[out])

        def ACT(out, in_, func, scale=None, bias=None, accum=None):
            kw = {}
            if scale is not None:
                kw["scale"] = scale
            if bias is not None:
                kw["bias"] = bias
            if accum is not None:
                kw["accum_out"] = accum
            S.op("act", lambda e: e.activation(out, in_, func, **kw), reads=aps(in_, scale, bias), writes=aps(out, accum))

        def TT(out, a, b, op, eng="dve"):
            S.op(eng, lambda e: e.tensor_tensor(out, a, b, op), reads=[a, b], writes=[out])

        def TS(out, a, s1, s2, op0, op1=None, eng="dve"):
            if op1 is None:
                S.op(eng, lambda e: e.tensor_scalar(out, a, s1, None, op0), reads=aps(a, s1), writes=[out])
            else:
                S.op(eng, lambda e: e.tensor_scalar(out, a, s1, s2, op0, op1), reads=aps(a, s1, s2), writes=[out])

        def STT(out, in0, sc, in1, op0, op1, eng="dve"):
            S.op(eng, lambda e: e.scalar_tensor_tensor(out, in0, sc, in1, op0, op1), reads=aps(in0, sc, in1), writes=[out])

        def CP(out, in_, eng="dve"):
            if eng == "act":
                S.op("act", lambda e: e.copy(out, in_), reads=[in_], writes=[out])
            else:
                S.op(eng, lambda e: e.tensor_copy(out, in_), reads=[in_], writes=[out])

        def RED(out, in_, op):
            S.op("dve", lambda e: e.tensor_reduce(out, in_, AX.X, op), reads=[in_], writes=[out])

        def RECIP(out, in_):
            S.op("dve", lambda e: e.reciprocal(out, in_), reads=[in_], writes=[out])

        def MEMSET(out, v, eng="dve"):
            S.op(eng, lambda e: e.memset(out, v), writes=[out])

        def WLOAD(dst, src):
            S.dma("pool", dst, src.rearrange("(k p) n -> p k n", p=128))

        xT = S.alloc([128, 8, T], BF16)
        identf = S.alloc([128, 128], F32)
        identb = S.alloc([128, 128], BF16)
        onesD = S.alloc([128, 128], BF16)
        selb = S.alloc([16, 16, 128], BF16)
        lnp = S.alloc([128, 4, 4, 8], F32)
        S.dma("sp", identf, dr["c_ident"])
        S.dma("pool", identb, dr["c_ident"])
        S.dma("pool", selb, dr["c_sel"])
        MEMSET(onesD, 1.0 / 1024.0)
        for wi, nm in enumerate(["ln_mix_g", "ln_mix_b", "ln_ffn_g", "ln_ffn_b"]):
            for l in range(4):
                S.dma("sp", lnp[:, wi, l, :], dr[nm][l].rearrange("(k p) -> p k", p=128), allow_slow_non_contiguous=True)

        def load_xT():
            m = S.mark()
            xt = [S.alloc([128, 1024], BF16) for _ in range(2)]
            for i in range(16):
                b = xt[i % 2]
                S.dma("pool", b, dr["x"][i * 128:(i + 1) * 128, :])
                pt = P(i % 2, [128, 8, 128], BF16)
                for kk in range(8):
                    TR(pt[:, kk, :], b[:, kk * 128:(kk + 1) * 128], identb)
                CP(xT[:, :, i * 128:(i + 1) * 128], pt, eng="act" if i % 2 else "dve")
            S.release(m)

        def ln_stage(ybuf, wi, l, final=False):
            m = S.mark()
            ysq = [S.alloc([128, 8, 512], BF16) for _ in range(2)]
            yb = [S.alloc([128, 8, 512], BF16) for _ in range(2)]
            mean_sb = [S.alloc([128, 512], F32) for _ in range(2)]
            m2 = [S.alloc([128, 512], F32) for _ in range(2)]
            rstd = [S.alloc([128, 512], F32) for _ in range(2)]
            otile = [S.alloc([128, 1024], F32) for _ in range(2)] if final else None

            def stats(c):
                p = c % 2
                tc = slice(c * 512, (c + 1) * 512)
                for kk in range(8):
                    ACT(ysq[p][:, kk, :], ybuf[:, kk, tc], AF.Square)
                    CP(yb[p][:, kk, :], ybuf[:, kk, tc], eng="dve")
                mps = P(2 * p, [128, 512])
                sps = P(2 * p + 1, [128, 512])
                for kk in range(8):
                    MM(mps, onesD, yb[p][:, kk, :], kk == 0, kk == 7)
                for kk in range(8):
                    MM(sps, onesD, ysq[p][:, kk, :], kk == 0, kk == 7)

            def fin(c):
                p = c % 2
                mps = P(2 * p, [128, 512])
                sps = P(2 * p + 1, [128, 512])
                CP(mean_sb[p], mps, eng="act")
                ACT(m2[p], mps, AF.Square)
                TT(m2[p], sps, m2[p], ALU.subtract)
                ACT(m2[p], m2[p], AF.Ln, bias=1e-5)
                ACT(rstd[p], m2[p], AF.Exp, scale=-0.5)

            def norm(c):
                p = c % 2
                tc = slice(c * 512, (c + 1) * 512)
                for kk in range(8):
                    yk = ybuf[:, kk, tc]
                    TT(yk, yk, mean_sb[p], ALU.subtract, eng="pool" if kk % 2 == 0 else "dve")
                    TT(yk, yk, rstd[p], ALU.mult)
                    if final:
                        ACT(yk, yk, AF.Identity, scale=lnp[:, wi, l, kk:kk + 1], bias=lnp[:, wi + 1, l, kk:kk + 1])
                    else:
                        ACT(xT[:, kk, tc], yk, AF.Identity, scale=lnp[:, wi, l, kk:kk + 1], bias=lnp[:, wi + 1, l, kk:kk + 1])
                if final:
                    for j in range(4):
                        tt = c * 4 + j
                        ot = otile[tt % 2]
                        for half in range(2):
                            pp = P(4 + half, [128, 4, 128])
                            for q in range(4):
                                kk = half * 4 + q
                                TR(pp[:, q, :], ybuf[:, kk, tt * 128:(tt + 1) * 128], identf)
                            CP(ot[:, half * 512:(half + 1) * 512], pp.rearrange("p a b -> p (a b)"), eng="act" if half else "dve")
                        k.outd.append(S.dma("sp", y_d[tt * 128:(tt + 1) * 128, :], ot))

            stats(0)
            fin(0)
            for c in range(4):
                if c + 1 < 4:
                    stats(c + 1)
                norm(c)
                if c + 1 < 4:
                    fin(c + 1)
            S.release(m)

        def moe_stage(l, ybuf):
            m = S.mark()
            wr = S.alloc([128, 8, 20], BF16)
            S.dma("pool", wr[:, :, 0:4], dr["w_group"][l].rearrange("(k p) n -> p k n", p=128))
            S.dma("pool", wr[:, :, 4:20], dr["w_router"][l].rearrange("(k p) n -> p k n", p=128))
            bias = S.alloc([128, 20], F32)
            S.dma("sp", bias[:, 0:4], dr["b_group"][l:l + 1, :].broadcast_to([128, 4]))
            S.dma("sp", bias[:, 4:20], dr["b_router"][l:l + 1, :].broadcast_to([128, 16]))
            PR = P(7, [128, 16, 20])
            for i in range(16):
                for kk in range(8):
                    MM(PR[:, i, :], xT[:, kk, i * 128:(i + 1) * 128], wr[:, kk, :], kk == 0, kk == 7)
            L = S.alloc([128, 16, 20], F32)
            TT(L, PR, bias.unsqueeze(1).broadcast_to([128, 16, 20]), ALU.add)
            Lg = L[:, :, 0:4]
            Le = L[:, :, 4:20]
            gmax = S.alloc([128, 16], F32)
            RED(gmax, Lg, ALU.max)
            gb3 = gmax.unsqueeze(2).broadcast_to([128, 16, 4])
            dg = S.alloc([128, 16, 4], F32)
            TT(dg, Lg, gb3, ALU.subtract)
            ACT(dg, dg, AF.Exp)
            sg = S.alloc([128, 16], F32)
            RED(sg, dg, ALU.add)
            ptop = S.alloc([128, 16], F32)
            RECIP(ptop, sg)
            ohg = S.alloc([128, 16, 4], F32)
            TT(ohg, Lg, gb3, ALU.is_equal)
            TS(ohg, ohg, -1.0, 1.0e4, ALU.add, ALU.mult)
            Lm = S.alloc([128, 16, 16], F32)
            Lm4 = Lm.rearrange("p a (g e) -> p a g e", g=4)
            TT(Lm4, Le.rearrange("p a (g e) -> p a g e", g=4), ohg.unsqueeze(3).broadcast_to([128, 16, 4, 4]), ALU.add)
            m1 = S.alloc([128, 16], F32)
            RED(m1, Lm, ALU.max)
            oh1 = S.alloc([128, 16, 16], F32)
            TT(oh1, Lm, m1.unsqueeze(2).broadcast_to([128, 16, 16]), ALU.is_equal)
            Lm2 = S.alloc([128, 16, 16], F32)
            STT(Lm2, oh1, -1.0e4, Lm, ALU.mult, ALU.add)
            mm2 = S.alloc([128, 16], F32)
            RED(mm2, Lm2, ALU.max)
            oh2 = S.alloc([128, 16, 16], F32)
            TT(oh2, Lm2, mm2.unsqueeze(2).broadcast_to([128, 16, 16]), ALU.is_equal)
            ee = S.alloc([128, 16], F32)
            TT(ee, mm2, m1, ALU.subtract)
            ACT(ee, ee, AF.Exp)
            den = S.alloc([128, 16], F32)
            TS(den, ee, 1.0, None, ALU.add)
            RECIP(den, den)
            g1 = S.alloc([128, 16], F32)
            TT(g1, den, ptop, ALU.mult)
            g2 = S.alloc([128, 16], F32)
            TT(g2, g1, ee, ALU.mult)
            TT(oh1, oh1, g1.unsqueeze(2).broadcast_to([128, 16, 16]), ALU.mult)
            TT(oh2, oh2, g2.unsqueeze(2).broadcast_to([128, 16, 16]), ALU.mult)
            gb = S.alloc([128, 16, 16], BF16)
            TT(gb, oh1, oh2, ALU.add)
            if mode == "gates":
                k.dbg = (oh1, oh2)
            PT = P(5, [16, T], BF16)
            for i in range(16):
                TR(PT[:, i * 128:(i + 1) * 128], gb[:, i, :], identb)
            gatesT = S.alloc([16, T], BF16)
            CP(gatesT, PT)
            wg = [S.alloc([128, 8, 256], BF16) for _ in range(2)]
            wu = [S.alloc([128, 8, 256], BF16) for _ in range(2)]
            wd = [S.alloc([128, 2, 1024], BF16) for _ in range(2)]
            hT = S.alloc([128, 2, T], BF16)
            sgt = [S.alloc([128, 512], F32) for _ in range(2)]
            nblk = [0]
            ndn = [0]
            gpcur = [None]
            ngp = [0]

            def gateup_mm(e, c, ft):
                b = e % 2
                tc = slice(c * 512, (c + 1) * 512)
                bb = nblk[0] % 2
                nblk[0] += 1
                Pg = P(bb * 2, [128, 512])
                Pu = P(bb * 2 + 1, [128, 512])
                for kk in range(8):
                    MM(Pg, wg[b][:, kk, ft * 128:(ft + 1) * 128], xT[:, kk, tc], kk == 0, kk == 7)
                for kk in range(8):
                    MM(Pu, wu[b][:, kk, ft * 128:(ft + 1) * 128], xT[:, kk, tc], kk == 0, kk == 7)
                return (bb, Pg, Pu, ft, tc, gpcur[0])

            def gateup_ev(st):
                bb, Pg, Pu, ft, tc, Gp_ = st
                ACT(sgt[bb], Pg, AF.Silu)
                TT(sgt[bb], sgt[bb], Pu, ALU.mult)
                TT(hT[:, ft, tc], sgt[bb], Gp_, ALU.mult)

            def down(e, c, kks):
                b = e % 2
                tc = slice(c * 512, (c + 1) * 512)
                for kk in kks:
                    Pd = P(5 + ndn[0] % 2, [128, 512])
                    ndn[0] += 1
                    MM(Pd, wd[b][:, 0, kk * 128:(kk + 1) * 128], hT[:, 0, tc], True, False)
                    MM(Pd, wd[b][:, 1, kk * 128:(kk + 1) * 128], hT[:, 1, tc], False, True)
                    TT(ybuf[:, kk, tc], ybuf[:, kk, tc], Pd, ALU.add)

            prev = None
            for e in range(16):
                b = e % 2
                WLOAD(wg[b], dr["w_gate"][l, e])
                WLOAD(wu[b], dr["w_up"][l, e])
                WLOAD(wd[b], dr["w_down"][l, e])
                for c in range(4):
                    tc = slice(c * 512, (c + 1) * 512)
                    Gp = P(4 if ngp[0] % 2 == 0 else 7, [128, 512])
                    ngp[0] += 1
                    gpcur[0] = Gp
                    MM(Gp, selb[0:16, e, :], gatesT[0:16, tc])
                    for ft in range(2):
                        st = gateup_mm(e, c, ft)
                        if prev is not None:
                            down(prev[0], prev[1], range(4 * ft, 4 * ft + 4))
                        gateup_ev(st)
                    prev = (e, c)
            down(prev[0], prev[1], range(0, 8))
            S.release(m)

        def init_ybuf(ybuf):
            for kk in range(8):
                TS(ybuf[:, kk, :], xT[:, kk, :], ALPHA, None, ALU.mult)


        ones1 = S.alloc([128, 128], BF16)
        MEMSET(ones1, 1.0)
        ones128 = S.alloc([128, 128], BF16)
        MEMSET(ones128, 1.0 / 128.0)
        Ub = S.alloc([128, 128], BF16)
        Lcb = S.alloc([128, 128], BF16)
        S.dma("pool", Ub, dr["c_U"])
        S.dma("pool", Lcb, dr["c_L"])
        memT = S.alloc([128, 8, 256], BF16)

        def MMs(out, lhsT, rhs, start, stop):
            S.op("pe", lambda e: e.matmul(out, lhsT, rhs, start=start, stop=stop, skip_group_check=True), reads=[lhsT, rhs], writes=[out])

        def load_memT():
            m = S.mark()
            mt_ = S.alloc([128, 2, 1024], BF16)
            S.dma("pool", mt_, dr["mem"].rearrange("(a p) d -> p a d", p=128))
            for a in range(2):
                pt = P(2 + a, [128, 8, 128], BF16)
                for kk in range(8):
                    TR(pt[:, kk, :], mt_[:, a, kk * 128:(kk + 1) * 128], identb)
                CP(memT[:, :, a * 128:(a + 1) * 128], pt)
            S.release(m)

        def proj_fm(wsrc, tiles, dst_fn, func=None):
            m = S.mark()
            wb = [S.alloc([128, 8, 128], BF16) for _ in range(3)]
            for n, c0 in enumerate(tiles):
                w = wb[n % 3]
                WLOAD(w, wsrc[:, c0:c0 + 128])
                for c in range(4):
                    tc = slice(c * 512, (c + 1) * 512)
                    pp = P((n * 4 + c) % 2, [128, 512])
                    for kk in range(8):
                        MM(pp, w[:, kk, :], xT[:, kk, tc], kk == 0, kk == 7)
                    if func is not None and func(n) is not None:
                        ACT(dst_fn(n, c), pp, func(n))
                    else:
                        CP(dst_fn(n, c), pp, eng="act" if (n * 4 + c) % 2 else "dve")
            S.release(m)

        def mem_attn(l, qmT, mixedT):
            m = S.mark()
            wm = S.alloc([128, 8, 512], BF16)
            WLOAD(wm, dr["w_mem_kv"][l])
            kmT = S.alloc([128, 2, 256], BF16)
            vm = S.alloc([128, 2, 256], BF16)
            for j in range(2):
                pp = P(2 + j, [128, 256])
                for kk in range(8):
                    MM(pp, wm[:, kk, j * 128:(j + 1) * 128], memT[:, kk, :], kk == 0, kk == 7)
                CP(kmT[:, j, :], pp)
            for mt in range(2):
                pp = P(4 + mt, [128, 256])
                for kk in range(8):
                    MM(pp, memT[:, kk, mt * 128:(mt + 1) * 128], wm[:, kk, 256:512], kk == 0, kk == 7)
                CP(vm[:, mt, :], pp, eng="act")
            em = [S.alloc([128, 512], BF16) for _ in range(4)]
            rec = S.alloc([128, 512], F32)
            n = 0
            for j in range(2):
                for half in range(2):
                    pb = 64 * half
                    for c in range(4):
                        tc = slice(c * 512, (c + 1) * 512)
                        es_ = []
                        for mt in range(2):
                            zp = P(2 + (n % 2), [128, 512])
                            MM(zp, kmT[pb:pb + 64, j, mt * 128:(mt + 1) * 128], qmT[pb:pb + 64, j, tc])
                            eb = em[n % 4]
                            ACT(eb, zp, AF.Exp, scale=0.125)
                            es_.append(eb)
                            n += 1
                        pn = P(4, [128, 512])
                        pd = P(5, [128, 512])
                        for mt in range(2):
                            MM(pn, vm[:, mt, j * 128:(j + 1) * 128], es_[mt], mt == 0, mt == 1)
                        for mt in range(2):
                            MM(pd, ones1, es_[mt], mt == 0, mt == 1)
                        ACT(rec[pb:pb + 64, :], pd[pb:pb + 64, :], AF.Ln)
                        ACT(rec[pb:pb + 64, :], rec[pb:pb + 64, :], AF.Exp, scale=-1.0)
                        TT(mixedT[pb:pb + 64, 6 + j, tc], pn[pb:pb + 64, :], rec[pb:pb + 64, :], ALU.mult)
            S.release(m)

        def wo_stage(l, mixedT, ybuf):
            m = S.mark()
            wob = [S.alloc([128, 8, 256], BF16) for _ in range(2)]
            n = 0
            for q in range(4):
                w = wob[q % 2]
                WLOAD(w, dr["w_o"][l][:, q * 256:(q + 1) * 256])
                for kq in range(2):
                    kk = 2 * q + kq
                    for c in range(4):
                        tc = slice(c * 512, (c + 1) * 512)
                        pp = P(n % 2, [128, 512])
                        n += 1
                        for f in range(8):
                            MM(pp, w[:, f, kq * 128:(kq + 1) * 128], mixedT[:, f, tc], f == 0, f == 7)
                        STT(ybuf[:, kk, tc], xT[:, kk, tc], ALPHA, pp, ALU.mult, ALU.add)
            S.release(m)

        def kv_stage(kT, vtok):
            proj_fm(dr["w_kv_shared"], [j * 128 for j in range(6)], lambda n, c: kT[:, n, c * 512:(c + 1) * 512])
            m = S.mark()
            wv = S.alloc([128, 8, 768], BF16)
            WLOAD(wv, dr["w_kv_shared"][:, 768:1536])
            for i in range(16):
                ts_ = slice(i * 128, (i + 1) * 128)
                pa = P(2 + 2 * (i % 2), [128, 512])
                pb_ = P(3 + 2 * (i % 2), [128, 256])
                for kk in range(8):
                    MM(pa, xT[:, kk, ts_], wv[:, kk, 0:512], kk == 0, kk == 7)
                for kk in range(8):
                    MM(pb_, xT[:, kk, ts_], wv[:, kk, 512:768], kk == 0, kk == 7)
                CP(vtok[:, i, 0:512], pa, eng="act")
                CP(vtok[:, i, 512:768], pb_, eng="dve")
            S.release(m)

        def sb_attn(qT, kT, vtok, mixedT):
            m = S.mark()
            dm = S.alloc([128, 4, 512], BF16)
            dmf = S.alloc([128, 128], F32)
            S.dma("sp", dmf, dr["c_dmask"])
            for q in range(4):
                if q > 0:
                    MEMSET(dm[:, q, 0:q * 128], 0.0)
                CP(dm[:, q, q * 128:(q + 1) * 128], dmf)
                if q < 3:
                    MEMSET(dm[:, q, (q + 1) * 128:512], 1.0)
            yo = [32768]

            def ya(shape, dt):
                nb = int(np.prod(shape[1:])) * esz(dt)
                v = yview(yo[0], shape, dt)
                yo[0] += nb
                assert yo[0] <= 65536
                return v
            REF, RLF, RLB, RAB = 5, 3, 4, 3
            ef = [ya([128, 512], F32) for _ in range(REF)]
            Lf = [ya([128, 512], F32) for _ in range(RLF)]
            Lb = [ya([128, 512], BF16) for _ in range(RLB)]
            ab = [ya([128, 512], BF16) for _ in range(RAB)]
            zbanks = [0, 1, 6, 7]
            items = []
            for j in range(6):
                for c in range(4):
                    top = 4 * c + 3
                    for I in range(top, -1, -1):
                        for half in range(2):
                            items.append((j, half, c, I, half, I == top, I == 0))
            N = len(items)

            def Zb(n):
                return P(zbanks[n % 4], [128, 512])

            def accb(n):
                return P(2 + items[n][4], [128, 512])

            def Ob(n):
                return P(4 + items[n][4], [128, 512])

            def s0(n):
                j, half, c, I, _, _, _ = items[n]
                pb = 64 * half
                MM(Zb(n), kT[pb:pb + 64, j, I * 128:(I + 1) * 128], qT[pb:pb + 64, j, c * 512:(c + 1) * 512])

            def s1(n):
                ACT(ef[n % REF], Zb(n), AF.Exp, scale=0.125)
                ACT(Lf[n % RLF], ef[n % REF], AF.Ln, bias=1.0)

            def s2(n):
                j, half, c, I, _, _, _ = items[n]
                if I >= 4 * c:
                    TT(Lb[n % RLB], Lf[n % RLF], dm[:, I - 4 * c, :], ALU.mult)
                else:
                    CP(Lb[n % RLB], Lf[n % RLF], eng="pool")
                STT(ef[n % REF], Zb(n), 0.125, Lf[n % RLF], ALU.mult, ALU.subtract)

            def s3a(n):
                MMs(accb(n), Ub, Lb[n % RLB], items[n][5], True)

            def s3b(n):
                TT(ef[n % REF], ef[n % REF], accb(n), ALU.subtract)

            def s3c(n):
                MMs(accb(n), Lcb, Lb[n % RLB], False, True)

            def s4(n):
                j, half, c, I, _, _, _ = items[n]
                ACT(ab[n % RAB], ef[n % REF], AF.Exp)
                if I >= 4 * c:
                    TT(ab[n % RAB], ab[n % RAB], dm[:, I - 4 * c, :], ALU.mult)

            def s5(n):
                j, half, c, I, _, first, last = items[n]
                MMs(Ob(n), vtok[:, I, j * 128:(j + 1) * 128], ab[n % RAB], first, last)
                if last:
                    pb = 64 * half
                    CP(mixedT[pb:pb + 64, j, c * 512:(c + 1) * 512], Ob(n)[pb:pb + 64, :], eng="act")

            def ok(n):
                return 0 <= n < N
            for t in range(N + 7):
                if ok(t - 5):
                    s4(t - 5)
                if ok(t):
                    s0(t)
                if ok(t - 1):
                    s1(t - 1)
                if ok(t - 2):
                    s2(t - 2)
                if ok(t - 5):
                    s3c(t - 5)
                if ok(t - 3):
                    s3a(t - 3)
                if ok(t - 4):
                    s3b(t - 4)
                if ok(t - 6):
                    s5(t - 6)
            S.release(m)

        def yview(off, shape, dt):
            return S.view(k.ybuf_off + off, shape, dt)

        def b_mix(l, kT, vtok, ybuf):
            m = S.mark()
            qT = yview(0, [128, 6, T], BF16)
            qmT = yview(6 * T * 2, [128, 2, T], BF16)
            proj_fm(dr["b_w_in"][l - 2], [j * 128 for j in range(8)],
                    lambda n, c: (qT[:, n, c * 512:(c + 1) * 512] if n < 6 else qmT[:, n - 6, c * 512:(c + 1) * 512]))
            mixedT = S.alloc([128, 8, T], BF16)
            mem_attn(l, qmT, mixedT)
            sb_attn(qT, kT, vtok, mixedT)
            if mode == "Bmix":
                k.dbg_mixedT = mixedT
                return
            wo_stage(l, mixedT, ybuf)
            S.release(m)

        def a_mix(l, ybuf):
            import os
            m = S.mark()
            qT = yview(0, [128, 6, T], BF16)
            gT = yview(6 * T * 2, [128, 6, T], BF16)
            qmT = yview(12 * T * 2, [128, 2, T], BF16)
            tiles = [j * 128 for j in range(6)] + [2304 + j * 128 for j in range(6)] + [3072, 3200]

            def dst(n, c):
                tc = slice(c * 512, (c + 1) * 512)
                if n < 6:
                    return qT[:, n, tc]
                if n < 12:
                    return gT[:, n - 6, tc]
                return qmT[:, n - 12, tc]
            proj_fm(dr["a_w_in"][l], tiles, dst, func=lambda n: AF.Silu if n < 12 else None)
            mixedT = S.alloc([128, 8, T], BF16)
            mem_attn(l, qmT, mixedT)
            Wfi = S.alloc([128, 8, 1536], BF16)
            if os.environ.get("NOWFI"):
                MEMSET(Wfi, 0.01)
            for ch_ in range(0 if os.environ.get("NOWFI") else 6):
                WLOAD(Wfi[:, :, ch_ * 256:(ch_ + 1) * 256], dr["a_w_in"][l][:, 768 + ch_ * 256:768 + (ch_ + 1) * 256])
            c1bc = S.alloc([128, 768], F32)
            c1fm = S.alloc([128, 6], F32)
            gnfm = S.alloc([128, 6], F32)
            S.dma("sp", gnfm, dr["a_gnorm"][l].rearrange("(h p) -> p h", p=128), allow_slow_non_contiguous=True)
            if l == 0:
                MEMSET(c1bc, 1.0)
                MEMSET(c1fm, 1.0)
            else:
                mk_ = S.mark()
                a1bc = S.alloc([128, 768], F32)
                S.release(mk_)
                S.dma("sp", c1bc, dr["a_lower_bounds"][0:1, :].broadcast_to([128, 768]))
                S.dma("sp", a1bc, dr["a_lower_bounds"][1:2, :].broadcast_to([128, 768]))
                TT(c1bc, c1bc, a1bc, ALU.subtract)
                ACT(c1bc, c1bc, AF.Sigmoid)
                a1fm = S.alloc([128, 6], F32)
                S.dma("sp", c1fm, dr["a_lower_bounds"][0].rearrange("(h p) -> p h", p=128), allow_slow_non_contiguous=True)
                S.dma("sp", a1fm, dr["a_lower_bounds"][1].rearrange("(h p) -> p h", p=128), allow_slow_non_contiguous=True)
                TT(c1fm, c1fm, a1fm, ALU.subtract)
                ACT(c1fm, c1fm, AF.Sigmoid)
            Mf = S.alloc([128, 132], F32)
            S.dma("sp", Mf, dr["c_M"])
            hmf = S.alloc([128, 128], F32)
            S.dma("sp", hmf, dr["c_hmask"])
            Sst = S.alloc([128, 6, 128], F32)
            MEMSET(Sst, 0.0)
            sneg = S.alloc([128, 768], F32)
            wtmp = S.alloc([128, 768], F32)
            einv = wtmp
            logf2 = [S.alloc([128, 768], F32) for _ in range(2)]
            ktok2 = [S.alloc([128, 768], BF16) for _ in range(2)]
            vtk2 = [S.alloc([128, 768], BF16) for _ in range(2)]
            E = [S.alloc([128, 132], F32) for _ in range(2)]
            qt = [S.alloc([128, 128], BF16) for _ in range(2)]
            kt = [S.alloc([128, 128], BF16) for _ in range(2)]
            sc = [S.alloc([128, 128], BF16) for _ in range(2)]
            SpA = [S.alloc([128, 128], BF16) for _ in range(2)]
            SpB = [S.alloc([128, 128], BF16) for _ in range(2)]
            SA2 = [S.alloc([128, 128], F32) for _ in range(2)]
            ptmp2 = [S.alloc([128, 128], F32) for _ in range(2)]
            sAB2 = [S.alloc([128, 2], F32) for _ in range(2)]
            osq = [S.alloc([128, 128], BF16) for _ in range(2)]
            rs = [S.alloc([128, 128], F32) for _ in range(2)]
            o12 = [S.alloc([128, 128], F32) for _ in range(2)]

            def TM(i, part):
                ts_ = slice(i * 128, (i + 1) * 128)
                logf, ktok, vtk = logf2[i % 2], ktok2[i % 2], vtk2[i % 2]
                pf = [P(0, [128, 512]), P(1, [128, 512]), P(2, [128, 512])]
                if part == 0:
                    for ch in range(3):
                        for kk in range(8):
                            MM(pf[ch], xT[:, kk, ts_], Wfi[:, kk, ch * 512:(ch + 1) * 512], kk == 0, kk == 7)
                elif part == 1:
                    ACT(sneg[:, 0:512], pf[0], AF.Exp)
                    ACT(sneg[:, 512:768], pf[1][:, 0:256], AF.Exp)
                    CP(vtk[:, 0:256], pf[1][:, 256:512])
                    CP(vtk[:, 256:768], pf[2])
                    ACT(sneg, sneg, AF.Ln, bias=1.0)
                    ACT(sneg, sneg, AF.Exp, scale=-1.0)
                elif part == 2:
                    TT(wtmp, sneg, c1bc, ALU.mult)
                    ACT(logf, wtmp, AF.Ln, scale=-1.0, bias=1.0)
                elif part == 3:
                    pdk = [P(0, [128, 512]), P(1, [128, 256])]
                    MM(pdk[0][:, 0:256], Mf[:, 0:128], logf[:, 0:256])
                    MM(pdk[0][:, 256:512], Mf[:, 0:128], logf[:, 256:512])
                    MM(pdk[1], Mf[:, 0:128], logf[:, 512:768])
                else:
                    pdk = [P(0, [128, 512]), P(1, [128, 256])]
                    ACT(einv[:, 0:512], pdk[0], AF.Exp, scale=-1.0)
                    ACT(einv[:, 512:768], pdk[1], AF.Exp, scale=-1.0)
                    TT(ktok, sneg, einv, ALU.mult)

            def hp(n):
                i, h = divmod(n, 6)
                r = n % 2
                X = 3 + 2 * r
                Y = 4 + 2 * r
                d = dict(i=i, h=h, r=r, ts=slice(i * 128, (i + 1) * 128), hs=slice(h * 128, (h + 1) * 128),
                         logf=logf2[i % 2], ktok=ktok2[i % 2], vtk=vtk2[i % 2],
                         PD=P(X, [128, 132]), PK=P(X, [128, 128], BF16, off=768),
                         PO=P(X, [128, 128], F32, off=1024), PM=P(X, [128, 128], F32, off=1536),
                         PS=P(Y, [128, 128], F32), PA=P(Y, [128, 128], F32, off=512), PB=P(Y, [128, 128], F32, off=1024))
                return d

            def A1(n):
                d = hp(n)
                MM(d["PA"], d["ktok"][0:64, d["hs"]], d["vtk"][0:64, d["hs"]])
                MM(d["PD"], d["logf"][:, d["hs"]], Mf)
                MM(d["PB"], d["ktok"][64:128, d["hs"]], d["vtk"][64:128, d["hs"]])
                TR(d["PK"], d["ktok"][:, d["hs"]], identb)

            def A2(n):
                d = hp(n)
                r, h = d["r"], d["h"]
                ACT(E[r], d["PD"], AF.Exp)
                ACT(kt[r], d["PK"], AF.Identity, scale=c1fm[:, h:h + 1])
                STT(qt[r], qT[:, h, d["ts"]], 128.0 ** -0.5, E[r][:, 0:128], ALU.mult, ALU.mult)
                TS(sAB2[r][:, 0:1], E[r][:, 63:64], c1fm[:, h:h + 1], None, ALU.mult)
                TS(sAB2[r][:, 1:2], E[r][:, 127:128], c1fm[:, h:h + 1], None, ALU.mult)
                MM(d["PS"], kt[r], qt[r])

            def B1(n):
                d = hp(n)
                r, h = d["r"], d["h"]
                TT(sc[r], d["PS"], hmf, ALU.mult)
                TS(SpA[r], Sst[:, h, :], E[r][:, 128:129], None, ALU.mult)
                TS(ptmp2[r], d["PA"], sAB2[r][:, 0:1], None, ALU.mult)
                STT(SA2[r], Sst[:, h, :], E[r][:, 129:130], ptmp2[r], ALU.mult, ALU.add)
                TS(SpB[r], SA2[r], E[r][:, 130:131], None, ALU.mult)
                TS(ptmp2[r], d["PB"], sAB2[r][:, 1:2], None, ALU.mult)
                STT(Sst[:, h, :], SA2[r], E[r][:, 131:132], ptmp2[r], ALU.mult, ALU.add)
                MMs(d["PO"], d["vtk"][:, d["hs"]], sc[r], True, True)
                MMs(d["PO"][:, 0:64], SpA[r], qt[r][:, 0:64], False, True)
                MMs(d["PO"][:, 64:128], SpB[r], qt[r][:, 64:128], False, True)
                ACT(osq[r], d["PO"], AF.Square)

            def B2(n):
                d = hp(n)
                r, h = d["r"], d["h"]
                MM(d["PM"], ones128, osq[r])
                ACT(rs[r], d["PM"], AF.Ln, bias=1e-6)
                ACT(rs[r], rs[r], AF.Exp, scale=-0.5)
                STT(o12[r], d["PO"], gnfm[:, h:h + 1], rs[r], ALU.mult, ALU.mult)
                TT(mixedT[:, h, d["ts"]], o12[r], gT[:, h, d["ts"]], ALU.mult)

            for part in range(5):
                TM(0, part)
            NH = 96
            A1(0)
            A2(0)
            for n in range(NH):
                i, h = divmod(n, 6)
                if n + 1 < NH and (n + 1) % 6 != 0:
                    A1(n + 1)
                if i + 1 < 16 and h < 5:
                    TM(i + 1, h)
                if n + 1 < NH and (n + 1) % 6 == 0:
                    A1(n + 1)
                B1(n)
                if n + 1 < NH:
                    A2(n + 1)
                B2(n)
            if mode == "Amix":
                k.dbg_mixedT = mixedT
                return
            wo_stage(l, mixedT, ybuf)
            S.release(m)

        def dump_fm(src):
            m = S.mark()
            ot = [S.alloc([128, 1024], F32) for _ in range(2)]
            for tt in range(16):
                o = ot[tt % 2]
                pp = P(tt % 2, [128, 8, 128], BF16)
                for kk in range(8):
                    TR(pp[:, kk, :], src[:, kk, tt * 128:(tt + 1) * 128], identb)
                CP(o, pp.rearrange("p a b -> p (a b)"))
                k.outd.append(S.dma("sp", y_d[tt * 128:(tt + 1) * 128, :], o))
            S.release(m)

        k.outd = []
        load_xT()
        load_memT()
        k.ybuf_off = S.mark()
        ybuf = S.alloc([128, 8, T], F32)
        if mode == "moe0":
            init_ybuf(ybuf)
            moe_stage(0, ybuf)
            ln_stage(ybuf, 2, 0, final=True)
        elif mode == "ln0":
            init_ybuf(ybuf)
            ln_stage(ybuf, 2, 0, final=True)
        elif mode == "Bmix":
            kT = S.alloc([128, 6, T], BF16)
            vtok = S.alloc([128, 16, 768], BF16)
            kv_stage(kT, vtok)
            b_mix(2, kT, vtok, ybuf)
            dump_fm(k.dbg_mixedT)
        elif mode == "Amix":
            a_mix(0, ybuf)
            dump_fm(k.dbg_mixedT)
        else:
            kT = vtok = None
            nl = 4 if mode == "full" else int(mode[1:])
            for l in range(nl):
                if l < 2:
                    a_mix(l, ybuf)
                else:
                    if l == 2:
                        kT = S.alloc([128, 6, T], BF16)
                        vtok = S.alloc([128, 16, 768], BF16)
                        kv_stage(kT, vtok)
                    b_mix(l, kT, vtok, ybuf)
                ln_stage(ybuf, 0, l)
                init_ybuf(ybuf)
                moe_stage(l, ybuf)
                ln_stage(ybuf, 2, l, final=(l == nl - 1))
        S.final_wait("sp", k.outd)
        S.emit(sems)
        k.nops = {e: len(S.ops[e]) for e in S.ENG}
    return nc, cs, k


_CACHE = {}


def kernel(**inputs):
    if "nc" not in _CACHE:
        _CACHE["nc"] = build("full")
    nc, cs, _k = _CACHE["nc"]
    x = np.ascontiguousarray(inputs["x"], dtype=np.float32)
    mem = np.ascontiguousarray(inputs["mem"], dtype=np.float32)
    in_maps = []
    for b in range(8):
        m = {"x": x[b], "mem": mem[b]}
        for nm, _shp in WSPEC:
            m[nm] = np.ascontiguousarray(inputs[nm], dtype=np.float32)
        m.update(cs)
        in_maps.append(m)
    res = run_bass_kernel_spmd(nc, in_maps, core_ids=list(range(8)))
    return np.stack([np.asarray(r["y"], dtype=np.float32) for r in res.results], axis=0)
```

```python
import numpy as np
import concourse.bass as bass
import concourse.mybir as mybir

F32 = mybir.dt.float32
BF16 = mybir.dt.bfloat16
U8 = mybir.dt.uint8
I32 = mybir.dt.int32
ALU = mybir.AluOpType
AF = mybir.ActivationFunctionType
AX = mybir.AxisListType

_ESZ = {F32: 4, BF16: 2, U8: 1, I32: 4}

SB_BYTES = 207 * 1024
N_DSEM = 32
BUCKET = 2048


def esz(dt):
    return _ESZ[dt]


class Sched:
    ENG = ("pe", "act", "dve", "pool", "sp")

    def __init__(self, nc, big, pbig):
        self.nc = nc
        self.big = big
        self.pbig = pbig
        self.ops = {e: [] for e in self.ENG}
        self.keys = list(self.ENG[:4]) + ["d%d" % i for i in range(N_DSEM)]
        self.kidx = {k: i for i, k in enumerate(self.keys)}
        self.nk = len(self.keys)
        self.know = {e: [0] * self.nk for e in self.ENG}
        self.clock = {}
        self.seq = {k: 0 for k in self.keys}
        self.hist = {"sb": {}, "ps": {}, "dr": {}}
        self.ndma = 0
        self.ndma_sw = 0
        self.sb_top = 0
        self.marked = set()
        self.out_deps = []

    def alloc(self, shape, dt, name=None):
        n = int(np.prod(shape[1:])) * esz(dt)
        n = (n + 63) // 64 * 64
        off = self.sb_top
        self.sb_top += n
        assert self.sb_top <= SB_BYTES, ("SBUF overflow", name, self.sb_top)
        return self.view(off, shape, dt)

    def view(self, off, shape, dt):
        nb = int(np.prod(shape[1:])) * esz(dt)
        v = self.big[0:shape[0], off:off + nb]
        if dt != U8:
            v = v.bitcast(dt)
        if len(shape) == 3:
            v = v.rearrange("p (a b) -> p a b", a=shape[1])
        elif len(shape) == 4:
            v = v.rearrange("p (a b c) -> p a b c", a=shape[1], b=shape[2])
        return v

    def mark(self):
        return self.sb_top

    def release(self, m):
        self.sb_top = m

    def psum(self, bank, shape, dt=F32, off=0):
        nb = int(np.prod(shape[1:])) * esz(dt)
        assert off + nb <= 2048 * 8
        e0 = (bank * 2048 + off) // 4
        v = self.pbig[0:shape[0], e0:e0 + (nb + 3) // 4]
        if dt != F32:
            v = v.bitcast(dt)
        if len(shape) == 3:
            v = v.rearrange("p (a b) -> p a b", a=shape[1])
        return v

    def rect(self, ap):
        t = ap.tensor
        nm = t.name
        e = esz(ap.dtype)
        a = ap.ap
        off = int(ap.offset)
        if nm == "big":
            sp = "sb"
        elif nm == "pbig":
            sp = "ps"
        else:
            return ("dr", nm, 0, 1, 0, 1)
        pstep, pn = a[0]
        if pstep == 0:
            pstep = 1 << 40
        p0 = off // pstep if pstep < (1 << 40) else 0
        fo = off - p0 * pstep if pstep < (1 << 40) else off
        span = 1
        for st, cn in a[1:]:
            span += abs(st) * (cn - 1)
        b0 = fo * e
        b1 = (fo + span) * e
        p1 = p0 + pn
        if sp == "ps":
            b0 = (b0 // 2048) * 2048
            b1 = ((b1 + 2047) // 2048) * 2048
            p0 = (p0 // 32) * 32
            p1 = ((p1 + 31) // 32) * 32
        return (sp, None, p0, p1, b0, b1)

    def _deps_for(self, accesses, own=None):
        deps = {}
        regs = []
        for r, kind in accesses:
            sp, nm, p0, p1, b0, b1 = r
            h = self.hist[sp]
            if sp == "dr":
                bks = [nm]
            else:
                bks = range(b0 // BUCKET, (b1 - 1) // BUCKET + 1)
            for bk in bks:
                d = h.get(bk)
                if d is None:
                    d = {}
                    h[bk] = d
                dead = []
                for (k2, kind2, q0, q1, c0, c1), s2 in d.items():
                    if q0 < p1 and p0 < q1 and c0 < b1 and b0 < c1:
                        rr_ps = (sp == "ps" and kind == "r" and kind2 == "r" and k2 != own)
                        if kind == "w" or kind2 == "w" or rr_ps:
                            skip = False
                            if k2 == own:
                                if own == "pe" or not (kind == "r" and kind2 == "w"):
                                    skip = True
                            if not skip and deps.get(k2, 0) < s2:
                                deps[k2] = s2
                        if kind == "w" and p0 <= q0 and q1 <= p1 and b0 <= c0 and c1 <= b1:
                            dead.append((k2, kind2, q0, q1, c0, c1))
                for kk in dead:
                    del d[kk]
                regs.append((d, kind, p0, p1, b0, b1))
        return deps, regs

    def _register(self, regs, key, seq):
        for d, kind, p0, p1, b0, b1 in regs:
            d[(key, kind, p0, p1, b0, b1)] = seq

    def op(self, eng, fn, reads=(), writes=()):
        acc = [(self.rect(a), "r") for a in reads] + [(self.rect(a), "w") for a in writes]
        key = eng
        deps, regs = self._deps_for(acc, own=key)
        self.seq[key] += 1
        seq = self.seq[key]
        waits = self._resolve(eng, deps, own=key)
        clk = list(self.know[eng])
        clk[self.kidx[key]] = seq
        self.clock[(key, seq)] = clk
        self._register(regs, key, seq)
        self.ops[eng].append({"fn": fn, "waits": waits, "inc": (key, seq)})

    def _resolve(self, eng, deps, own=None):
        know = list(self.know[eng])
        waits = []
        items = sorted(deps.items(), key=lambda kv: -kv[1])
        for k, s in items:
            if k == own:
                pass
            i = self.kidx[k]
            if know[i] >= s:
                continue
            waits.append((k, s))
            self.marked.add((k, s))
            c = self.clock[(k, s)]
            know = [a if a >= b else b for a, b in zip(know, c)]
        self.know[eng] = know
        return waits

    def dma(self, eng, out, in_, reads=None, writes=None, **kw):
        rd = [in_] if reads is None else reads
        wr = [out] if writes is None else writes
        acc = [(self.rect(a), "r") for a in rd] + [(self.rect(a), "w") for a in wr]
        deps, regs = self._deps_for(acc)
        half = N_DSEM // 2
        if eng == "pool":
            k = "d%d" % (half + self.ndma_sw % half)
            self.ndma_sw += 1
        else:
            k = "d%d" % (self.ndma % half)
            self.ndma += 1
        prev = self.seq[k]
        if prev > 0:
            deps[k] = max(deps.get(k, 0), prev)
        self.seq[k] += 1
        seq = self.seq[k]
        waits = self._resolve(eng, deps)
        clk = list(self.know[eng])
        clk[self.kidx[k]] = seq
        self.clock[(k, seq)] = clk
        self._register(regs, k, seq)
        self.ops[eng].append({"dma": (out, in_), "kw": kw, "waits": waits, "inc": (k, seq)})
        return (k, seq)

    def final_wait(self, eng, deps):
        waits = self._resolve(eng, dict(deps))
        self.ops[eng].append({"waits": waits})

    def emit(self, sems):
        rank = {}
        for k in self.ENG[:4]:
            ms = sorted(s for (kk, s) in self.marked if kk == k)
            rank[k] = {s: i + 1 for i, s in enumerate(ms)}

        def val(k, s):
            if k[0] == "d" and k[1:].isdigit():
                return 16 * s
            return rank[k][s]

        nc = self.nc
        engobj = {"pe": "tensor", "act": "scalar", "dve": "vector", "pool": "gpsimd", "sp": "sync"}
        with nc.Block() as block:
            for e in self.ENG:
                ops = self.ops[e]

                def body(engine, ops=ops, e=e):
                    for o in ops:
                        for (k, s) in o["waits"]:
                            engine.wait_ge(sems[k], val(k, s))
                        if "fn" in o:
                            ins = o["fn"](engine)
                            k, s = o["inc"]
                            if (k, s) in self.marked:
                                ins.then_inc(sems[k], 1)
                        elif "dma" in o:
                            out, in_ = o["dma"]
                            k, s = o["inc"]
                            engine.dma_start(out=out, in_=in_, **o["kw"]).then_inc(sems[k], 16)

                getattr(block, engobj[e])(body)
from contextlib import ExitStack
from concourse.bass_utils import run_bass_kernel_spmd

T = 2048
D = 1024
ALPHA = 8 ** 0.25
WSPEC = [("a_w_in", [2, 1024, 3328]), ("a_lower_bounds", [2, 768]), ("a_gnorm", [2, 768]),
         ("b_w_in", [2, 1024, 1024]), ("w_kv_shared", [1024, 1536]), ("w_mem_kv", [4, 1024, 512]),
         ("w_o", [4, 1024, 1024]), ("ln_mix_g", [4, 1024]), ("ln_mix_b", [4, 1024]),
         ("ln_ffn_g", [4, 1024]), ("ln_ffn_b", [4, 1024]), ("w_group", [4, 1024, 4]),
         ("b_group", [4, 4]), ("w_router", [4, 1024, 16]), ("b_router", [4, 16]),
         ("w_gate", [4, 16, 1024, 256]), ("w_up", [4, 16, 1024, 256]), ("w_down", [4, 16, 256, 1024])]


def make_consts():
    c = {}
    c["c_ident"] = np.eye(128, dtype=np.float32)
    sel = np.zeros((16, 16, 128), np.float32)
    for e in range(16):
        sel[e, e, :] = 1.0
    c["c_sel"] = sel
    s = np.arange(128)
    c["c_hmask"] = ((s[:, None] // 64 == s[None, :] // 64) & (s[None, :] >= s[:, None])).astype(np.float32)
    M = np.zeros((128, 132), np.float32)
    for t in range(128):
        ch = t // 64
        ref = ch * 64 + 31
        for sp in range(ch * 64, ch * 64 + 64):
            M[sp, t] = float(sp <= t) - float(sp <= ref)
    for ch in range(2):
        for sp in range(ch * 64, ch * 64 + 64):
            M[sp, 128 + 2 * ch] = float(sp <= ch * 64 + 31)
            M[sp, 129 + 2 * ch] = 1.0
    c["c_M"] = M
    c["c_U"] = (s[:, None] > s[None, :]).astype(np.float32)
    c["c_L"] = (s[:, None] <= s[None, :]).astype(np.float32)
    c["c_dmask"] = (s[:, None] < s[None, :]).astype(np.float32)
    oz = np.zeros((128, 2, 128), np.float32)
    oz[:, 0, 0:64] = 1.0
    oz[:, 1, 64:128] = 1.0
    c["c_onesz"] = oz
    return c


class K:
    pass


def build(mode="full"):
    nc = bass.Bass("TRN2", target_bir_lowering=False)
    k = K()
    dr = {}
    dr["x"] = nc.dram_tensor("x", [T, D], F32, kind="ExternalInput").ap()
    dr["mem"] = nc.dram_tensor("mem", [256, D], F32, kind="ExternalInput").ap()
    for nm, shp in WSPEC:
        dr[nm] = nc.dram_tensor(nm, shp, F32, kind="ExternalInput").ap()
    cs = make_consts()
    for nm, arr in cs.items():
        dr[nm] = nc.dram_tensor(nm, list(arr.shape), F32, kind="ExternalInput").ap()
    y_d = nc.dram_tensor("y", [T, D], F32, kind="ExternalOutput").ap()
    with ExitStack() as es:
        big = es.enter_context(nc.sbuf_tensor("big", [128, SB_BYTES], U8))
        pbig = es.enter_context(nc.psum_tensor("pbig", [128, 4096], F32))
        S = Sched(nc, big, pbig)
        sems = {kk: es.enter_context(nc.semaphore("q%d" % i)) for i, kk in enumerate(S.keys)}
        P = S.psum

        def aps(*xs):
            return [a for a in xs if a is not None and not isinstance(a, (int, float))]

        def MM(out, lhsT, rhs, start=True, stop=True):
            S.op("pe", lambda e: e.matmul(out, lhsT, rhs, start=start, stop=stop), reads=[lhsT, rhs], writes=[out])

        def TR(out, in_, ident):
            S.op("pe", lambda e: e.transpose(out, in_, ident), reads=[in_, ident], writes=[out])

        def ACT(out, in_, func, scale=None, bias=None, accum=None):
            kw = {}
            if scale is not None:
                kw["scale"] = scale
            if bias is not None:
                kw["bias"] = bias
            if accum is not None:
                kw["accum_out"] = accum
            S.op("act", lambda e: e.activation(out, in_, func, **kw), reads=aps(in_, scale, bias), writes=aps(out, accum))

        def TT(out, a, b, op, eng="dve"):
            S.op(eng, lambda e: e.tensor_tensor(out, a, b, op), reads=[a, b], writes=[out])

        def TS(out, a, s1, s2, op0, op1=None, eng="dve"):
            if op1 is None:
                S.op(eng, lambda e: e.tensor_scalar(out, a, s1, None, op0), reads=aps(a, s1), writes=[out])
            else:
                S.op(eng, lambda e: e.tensor_scalar(out, a, s1, s2, op0, op1), reads=aps(a, s1, s2), writes=[out])

        def STT(out, in0, sc, in1, op0, op1, eng="dve"):
            S.op(eng, lambda e: e.scalar_tensor_tensor(out, in0, sc, in1, op0, op1), reads=aps(in0, sc, in1), writes=[out])

        def CP(out, in_, eng="dve"):
            if eng == "act":
                S.op("act", lambda e: e.copy(out, in_), reads=[in_], writes=[out])
            else:
                S.op(eng, lambda e: e.tensor_copy(out, in_), reads=[in_], writes=[out])

        def RED(out, in_, op):
            S.op("dve", lambda e: e.tensor_reduce(out, in_, AX.X, op), reads=[in_], writes=[out])

        def RECIP(out, in_):
            S.op("dve", lambda e: e.reciprocal(out, in_), reads=[in_], writes=[out])

        def MEMSET(out, v, eng="dve"):
            S.op(eng, lambda e: e.memset(out, v), writes=[out])

        def WLOAD(dst, src):
            S.dma("pool", dst, src.rearrange("(k p) n -> p k n", p=128))

        xT = S.alloc([128, 8, T], BF16)
        identf = S.alloc([128, 128], F32)
        identb = S.alloc([128, 128], BF16)
        onesD = S.alloc([128, 128], BF16)
        selb = S.alloc([16, 16, 128], BF16)
        lnp = S.alloc([128, 4, 4, 8], F32)
        S.dma("sp", identf, dr["c_ident"])
        S.dma("pool", identb, dr["c_ident"])
        S.dma("pool", selb, dr["c_sel"])
        MEMSET(onesD, 1.0 / 1024.0)
        for wi, nm in enumerate(["ln_mix_g", "ln_mix_b", "ln_ffn_g", "ln_ffn_b"]):
            for l in range(4):
                S.dma("sp", lnp[:, wi, l, :], dr[nm][l].rearrange("(k p) -> p k", p=128), allow_slow_non_contiguous=True)

        def load_xT():
            m = S.mark()
            xt = [S.alloc([128, 1024], BF16) for _ in range(2)]
            for i in range(16):
                b = xt[i % 2]
                S.dma("pool", b, dr["x"][i * 128:(i + 1) * 128, :])
                pt = P(i % 2, [128, 8, 128], BF16)
                for kk in range(8):
                    TR(pt[:, kk, :], b[:, kk * 128:(kk + 1) * 128], identb)
                CP(xT[:, :, i * 128:(i + 1) * 128], pt, eng="act" if i % 2 else "dve")
            S.release(m)

        def ln_stage(ybuf, wi, l, final=False):
            m = S.mark()
            ysq = [S.alloc([128, 8, 512], BF16) for _ in range(2)]
            yb = [S.alloc([128, 8, 512], BF16) for _ in range(2)]
            mean_sb = [S.alloc([128, 512], F32) for _ in range(2)]
            m2 = [S.alloc([128, 512], F32) for _ in range(2)]
            rstd = [S.alloc([128, 512], F32) for _ in range(2)]
            otile = [S.alloc([128, 1024], F32) for _ in range(2)] if final else None

            def stats(c):
                p = c % 2
                tc = slice(c * 512, (c + 1) * 512)
                for kk in range(8):
                    ACT(ysq[p][:, kk, :], ybuf[:, kk, tc], AF.Square)
                    CP(yb[p][:, kk, :], ybuf[:, kk, tc], eng="dve")
                mps = P(2 * p, [128, 512])
                sps = P(2 * p + 1, [128, 512])
                for kk in range(8):
                    MM(mps, onesD, yb[p][:, kk, :], kk == 0, kk == 7)
                for kk in range(8):
                    MM(sps, onesD, ysq[p][:, kk, :], kk == 0, kk == 7)

            def fin(c):
                p = c % 2
                mps = P(2 * p, [128, 512])
                sps = P(2 * p + 1, [128, 512])
                CP(mean_sb[p], mps, eng="act")
                ACT(m2[p], mps, AF.Square)
                TT(m2[p], sps, m2[p], ALU.subtract)
                ACT(m2[p], m2[p], AF.Ln, bias=1e-5)
                ACT(rstd[p], m2[p], AF.Exp, scale=-0.5)

            def norm(c):
                p = c % 2
                tc = slice(c * 512, (c + 1) * 512)
                for kk in range(8):
                    yk = ybuf[:, kk, tc]
                    TT(yk, yk, mean_sb[p], ALU.subtract, eng="pool" if kk % 2 == 0 else "dve")
                    TT(yk, yk, rstd[p], ALU.mult)
                    if final:
                        ACT(yk, yk, AF.Identity, scale=lnp[:, wi, l, kk:kk + 1], bias=lnp[:, wi + 1, l, kk:kk + 1])
                    else:
                        ACT(xT[:, kk, tc], yk, AF.Identity, scale=lnp[:, wi, l, kk:kk + 1], bias=lnp[:, wi + 1, l, kk:kk + 1])
                if final:
                    for j in range(4):
                        tt = c * 4 + j
                        ot = otile[tt % 2]
                        for half in range(2):
                            pp = P(4 + half, [128, 4, 128])
                            for q in range(4):
                                kk = half * 4 + q
                                TR(pp[:, q, :], ybuf[:, kk, tt * 128:(tt + 1) * 128], identf)
                            CP(ot[:, half * 512:(half + 1) * 512], pp.rearrange("p a b -> p (a b)"), eng="act" if half else "dve")
                        k.outd.append(S.dma("sp", y_d[tt * 128:(tt + 1) * 128, :], ot))

            stats(0)
            fin(0)
            for c in range(4):
                if c + 1 < 4:
                    stats(c + 1)
                norm(c)
                if c + 1 < 4:
                    fin(c + 1)
            S.release(m)

        def moe_stage(l, ybuf):
            m = S.mark()
            wr = S.alloc([128, 8, 20], BF16)
            S.dma("pool", wr[:, :, 0:4], dr["w_group"][l].rearrange("(k p) n -> p k n", p=128))
            S.dma("pool", wr[:, :, 4:20], dr["w_router"][l].rearrange("(k p) n -> p k n", p=128))
            bias = S.alloc([128, 20], F32)
            S.dma("sp", bias[:, 0:4], dr["b_group"][l:l + 1, :].broadcast_to([128, 4]))
            S.dma("sp", bias[:, 4:20], dr["b_router"][l:l + 1, :].broadcast_to([128, 16]))
            PR = P(7, [128, 16, 20])
            for i in range(16):
                for kk in range(8):
                    MM(PR[:, i, :], xT[:, kk, i * 128:(i + 1) * 128], wr[:, kk, :], kk == 0, kk == 7)
            L = S.alloc([128, 16, 20], F32)
            TT(L, PR, bias.unsqueeze(1).broadcast_to([128, 16, 20]), ALU.add)
            Lg = L[:, :, 0:4]
            Le = L[:, :, 4:20]
            gmax = S.alloc([128, 16], F32)
            RED(gmax, Lg, ALU.max)
            gb3 = gmax.unsqueeze(2).broadcast_to([128, 16, 4])
            dg = S.alloc([128, 16, 4], F32)
            TT(dg, Lg, gb3, ALU.subtract)
            ACT(dg, dg, AF.Exp)
            sg = S.alloc([128, 16], F32)
            RED(sg, dg, ALU.add)
            ptop = S.alloc([128, 16], F32)
            RECIP(ptop, sg)
            ohg = S.alloc([128, 16, 4], F32)
            TT(ohg, Lg, gb3, ALU.is_equal)
            TS(ohg, ohg, -1.0, 1.0e4, ALU.add, ALU.mult)
            Lm = S.alloc([128, 16, 16], F32)
            Lm4 = Lm.rearrange("p a (g e) -> p a g e", g=4)
            TT(Lm4, Le.rearrange("p a (g e) -> p a g e", g=4), ohg.unsqueeze(3).broadcast_to([128, 16, 4, 4]), ALU.add)
            m1 = S.alloc([128, 16], F32)
            RED(m1, Lm, ALU.max)
            oh1 = S.alloc([128, 16, 16], F32)
            TT(oh1, Lm, m1.unsqueeze(2).broadcast_to([128, 16, 16]), ALU.is_equal)
            Lm2 = S.alloc([128, 16, 16], F32)
            STT(Lm2, oh1, -1.0e4, Lm, ALU.mult, ALU.add)
            mm2 = S.alloc([128, 16], F32)
            RED(mm2, Lm2, ALU.max)
            oh2 = S.alloc([128, 16, 16], F32)
            TT(oh2, Lm2, mm2.unsqueeze(2).broadcast_to([128, 16, 16]), ALU.is_equal)
            ee = S.alloc([128, 16], F32)
            TT(ee, mm2, m1, ALU.subtract)
            ACT(ee, ee, AF.Exp)
            den = S.alloc([128, 16], F32)
            TS(den, ee, 1.0, None, ALU.add)
            RECIP(den, den)
            g1 = S.alloc([128, 16], F32)
            TT(g1, den, ptop, ALU.mult)
            g2 = S.alloc([128, 16], F32)
            TT(g2, g1, ee, ALU.mult)
            TT(oh1, oh1, g1.unsqueeze(2).broadcast_to([128, 16, 16]), ALU.mult)
            TT(oh2, oh2, g2.unsqueeze(2).broadcast_to([128, 16, 16]), ALU.mult)
            gb = S.alloc([128, 16, 16], BF16)
            TT(gb, oh1, oh2, ALU.add)
            if mode == "gates":
                k.dbg = (oh1, oh2)
            PT = P(5, [16, T], BF16)
            for i in range(16):
                TR(PT[:, i * 128:(i + 1) * 128], gb[:, i, :], identb)
            gatesT = S.alloc([16, T], BF16)
            CP(gatesT, PT)
            wg = [S.alloc([128, 8, 256], BF16) for _ in range(2)]
            wu = [S.alloc([128, 8, 256], BF16) for _ in range(2)]
            wd = [S.alloc([128, 2, 1024], BF16) for _ in range(2)]
            hT = S.alloc([128, 2, T], BF16)
            sgt = [S.alloc([128, 512], F32) for _ in range(2)]
            nblk = [0]
            ndn = [0]
            gpcur = [None]
            ngp = [0]

            def gateup_mm(e, c, ft):
                b = e % 2
                tc = slice(c * 512, (c + 1) * 512)
                bb = nblk[0] % 2
                nblk[0] += 1
                Pg = P(bb * 2, [128, 512])
                Pu = P(bb * 2 + 1, [128, 512])
                for kk in range(8):
                    MM(Pg, wg[b][:, kk, ft * 128:(ft + 1) * 128], xT[:, kk, tc], kk == 0, kk == 7)
                for kk in range(8):
                    MM(Pu, wu[b][:, kk, ft * 128:(ft + 1) * 128], xT[:, kk, tc], kk == 0, kk == 7)
                return (bb, Pg, Pu, ft, tc, gpcur[0])

            def gateup_ev(st):
                bb, Pg, Pu, ft, tc, Gp_ = st
                ACT(sgt[bb], Pg, AF.Silu)
                TT(sgt[bb], sgt[bb], Pu, ALU.mult)
                TT(hT[:, ft, tc], sgt[bb], Gp_, ALU.mult)

            def down(e, c, kks):
                b = e % 2
                tc = slice(c * 512, (c + 1) * 512)
                for kk in kks:
                    Pd = P(5 + ndn[0] % 2, [128, 512])
                    ndn[0] += 1
                    MM(Pd, wd[b][:, 0, kk * 128:(kk + 1) * 128], hT[:, 0, tc], True, False)
                    MM(Pd, wd[b][:, 1, kk * 128:(kk + 1) * 128], hT[:, 1, tc], False, True)
                    TT(ybuf[:, kk, tc], ybuf[:, kk, tc], Pd, ALU.add)

            prev = None
            for e in range(16):
                b = e % 2
                WLOAD(wg[b], dr["w_gate"][l, e])
                WLOAD(wu[b], dr["w_up"][l, e])
                WLOAD(wd[b], dr["w_down"][l, e])
                for c in range(4):
                    tc = slice(c * 512, (c + 1) * 512)
                    Gp = P(4 if ngp[0] % 2 == 0 else 7, [128, 512])
                    ngp[0] += 1
                    gpcur[0] = Gp
                    MM(Gp, selb[0:16, e, :], gatesT[0:16, tc])
                    for ft in range(2):
                        st = gateup_mm(e, c, ft)
                        if prev is not None:
                            down(prev[0], prev[1], range(4 * ft, 4 * ft + 4))
                        gateup_ev(st)
                    prev = (e, c)
            down(prev[0], prev[1], range(0, 8))
            S.release(m)

        def init_ybuf(ybuf):
            for kk in range(8):
                ACT(ybuf[:, kk, :], xT[:, kk, :], AF.Identity, scale=float(ALPHA))


        ones1 = S.alloc([128, 128], BF16)
        MEMSET(ones1, 1.0)
        ones128 = S.alloc([128, 128], BF16)
        MEMSET(ones128, 1.0 / 128.0)
        Ub = S.alloc([128, 128], BF16)
        Lcb = S.alloc([128, 128], BF16)
        S.dma("pool", Ub, dr["c_U"])
        S.dma("pool", Lcb, dr["c_L"])
        memT = S.alloc([128, 8, 256], BF16)

        def MMs(out, lhsT, rhs, start, stop):
            S.op("pe", lambda e: e.matmul(out, lhsT, rhs, start=start, stop=stop, skip_group_check=True), reads=[lhsT, rhs], writes=[out])

        def load_memT():
            m = S.mark()
            mt_ = S.alloc([128, 2, 1024], BF16)
            S.dma("pool", mt_, dr["mem"].rearrange("(a p) d -> p a d", p=128))
            for a in range(2):
                pt = P(2 + a, [128, 8, 128], BF16)
                for kk in range(8):
                    TR(pt[:, kk, :], mt_[:, a, kk * 128:(kk + 1) * 128], identb)
                CP(memT[:, :, a * 128:(a + 1) * 128], pt)
            S.release(m)

        def proj_fm(wsrc, tiles, dst_fn, func=None):
            m = S.mark()
            wb = [S.alloc([128, 8, 128], BF16) for _ in range(3)]
            for n, c0 in enumerate(tiles):
                w = wb[n % 3]
                WLOAD(w, wsrc[:, c0:c0 + 128])
                for c in range(4):
                    tc = slice(c * 512, (c + 1) * 512)
                    pp = P((n * 4 + c) % 2, [128, 512])
                    for kk in range(8):
                        MM(pp, w[:, kk, :], xT[:, kk, tc], kk == 0, kk == 7)
                    if func is not None and func(n) is not None:
                        ACT(dst_fn(n, c), pp, func(n))
                    else:
                        CP(dst_fn(n, c), pp, eng="act" if (n * 4 + c) % 2 else "dve")
            S.release(m)

        def mem_attn(l, qmT, mixedT):
            m = S.mark()
            wm = S.alloc([128, 8, 512], BF16)
            WLOAD(wm, dr["w_mem_kv"][l])
            kmT = S.alloc([128, 2, 256], BF16)
            vm = S.alloc([128, 2, 256], BF16)
            for j in range(2):
                pp = P(2 + j, [128, 256])
                for kk in range(8):
                    MM(pp, wm[:, kk, j * 128:(j + 1) * 128], memT[:, kk, :], kk == 0, kk == 7)
                CP(kmT[:, j, :], pp)
            for mt in range(2):
                pp = P(4 + mt, [128, 256])
                for kk in range(8):
                    MM(pp, memT[:, kk, mt * 128:(mt + 1) * 128], wm[:, kk, 256:512], kk == 0, kk == 7)
                CP(vm[:, mt, :], pp, eng="act")
            em = [S.alloc([128, 512], BF16) for _ in range(4)]
            rec = S.alloc([128, 512], F32)
            n = 0
            for j in range(2):
                for half in range(2):
                    pb = 64 * half
                    for c in range(4):
                        tc = slice(c * 512, (c + 1) * 512)
                        es_ = []
                        for mt in range(2):
                            zp = P(2 + (n % 2), [128, 512])
                            MM(zp, kmT[pb:pb + 64, j, mt * 128:(mt + 1) * 128], qmT[pb:pb + 64, j, tc])
                            eb = em[n % 4]
                            ACT(eb, zp, AF.Exp, scale=0.125)
                            es_.append(eb)
                            n += 1
                        pn = P(4, [128, 512])
                        pd = P(5, [128, 512])
                        for mt in range(2):
                            MM(pn, vm[:, mt, j * 128:(j + 1) * 128], es_[mt], mt == 0, mt == 1)
                        for mt in range(2):
                            MM(pd, ones1, es_[mt], mt == 0, mt == 1)
                        ACT(rec[pb:pb + 64, :], pd[pb:pb + 64, :], AF.Ln)
                        ACT(rec[pb:pb + 64, :], rec[pb:pb + 64, :], AF.Exp, scale=-1.0)
                        TT(mixedT[pb:pb + 64, 6 + j, tc], pn[pb:pb + 64, :], rec[pb:pb + 64, :], ALU.mult)
            S.release(m)

        def wo_stage(l, mixedT, ybuf):
            m = S.mark()
            wob = [S.alloc([128, 8, 256], BF16) for _ in range(2)]
            n = 0
            for q in range(4):
                w = wob[q % 2]
                WLOAD(w, dr["w_o"][l][:, q * 256:(q + 1) * 256])
                for kq in range(2):
                    kk = 2 * q + kq
                    for c in range(4):
                        tc = slice(c * 512, (c + 1) * 512)
                        pp = P(n % 2, [128, 512])
                        n += 1
                        for f in range(8):
                            MM(pp, w[:, f, kq * 128:(kq + 1) * 128], mixedT[:, f, tc], f == 0, f == 7)
                        STT(ybuf[:, kk, tc], xT[:, kk, tc], ALPHA, pp, ALU.mult, ALU.add)
            S.release(m)

        def kv_stage(kT, vtok):
            proj_fm(dr["w_kv_shared"], [j * 128 for j in range(6)], lambda n, c: kT[:, n, c * 512:(c + 1) * 512])
            m = S.mark()
            wv = S.alloc([128, 8, 768], BF16)
            WLOAD(wv, dr["w_kv_shared"][:, 768:1536])
            for i in range(16):
                ts_ = slice(i * 128, (i + 1) * 128)
                pa = P(2 + 2 * (i % 2), [128, 512])
                pb_ = P(3 + 2 * (i % 2), [128, 256])
                for kk in range(8):
                    MM(pa, xT[:, kk, ts_], wv[:, kk, 0:512], kk == 0, kk == 7)
                for kk in range(8):
                    MM(pb_, xT[:, kk, ts_], wv[:, kk, 512:768], kk == 0, kk == 7)
                CP(vtok[:, i, 0:512], pa, eng="act")
                CP(vtok[:, i, 512:768], pb_, eng="dve")
            S.release(m)

        def sb_attn(qT, kT, vtok, mixedT):
            m = S.mark()
            dm = S.alloc([128, 4, 512], BF16)
            dmf = S.alloc([128, 128], F32)
            S.dma("sp", dmf, dr["c_dmask"])
            for q in range(4):
                if q > 0:
                    MEMSET(dm[:, q, 0:q * 128], 0.0)
                CP(dm[:, q, q * 128:(q + 1) * 128], dmf)
                if q < 3:
                    MEMSET(dm[:, q, (q + 1) * 128:512], 1.0)
            yo = [32768]

            def ya(shape, dt):
                nb = int(np.prod(shape[1:])) * esz(dt)
                v = yview(yo[0], shape, dt)
                yo[0] += nb
                assert yo[0] <= 65536
                return v
            REF, RLF, RLB, RAB = 5, 3, 4, 3
            ef = [ya([128, 512], F32) for _ in range(REF)]
            Lf = [ya([128, 512], F32) for _ in range(RLF)]
            Lb = [ya([128, 512], BF16) for _ in range(RLB)]
            ab = [ya([128, 512], BF16) for _ in range(RAB)]
            zbanks = [0, 1, 6, 7]
            items = []
            for j in range(6):
                for c in range(4):
                    top = 4 * c + 3
                    for I in range(top, -1, -1):
                        for half in range(2):
                            items.append((j, half, c, I, half, I == top, I == 0))
            N = len(items)

            def Zb(n):
                return P(zbanks[n % 4], [128, 512])

            def accb(n):
                return P(2 + items[n][4], [128, 512])

            def Ob(n):
                return P(4 + items[n][4], [128, 512])

            def s0(n):
                j, half, c, I, _, _, _ = items[n]
                pb = 64 * half
                MM(Zb(n), kT[pb:pb + 64, j, I * 128:(I + 1) * 128], qT[pb:pb + 64, j, c * 512:(c + 1) * 512])

            def s1(n):
                ACT(ef[n % REF], Zb(n), AF.Exp, scale=0.125)
                ACT(Lf[n % RLF], ef[n % REF], AF.Ln, bias=1.0)

            def s2(n):
                j, half, c, I, _, _, _ = items[n]
                if I >= 4 * c:
                    TT(Lb[n % RLB], Lf[n % RLF], dm[:, I - 4 * c, :], ALU.mult)
                else:
                    CP(Lb[n % RLB], Lf[n % RLF], eng="pool")
                STT(ef[n % REF], Zb(n), 0.125, Lf[n % RLF], ALU.mult, ALU.subtract)

            def s3a(n):
                MMs(accb(n), Ub, Lb[n % RLB], items[n][5], True)

            def s3b(n):
                TT(ef[n % REF], ef[n % REF], accb(n), ALU.subtract)

            def s3c(n):
                MMs(accb(n), Lcb, Lb[n % RLB], False, True)

            def s4(n):
                j, half, c, I, _, _, _ = items[n]
                ACT(ab[n % RAB], ef[n % REF], AF.Exp)
                if I >= 4 * c:
                    TT(ab[n % RAB], ab[n % RAB], dm[:, I - 4 * c, :], ALU.mult, eng="pool")

            def s5(n):
                j, half, c, I, _, first, last = items[n]
                MMs(Ob(n), vtok[:, I, j * 128:(j + 1) * 128], ab[n % RAB], first, last)
                if last:
                    pb = 64 * half
                    CP(mixedT[pb:pb + 64, j, c * 512:(c + 1) * 512], Ob(n)[pb:pb + 64, :], eng="act")

            def ok(n):
                return 0 <= n < N
            for t in range(N + 7):
                if ok(t - 5):
                    s4(t - 5)
                if ok(t):
                    s0(t)
                if ok(t - 1):
                    s1(t - 1)
                if ok(t - 2):
                    s2(t - 2)
                if ok(t - 5):
                    s3c(t - 5)
                if ok(t - 3):
                    s3a(t - 3)
                if ok(t - 4):
                    s3b(t - 4)
                if ok(t - 6):
                    s5(t - 6)
            S.release(m)

        def yview(off, shape, dt):
            return S.view(k.ybuf_off + off, shape, dt)

        def b_mix(l, kT, vtok, ybuf):
            m = S.mark()
            qT = yview(0, [128, 6, T], BF16)
            qmT = yview(6 * T * 2, [128, 2, T], BF16)
            proj_fm(dr["b_w_in"][l - 2], [j * 128 for j in range(8)],
                    lambda n, c: (qT[:, n, c * 512:(c + 1) * 512] if n < 6 else qmT[:, n - 6, c * 512:(c + 1) * 512]))
            mixedT = S.alloc([128, 8, T], BF16)
            mem_attn(l, qmT, mixedT)
            sb_attn(qT, kT, vtok, mixedT)
            if mode == "Bmix":
                k.dbg_mixedT = mixedT
                return
            wo_stage(l, mixedT, ybuf)
            S.release(m)

        def a_mix(l, ybuf):
            import os
            m = S.mark()
            qT = yview(0, [128, 6, T], BF16)
            gT = yview(6 * T * 2, [128, 6, T], BF16)
            qmT = yview(12 * T * 2, [128, 2, T], BF16)
            tiles = [j * 128 for j in range(6)] + [2304 + j * 128 for j in range(6)] + [3072, 3200]

            def dst(n, c):
                tc = slice(c * 512, (c + 1) * 512)
                if n < 6:
                    return qT[:, n, tc]
                if n < 12:
                    return gT[:, n - 6, tc]
                return qmT[:, n - 12, tc]
            proj_fm(dr["a_w_in"][l], tiles, dst, func=lambda n: AF.Silu if n < 12 else None)
            mixedT = S.alloc([128, 8, T], BF16)
            mem_attn(l, qmT, mixedT)
            Wfi = S.alloc([128, 8, 1536], BF16)
            if os.environ.get("NOWFI"):
                MEMSET(Wfi, 0.01)
            for ch_ in range(0 if os.environ.get("NOWFI") else 6):
                WLOAD(Wfi[:, :, ch_ * 256:(ch_ + 1) * 256], dr["a_w_in"][l][:, 768 + ch_ * 256:768 + (ch_ + 1) * 256])
            c1bc = S.alloc([128, 768], F32)
            c1fm = S.alloc([128, 6], F32)
            gnfm = S.alloc([128, 6], F32)
            S.dma("sp", gnfm, dr["a_gnorm"][l].rearrange("(h p) -> p h", p=128), allow_slow_non_contiguous=True)
            if l == 0:
                MEMSET(c1bc, 1.0)
                MEMSET(c1fm, 1.0)
            else:
                mk_ = S.mark()
                a1bc = S.alloc([128, 768], F32)
                S.release(mk_)
                S.dma("sp", c1bc, dr["a_lower_bounds"][0:1, :].broadcast_to([128, 768]))
                S.dma("sp", a1bc, dr["a_lower_bounds"][1:2, :].broadcast_to([128, 768]))
                TT(c1bc, c1bc, a1bc, ALU.subtract)
                ACT(c1bc, c1bc, AF.Sigmoid)
                a1fm = S.alloc([128, 6], F32)
                S.dma("sp", c1fm, dr["a_lower_bounds"][0].rearrange("(h p) -> p h", p=128), allow_slow_non_contiguous=True)
                S.dma("sp", a1fm, dr["a_lower_bounds"][1].rearrange("(h p) -> p h", p=128), allow_slow_non_contiguous=True)
                TT(c1fm, c1fm, a1fm, ALU.subtract)
                ACT(c1fm, c1fm, AF.Sigmoid)
            Mf = S.alloc([128, 132], F32)
            S.dma("sp", Mf, dr["c_M"])
            hmf = S.alloc([128, 128], F32)
            S.dma("sp", hmf, dr["c_hmask"])
            Sst = S.alloc([128, 6, 128], F32)
            MEMSET(Sst, 0.0)
            sneg = S.alloc([128, 768], F32)
            wtmp = S.alloc([128, 768], F32)
            einv = wtmp
            logf2 = [S.alloc([128, 768], F32) for _ in range(2)]
            ktok2 = [S.alloc([128, 768], BF16) for _ in range(2)]
            vtk2 = [S.alloc([128, 768], BF16) for _ in range(2)]
            E = [S.alloc([128, 132], F32) for _ in range(2)]
            qt = [S.alloc([128, 128], BF16) for _ in range(2)]
            kt = [S.alloc([128, 128], BF16) for _ in range(2)]
            sc = [S.alloc([128, 128], BF16) for _ in range(2)]
            SpA = [S.alloc([128, 128], BF16) for _ in range(2)]
            SpB = [S.alloc([128, 128], BF16) for _ in range(2)]
            SA2 = [S.alloc([128, 128], F32) for _ in range(2)]
            ptmp2 = [S.alloc([128, 128], F32) for _ in range(2)]
            sAB2 = [S.alloc([128, 2], F32) for _ in range(2)]
            osq = [S.alloc([128, 128], BF16) for _ in range(2)]
            rs = [S.alloc([128, 128], F32) for _ in range(2)]
            o12 = [S.alloc([128, 128], F32) for _ in range(2)]

            def TM(i, part):
                ts_ = slice(i * 128, (i + 1) * 128)
                logf, ktok, vtk = logf2[i % 2], ktok2[i % 2], vtk2[i % 2]
                pf = [P(0, [128, 512]), P(1, [128, 512]), P(2, [128, 512])]
                if part == 0:
                    for ch in range(3):
                        for kk in range(8):
                            MM(pf[ch], xT[:, kk, ts_], Wfi[:, kk, ch * 512:(ch + 1) * 512], kk == 0, kk == 7)
                elif part == 1:
                    ACT(sneg[:, 0:512], pf[0], AF.Exp)
                    ACT(sneg[:, 512:768], pf[1][:, 0:256], AF.Exp)
                    CP(vtk[:, 0:256], pf[1][:, 256:512])
                    CP(vtk[:, 256:768], pf[2])
                    ACT(sneg, sneg, AF.Ln, bias=1.0)
                    ACT(sneg, sneg, AF.Exp, scale=-1.0)
                elif part == 2:
                    TT(wtmp, sneg, c1bc, ALU.mult)
                    ACT(logf, wtmp, AF.Ln, scale=-1.0, bias=1.0)
                elif part == 3:
                    pdk = [P(0, [128, 512]), P(1, [128, 256])]
                    MM(pdk[0][:, 0:256], Mf[:, 0:128], logf[:, 0:256])
                    MM(pdk[0][:, 256:512], Mf[:, 0:128], logf[:, 256:512])
                    MM(pdk[1], Mf[:, 0:128], logf[:, 512:768])
                else:
                    pdk = [P(0, [128, 512]), P(1, [128, 256])]
                    ACT(einv[:, 0:512], pdk[0], AF.Exp, scale=-1.0)
                    ACT(einv[:, 512:768], pdk[1], AF.Exp, scale=-1.0)
                    TT(ktok, sneg, einv, ALU.mult)

            def hp(n):
                i, h = divmod(n, 6)
                r = n % 2
                X = 3 + 2 * r
                Y = 4 + 2 * r
                d = dict(i=i, h=h, r=r, ts=slice(i * 128, (i + 1) * 128), hs=slice(h * 128, (h + 1) * 128),
                         logf=logf2[i % 2], ktok=ktok2[i % 2], vtk=vtk2[i % 2],
                         PD=P(X, [128, 132]), PK=P(X, [128, 128], BF16, off=768),
                         PO=P(X, [128, 128], F32, off=1024), PM=P(X, [128, 128], F32, off=1536),
                         PS=P(Y, [128, 128], F32), PA=P(Y, [128, 128], F32, off=512), PB=P(Y, [128, 128], F32, off=1024))
                return d

            def A1(n):
                d = hp(n)
                MM(d["PA"], d["ktok"][0:64, d["hs"]], d["vtk"][0:64, d["hs"]])
                MM(d["PD"], d["logf"][:, d["hs"]], Mf)
                MM(d["PB"], d["ktok"][64:128, d["hs"]], d["vtk"][64:128, d["hs"]])
                TR(d["PK"], d["ktok"][:, d["hs"]], identb)

            def A2(n):
                d = hp(n)
                r, h = d["r"], d["h"]
                ACT(E[r], d["PD"], AF.Exp)
                ACT(kt[r], d["PK"], AF.Identity, scale=c1fm[:, h:h + 1])
                STT(qt[r], qT[:, h, d["ts"]], 128.0 ** -0.5, E[r][:, 0:128], ALU.mult, ALU.mult)
                TS(sAB2[r][:, 0:1], E[r][:, 63:64], c1fm[:, h:h + 1], None, ALU.mult)
                TS(sAB2[r][:, 1:2], E[r][:, 127:128], c1fm[:, h:h + 1], None, ALU.mult)
                MM(d["PS"], kt[r], qt[r])

            def B1(n):
                d = hp(n)
                r, h = d["r"], d["h"]
                TT(sc[r], d["PS"], hmf, ALU.mult)
                ACT(SpA[r], Sst[:, h, :], AF.Identity, scale=E[r][:, 128:129])
                TS(ptmp2[r], d["PA"], sAB2[r][:, 0:1], None, ALU.mult)
                STT(SA2[r], Sst[:, h, :], E[r][:, 129:130], ptmp2[r], ALU.mult, ALU.add)
                ACT(SpB[r], SA2[r], AF.Identity, scale=E[r][:, 130:131])
                TS(ptmp2[r], d["PB"], sAB2[r][:, 1:2], None, ALU.mult)
                STT(Sst[:, h, :], SA2[r], E[r][:, 131:132], ptmp2[r], ALU.mult, ALU.add)
                MMs(d["PO"], d["vtk"][:, d["hs"]], sc[r], True, True)
                MMs(d["PO"][:, 0:64], SpA[r], qt[r][:, 0:64], False, True)
                MMs(d["PO"][:, 64:128], SpB[r], qt[r][:, 64:128], False, True)
                ACT(osq[r], d["PO"], AF.Square)

            def B2(n):
                d = hp(n)
                r, h = d["r"], d["h"]
                MM(d["PM"], ones128, osq[r])
                ACT(rs[r], d["PM"], AF.Ln, bias=1e-6)
                ACT(rs[r], rs[r], AF.Exp, scale=-0.5)
                STT(o12[r], d["PO"], gnfm[:, h:h + 1], rs[r], ALU.mult, ALU.mult)
                TT(mixedT[:, h, d["ts"]], o12[r], gT[:, h, d["ts"]], ALU.mult)

            for part in range(5):
                TM(0, part)
            NH = 96
            A1(0)
            A2(0)
            for n in range(NH):
                i, h = divmod(n, 6)
                if n + 1 < NH and (n + 1) % 6 != 0:
                    A1(n + 1)
                if i + 1 < 16 and h < 5:
                    TM(i + 1, h)
                if n + 1 < NH and (n + 1) % 6 == 0:
                    A1(n + 1)
                B1(n)
                if n + 1 < NH:
                    A2(n + 1)
                B2(n)
            if mode == "Amix":
                k.dbg_mixedT = mixedT
                return
            wo_stage(l, mixedT, ybuf)
            S.release(m)

        def dump_fm(src):
            m = S.mark()
            ot = [S.alloc([128, 1024], F32) for _ in range(2)]
            for tt in range(16):
                o = ot[tt % 2]
                pp = P(tt % 2, [128, 8, 128], BF16)
                for kk in range(8):
                    TR(pp[:, kk, :], src[:, kk, tt * 128:(tt + 1) * 128], identb)
                CP(o, pp.rearrange("p a b -> p (a b)"))
                k.outd.append(S.dma("sp", y_d[tt * 128:(tt + 1) * 128, :], o))
            S.release(m)

        k.outd = []
        load_xT()
        load_memT()
        k.ybuf_off = S.mark()
        ybuf = S.alloc([128, 8, T], F32)
        if mode == "moe0":
            init_ybuf(ybuf)
            moe_stage(0, ybuf)
            ln_stage(ybuf, 2, 0, final=True)
        elif mode == "ln0":
            init_ybuf(ybuf)
            ln_stage(ybuf, 2, 0, final=True)
        elif mode == "Bmix":
            kT = S.alloc([128, 6, T], BF16)
            vtok = S.alloc([128, 16, 768], BF16)
            kv_stage(kT, vtok)
            b_mix(2, kT, vtok, ybuf)
            dump_fm(k.dbg_mixedT)
        elif mode == "Amix":
            a_mix(0, ybuf)
            dump_fm(k.dbg_mixedT)
        else:
            kT = vtok = None
            nl = 4 if mode == "full" else int(mode[1:])
            for l in range(nl):
                if l < 2:
                    a_mix(l, ybuf)
                else:
                    if l == 2:
                        kT = S.alloc([128, 6, T], BF16)
                        vtok = S.alloc([128, 16, 768], BF16)
                        kv_stage(kT, vtok)
                    b_mix(l, kT, vtok, ybuf)
                ln_stage(ybuf, 0, l)
                init_ybuf(ybuf)
                moe_stage(l, ybuf)
                ln_stage(ybuf, 2, l, final=(l == nl - 1))
        S.final_wait("sp", k.outd)
        S.emit(sems)
        k.nops = {e: len(S.ops[e]) for e in S.ENG}
    return nc, cs, k


_CACHE = {}


def kernel(**inputs):
    if "nc" not in _CACHE:
        _CACHE["nc"] = build("full")
    nc, cs, _k = _CACHE["nc"]
    x = np.ascontiguousarray(inputs["x"], dtype=np.float32)
    mem = np.ascontiguousarray(inputs["mem"], dtype=np.float32)
    in_maps = []
    for b in range(8):
        m = {"x": x[b], "mem": mem[b]}
        for nm, _shp in WSPEC:
            m[nm] = np.ascontiguousarray(inputs[nm], dtype=np.float32)
        m.update(cs)
        in_maps.append(m)
    res = run_bass_kernel_spmd(nc, in_maps, core_ids=list(range(8)))
    return np.stack([np.asarray(r["y"], dtype=np.float32) for r in res.results], axis=0)
```
